# Optimizing a Trainium2 kernel written in Bass

```python
import math
import jax
import jax.numpy as jnp
from jax import lax
import numpy as np

D_MODEL = 1024
BATCH = 4
SEQ = 8192
DEPTH = 2

MIX_WIDTH = D_MODEL
BRANCH_W = MIX_WIDTH // 4
S5_GROUP = 16
S5_GROUPS = BRANCH_W // S5_GROUP
S5_STATE = 64
GMLP_HEADS = 4
GMLP_HEAD_DIM = BRANCH_W // GMLP_HEADS
GMLP_CHUNK = 128
GDN_HEAD_DIM = 64
GDN_HEADS = BRANCH_W // GDN_HEAD_DIM
GDN_CONV = 4
GDN_CHUNK = 64
SC_CONV = 3
IN_SIZES = (BRANCH_W,
            2 * BRANCH_W,
            3 * BRANCH_W,
            BRANCH_W,
            GDN_HEADS,
            GDN_HEADS,
            3 * BRANCH_W)
IN_COLS = sum(IN_SIZES)
D_FF = 2816
N_EXPERTS = 8
TOP_K = 2
D_FF_EXPERT = 3584
MOE_BLOCK = 512
N_DENSE = (DEPTH + 1) // 2
N_MOE = DEPTH // 2
EPS = 1e-6

kernel_name = 'hybrid_parallel_s5_gmlp_gdn_shortconv_moe'


def rmsnorm(x, g):
    xf = x.astype(jnp.float32)
    y = xf * lax.rsqrt(jnp.mean(xf * xf, axis=-1, keepdims=True) + EPS)
    return (y * g.astype(jnp.float32)).astype(x.dtype)


def layernorm(x, g, b):
    xf = x.astype(jnp.float32)
    xc = xf - jnp.mean(xf, axis=-1, keepdims=True)
    y = xc * lax.rsqrt(jnp.mean(xc * xc, axis=-1, keepdims=True) + EPS)
    return (y * g.astype(jnp.float32) + b.astype(jnp.float32)).astype(x.dtype)


def l2norm(x):
    return x * lax.rsqrt(jnp.sum(x * x, axis=-1, keepdims=True) + EPS)


def split_cols(p, sizes):
    out, start = [], 0
    for s in sizes:
        out.append(p[..., start:start + s])
        start += s
    return out


def causal_dwconv(x, w):
    k, c = w.shape
    return lax.conv_general_dilated(x, w[:, None, :].astype(x.dtype), window_strides=(1,),
                                    padding=[(k - 1, 0)], dimension_numbers=('NWC', 'WIO', 'NWC'),
                                    feature_group_count=c)


def s5_mixer(u, lam_re, lam_im, log_step, b_re, b_im, c_re, c_im, d_skip, w_glu, b_glu):
    f32 = jnp.float32
    bsz, seqlen, _ = u.shape
    uf = u.astype(f32)
    ug = uf.reshape(bsz, seqlen, S5_GROUPS, S5_GROUP)
    lr, li = lam_re.astype(f32), lam_im.astype(f32)
    dt = jnp.exp(log_step.astype(f32))[:, None]
    mag = jnp.exp(lr * dt)
    ar, ai = mag * jnp.cos(li * dt), mag * jnp.sin(li * dt)
    den = lr * lr + li * li
    fr = ((ar - 1.0) * lr + ai * li) / den
    fi = (ai * lr - (ar - 1.0) * li) / den
    br, bi = b_re.astype(f32), b_im.astype(f32)
    bbr = fr[..., None] * br - fi[..., None] * bi
    bbi = fr[..., None] * bi + fi[..., None] * br
    bu_r = jnp.einsum('blgp,gnp->blgn', ug, bbr)
    bu_i = jnp.einsum('blgp,gnp->blgn', ug, bbi)
    a_r = jnp.broadcast_to(ar, bu_r.shape)
    a_i = jnp.broadcast_to(ai, bu_i.shape)

    def combine(e1, e2):
        a1r, a1i, b1r, b1i = e1
        a2r, a2i, b2r, b2i = e2
        return (a2r * a1r - a2i * a1i, a2r * a1i + a2i * a1r,
                a2r * b1r - a2i * b1i + b2r, a2r * b1i + a2i * b1r + b2i)

    _, _, s_r, s_i = lax.associative_scan(combine, (a_r, a_i, bu_r, bu_i), axis=1)
    y = (jnp.einsum('blgn,gpn->blgp', s_r, c_re.astype(f32))
         - jnp.einsum('blgn,gpn->blgp', s_i, c_im.astype(f32)))
    y = y.reshape(bsz, seqlen, BRANCH_W) + d_skip.astype(f32) * uf
    y = jax.nn.gelu(y)
    y = y * jax.nn.sigmoid(y @ w_glu.astype(f32) + b_glu.astype(f32))
    return y.astype(u.dtype)


def gmlp_mixer(p, ln_g, ln_b, w_sp, b_sp):
    bsz, seqlen, _ = p.shape
    z = jax.nn.gelu(p)
    u, v = z[..., :BRANCH_W], z[..., BRANCH_W:]
    v = layernorm(v, ln_g, ln_b)
    n = seqlen // GMLP_CHUNK
    vc = v.reshape(bsz, n, GMLP_CHUNK, GMLP_HEADS, GMLP_HEAD_DIM)
    mask = jnp.tril(jnp.ones((GMLP_CHUNK, GMLP_CHUNK), dtype=bool))
    w = jnp.where(mask, w_sp, 0.0).astype(v.dtype)
    s = jnp.einsum('hts,bnshd->bnthd', w, vc) + b_sp.T[:, :, None].astype(v.dtype)
    return u * s.reshape(bsz, seqlen, BRANCH_W)


def gated_delta_rule(q, k, v, g, beta):
    bsz, seqlen, nh, dk = q.shape
    dv = v.shape[-1]
    c = GDN_CHUNK
    n = seqlen // c

    def chunked(t):
        return jnp.moveaxis(t.reshape(bsz, n, c, nh, -1), 3, 1)

    q = chunked(q) * (dk ** -0.5)
    k = chunked(k)
    v = chunked(v)
    g = chunked(g[..., None])[..., 0]
    beta = chunked(beta[..., None])[..., 0]
    G = jnp.cumsum(g, axis=-1)
    causal = jnp.tril(jnp.ones((c, c), dtype=bool))
    strict = jnp.tril(jnp.ones((c, c), dtype=bool), k=-1)
    decay = jnp.exp(jnp.where(causal, G[..., :, None] - G[..., None, :], -jnp.inf))
    a_mat = jnp.where(strict, beta[..., None] * jnp.einsum('bhnid,bhnjd->bhnij', k, k) * decay, 0.0)
    eye = jnp.eye(c, dtype=q.dtype)
    rhs = jnp.concatenate([v * beta[..., None], k * (beta * jnp.exp(G))[..., None]], axis=-1)
    sol = lax.linalg.triangular_solve(eye + a_mat, rhs, left_side=True, lower=True, unit_diagonal=True)
    w_v, w_k = sol[..., :dv], sol[..., dv:]
    qk = jnp.einsum('bhnid,bhnjd->bhnij', q, k) * decay
    q_dec = q * jnp.exp(G)[..., None]
    k_dec = k * jnp.exp(G[..., -1:] - G)[..., None]
    g_tot = jnp.exp(G[..., -1])

    def step(state, xs):
        wv_i, wk_i, qk_i, qd_i, kd_i, gt_i = xs
        v_new = wv_i - jnp.einsum('bhck,bhkv->bhcv', wk_i, state)
        o_i = jnp.einsum('bhck,bhkv->bhcv', qd_i, state) + jnp.einsum('bhcs,bhsv->bhcv', qk_i, v_new)
        state = state * gt_i[..., None, None] + jnp.einsum('bhck,bhcv->bhkv', kd_i, v_new)
        return state, o_i

    xs = tuple(jnp.moveaxis(t, 2, 0) for t in (w_v, w_k, qk, q_dec, k_dec, g_tot))
    s0 = jnp.zeros((bsz, nh, dk, dv), q.dtype)
    _, o = lax.scan(step, s0, xs)
    return jnp.transpose(o, (1, 0, 3, 2, 4)).reshape(bsz, seqlen, nh, dv)


def gdn_mixer(p_qkv, p_z, p_a, p_b, conv_w, a_log, dt_bias, norm_g):
    f32 = jnp.float32
    bsz, seqlen, _ = p_qkv.shape
    qkv = jax.nn.silu(causal_dwconv(p_qkv, conv_w)).astype(f32)
    shp = (bsz, seqlen, GDN_HEADS, GDN_HEAD_DIM)
    q = l2norm(qkv[..., :BRANCH_W].reshape(shp))
    k = l2norm(qkv[..., BRANCH_W:2 * BRANCH_W].reshape(shp))
    v = qkv[..., 2 * BRANCH_W:].reshape(shp)
    beta = jax.nn.sigmoid(p_b.astype(f32))
    g = -jnp.exp(a_log.astype(f32)) * jax.nn.softplus(p_a.astype(f32) + dt_bias.astype(f32))
    o = gated_delta_rule(q, k, v, g, beta)
    o = rmsnorm(o, norm_g) * jax.nn.silu(p_z.astype(f32).reshape(shp))
    return o.reshape(bsz, seqlen, BRANCH_W).astype(p_qkv.dtype)


def shortconv_mixer(p, conv_w):
    b_gate, c_gate, xin = p[..., :BRANCH_W], p[..., BRANCH_W:2 * BRANCH_W], p[..., 2 * BRANCH_W:]
    return b_gate * causal_dwconv(c_gate * xin, conv_w)


def hybrid_mixer(h, w_in, s5_lam_re, s5_lam_im, s5_log_step, s5_b_re, s5_b_im, s5_c_re, s5_c_im,
                 s5_d, s5_w_glu, s5_b_glu, s5_out_norm, sgu_ln_g, sgu_ln_b, sgu_w, sgu_b,
                 gmlp_out_norm, gdn_conv, gdn_a_log, gdn_dt_bias, gdn_norm, sc_conv, sc_out_norm, w_out):
    p = h @ w_in
    p_s5, p_gm, p_qkv, p_z, p_a, p_b, p_sc = split_cols(p, IN_SIZES)
    y_s5 = rmsnorm(s5_mixer(p_s5, s5_lam_re, s5_lam_im, s5_log_step, s5_b_re, s5_b_im,
                            s5_c_re, s5_c_im, s5_d, s5_w_glu, s5_b_glu), s5_out_norm)
    y_gm = rmsnorm(gmlp_mixer(p_gm, sgu_ln_g, sgu_ln_b, sgu_w, sgu_b), gmlp_out_norm)
    y_gdn = gdn_mixer(p_qkv, p_z, p_a, p_b, gdn_conv, gdn_a_log, gdn_dt_bias, gdn_norm)
    y_sc = rmsnorm(shortconv_mixer(p_sc, sc_conv), sc_out_norm)
    return jnp.concatenate([y_s5, y_gm, y_gdn, y_sc], axis=-1) @ w_out


def swiglu(x, w_gate, w_up, w_down):
    return (jax.nn.silu(x @ w_gate) * (x @ w_up)) @ w_down


def moe_swiglu(x2, w_router, w_gate, w_up, w_down):
    t = x2.shape[0]
    n_assign = t * TOP_K
    logits = (x2 @ w_router).astype(jnp.float32)
    top_logit, top_idx = lax.top_k(logits, TOP_K)
    gates = jax.nn.softmax(top_logit, axis=-1)
    flat_e = top_idx.reshape(-1)
    order = jnp.argsort(flat_e)
    sorted_e = flat_e[order]
    tok = order // TOP_K
    counts = jnp.bincount(flat_e, length=N_EXPERTS)
    starts = jnp.cumsum(counts) - counts
    padded = (counts + MOE_BLOCK - 1) // MOE_BLOCK * MOE_BLOCK
    pad_ends = jnp.cumsum(padded)
    pad_starts = pad_ends - padded
    dest = pad_starts[sorted_e] + (jnp.arange(n_assign) - starts[sorted_e])
    n_blocks = -(-n_assign // MOE_BLOCK) + N_EXPERTS
    buf = jnp.zeros((n_blocks * MOE_BLOCK, x2.shape[1]), x2.dtype).at[dest].set(x2[tok])
    block_e = jnp.minimum(jnp.searchsorted(pad_ends, jnp.arange(n_blocks) * MOE_BLOCK, side='right'),
                          N_EXPERTS - 1)

    def run(args):
        xb, e = args
        return swiglu(xb, w_gate[e], w_up[e], w_down[e])

    y_buf = lax.map(run, (buf.reshape(n_blocks, MOE_BLOCK, -1), block_e)).reshape(n_blocks * MOE_BLOCK, -1)
    y = y_buf[dest] * gates.reshape(-1)[order][:, None].astype(x2.dtype)
    return jnp.zeros_like(x2).at[tok].add(y)


def setup_inputs(seed: int = 0) -> dict:
    key = jax.random.key(seed)
    keys = iter(jax.random.split(key, 40))
    f32 = jnp.float32

    def nrm(shape, scale):
        return scale * jax.random.normal(next(keys), shape, f32)

    def gain(shape):
        return 1.0 + 0.02 * jax.random.normal(next(keys), shape, f32)

    L = DEPTH
    n_idx = jnp.arange(S5_STATE, dtype=f32)
    dt = jnp.exp(jax.random.uniform(next(keys), (L, GDN_HEADS), f32, math.log(1e-3), math.log(1e-1)))
    return {
        'x': nrm((BATCH, SEQ, D_MODEL), 1.0),
        'mix_norm': gain((L, D_MODEL)),
        'w_in': nrm((L, D_MODEL, IN_COLS), D_MODEL ** -0.5),
        's5_lam_re': -0.5 + nrm((L, S5_GROUPS, S5_STATE), 0.01),
        's5_lam_im': math.pi * n_idx + nrm((L, S5_GROUPS, S5_STATE), 0.01),
        's5_log_step': jax.random.uniform(next(keys), (L, S5_GROUPS), f32, math.log(1e-3), math.log(1e-1)),
        's5_b_re': nrm((L, S5_GROUPS, S5_STATE, S5_GROUP), (2 * S5_GROUP) ** -0.5),
        's5_b_im': nrm((L, S5_GROUPS, S5_STATE, S5_GROUP), (2 * S5_GROUP) ** -0.5),
        's5_c_re': nrm((L, S5_GROUPS, S5_GROUP, S5_STATE), 0.5),
        's5_c_im': nrm((L, S5_GROUPS, S5_GROUP, S5_STATE), 0.5),
        's5_d': nrm((L, BRANCH_W), 1.0),
        's5_w_glu': nrm((L, BRANCH_W, BRANCH_W), BRANCH_W ** -0.5),
        's5_b_glu': nrm((L, BRANCH_W), 0.02),
        's5_out_norm': gain((L, BRANCH_W)),
        'sgu_ln_g': gain((L, BRANCH_W)),
        'sgu_ln_b': nrm((L, BRANCH_W), 0.02),
        'sgu_w': nrm((L, GMLP_HEADS, GMLP_CHUNK, GMLP_CHUNK), 0.5 * GMLP_CHUNK ** -0.5),
        'sgu_b': 1.0 + nrm((L, GMLP_HEADS, GMLP_CHUNK), 0.1),
        'gmlp_out_norm': gain((L, BRANCH_W)),
        'gdn_conv': nrm((L, GDN_CONV, 3 * BRANCH_W), GDN_CONV ** -0.5),
        'gdn_a_log': jnp.log(jax.random.uniform(next(keys), (L, GDN_HEADS), f32, 1.0, 16.0)),
        'gdn_dt_bias': dt + jnp.log(-jnp.expm1(-dt)),
        'gdn_norm': gain((L, GDN_HEAD_DIM)),
        'sc_conv': nrm((L, SC_CONV, BRANCH_W), SC_CONV ** -0.5),
        'sc_out_norm': gain((L, BRANCH_W)),
        'w_out': nrm((L, MIX_WIDTH, D_MODEL), MIX_WIDTH ** -0.5),
        'ffn_norm': gain((L, D_MODEL)),
        'ffn_w_gate': nrm((N_DENSE, D_MODEL, D_FF), D_MODEL ** -0.5),
        'ffn_w_up': nrm((N_DENSE, D_MODEL, D_FF), D_MODEL ** -0.5),
        'ffn_w_down': nrm((N_DENSE, D_FF, D_MODEL), D_FF ** -0.5),
        'moe_router': nrm((N_MOE, D_MODEL, N_EXPERTS), D_MODEL ** -0.5),
        'moe_w_gate': nrm((N_MOE, N_EXPERTS, D_MODEL, D_FF_EXPERT), D_MODEL ** -0.5),
        'moe_w_up': nrm((N_MOE, N_EXPERTS, D_MODEL, D_FF_EXPERT), D_MODEL ** -0.5),
        'moe_w_down': nrm((N_MOE, N_EXPERTS, D_FF_EXPERT, D_MODEL), D_FF_EXPERT ** -0.5),
        'final_norm': gain((D_MODEL,)),
    }


def reference(x, mix_norm, w_in, s5_lam_re, s5_lam_im, s5_log_step, s5_b_re, s5_b_im, s5_c_re, s5_c_im,
              s5_d, s5_w_glu, s5_b_glu, s5_out_norm, sgu_ln_g, sgu_ln_b, sgu_w, sgu_b, gmlp_out_norm,
              gdn_conv, gdn_a_log, gdn_dt_bias, gdn_norm, sc_conv, sc_out_norm, w_out, ffn_norm,
              ffn_w_gate, ffn_w_up, ffn_w_down, moe_router, moe_w_gate, moe_w_up, moe_w_down, final_norm):
    bsz, seqlen, d = x.shape
    for l in range(DEPTH):
        h = rmsnorm(x, mix_norm[l])
        x = x + hybrid_mixer(h, w_in[l], s5_lam_re[l], s5_lam_im[l], s5_log_step[l], s5_b_re[l], s5_b_im[l],
                             s5_c_re[l], s5_c_im[l], s5_d[l], s5_w_glu[l], s5_b_glu[l], s5_out_norm[l],
                             sgu_ln_g[l], sgu_ln_b[l], sgu_w[l], sgu_b[l], gmlp_out_norm[l],
                             gdn_conv[l], gdn_a_log[l], gdn_dt_bias[l], gdn_norm[l],
                             sc_conv[l], sc_out_norm[l], w_out[l])
        h = rmsnorm(x, ffn_norm[l])
        i = l // 2
        if l % 2 == 0:
            x = x + swiglu(h, ffn_w_gate[i], ffn_w_up[i], ffn_w_down[i])
        else:
            x = x + moe_swiglu(h.reshape(bsz * seqlen, d), moe_router[i], moe_w_gate[i],
                               moe_w_up[i], moe_w_down[i]).reshape(bsz, seqlen, d)
    return rmsnorm(x, final_norm)
```

```python
import numpy as np
import ml_dtypes
import concourse.bass as bass
import concourse.mybir as mybir
from concourse.bass_utils import run_bass_kernel_spmd
from contextlib import ExitStack, contextmanager

F32 = mybir.dt.float32
BF16 = mybir.dt.bfloat16
AF = mybir.ActivationFunctionType
ALU = mybir.AluOpType
AX = mybir.AxisListType

D = 1024
KD = 8
EPS = 1e-6
N_EXP = 8
D_FF = 2816
D_FFE = 3584
IN_COLS = 2568


class Buf:
    __slots__ = ("lw", "rd", "x")

    def __init__(self, x=False):
        self.lw = None
        self.rd = []
        self.x = x


def bufs(n):
    return [Buf() for _ in range(n)]


class Prog:
    NDMA = 16

    def __init__(self, nc, es):
        self.nc = nc
        self.es = es
        self.stack = [es]
        self.cnt = {}
        self.sem = {}
        self.known = {}
        self.eng = {"pe": nc.tensor, "act": nc.scalar, "dve": nc.vector, "pool": nc.gpsimd, "sp": nc.sync}
        for e in self.eng:
            self.cnt[e] = 0
            self.sem[e] = es.enter_context(nc.semaphore("s_" + e))
            self.known[e] = {}
        self.dsem, self.dcnt, self.drot = {}, {}, {}
        for e in ("sp", "pool", "act"):
            self.dsem[e] = [es.enter_context(nc.semaphore("d_%s%d" % (e, i))) for i in range(self.NDMA)]
            self.dcnt[e] = [0] * self.NDMA
            self.drot[e] = 0
        self.nid = 0
        self.ninst = 0
        self.consts_ = {}

    @contextmanager
    def scope(self):
        with ExitStack() as es:
            self.stack.append(es)
            try:
                yield
            finally:
                self.barrier()
                self.stack.pop()

    def barrier(self):
        for e in self.eng:
            waits = []
            kn = self.known[e]
            for f in self.eng:
                if self.cnt[f] and kn.get(f, 0) < self.cnt[f]:
                    kn[f] = self.cnt[f]
                    waits.append((f, self.cnt[f]))
            for q in self.dsem:
                for i in range(self.NDMA):
                    key, v = (q, i), self.dcnt[q][i]
                    if v and kn.get(key, 0) < v:
                        kn[key] = v
                        waits.append((key, v))
            self._emit(e, waits, None, None)

    def sb(self, shape, dt=F32):
        self.nid += 1
        return self.stack[-1].enter_context(self.nc.sbuf_tensor("sb%d" % self.nid, list(shape), dt))

    def ps(self, shape, dt=F32):
        self.nid += 1
        return self.stack[-1].enter_context(self.nc.psum_tensor("ps%d" % self.nid, list(shape), dt))

    def _semobj(self, key):
        if isinstance(key, str):
            return self.sem[key]
        return self.dsem[key[0]][key[1]]

    def _deps(self, eng, reads, writes):
        need = {}
        for b in reads:
            if b.lw is not None:
                k, v = b.lw
                if need.get(k, 0) < v:
                    need[k] = v
        for b in writes:
            if b.lw is not None and b.lw[0] != eng:
                k, v = b.lw
                if need.get(k, 0) < v:
                    need[k] = v
            for k, v in b.rd:
                if k != eng and need.get(k, 0) < v:
                    need[k] = v
        kn = self.known[eng]
        out = []
        for k, v in need.items():
            if kn.get(k, 0) < v:
                kn[k] = v
                out.append((k, v))
        return out

    def _emit(self, engname, waits, fn, inc):
        e = self.eng[engname]
        for k, v in waits:
            e.wait_ge(self._semobj(k), v)
        if fn is None:
            return
        ins = fn(e)
        ins.then_inc(self._semobj(inc[0]), inc[1])
        self.ninst += 1

    def op(self, eng, fn, reads=(), writes=()):
        if any(b.x for b in reads):
            writes = list(writes) + [b for b in reads if b.x and b not in writes]
            reads = [b for b in reads if not b.x]
        waits = self._deps(eng, reads, writes)
        self.cnt[eng] += 1
        tag = (eng, self.cnt[eng])
        self._emit(eng, waits, fn, (eng, 1))
        for b in reads:
            b.rd.append(tag)
        for b in writes:
            b.lw = tag
            b.rd = []

    def dma(self, out, in_, reads=(), writes=(), q="sp", **kw):
        i = self.drot[q]
        self.drot[q] = (i + 1) % self.NDMA
        key = (q, i)
        waits = self._deps(q, reads, writes)
        prev = self.dcnt[q][i]
        if prev and self.known[q].get(key, 0) < prev:
            self.known[q][key] = prev
            waits.append((key, prev))
        self.dcnt[q][i] = prev + 16
        tag = (key, prev + 16)
        self._emit(q, waits, lambda e: e.dma_start(out=out, in_=in_, **kw), (key, 16))
        for b in reads:
            b.rd.append(tag)
        for b in writes:
            b.lw = tag
            b.rd = []

    def eps_ap(self, like, eps):
        return self.epst[0:like.shape[0], 0:1]

    def const_ap(self, val):
        if val not in self.consts_:
            t = self.es.enter_context(self.nc.sbuf_tensor("cst%d" % len(self.consts_), [128, 1], F32))
            b = Buf()
            self.op("dve", lambda e: e.memset(t[:], float(val)), [], [b])
            for e_ in ("act", "pool", "pe"):
                self.wait_all(e_, [b])
            self.consts_[val] = t
        return self.consts_[val][:, 0:1]

    def join(self, dst, srcs):
        self.op("sp", lambda e: e.nop(), reads=list(srcs), writes=list(dst))

    def wait_all(self, eng, bl):
        self._emit(eng, self._deps(eng, bl, ()), None, None)


def bcast(ap, shape):
    return ap.broadcast_to(list(shape))


def rsqrt_op(P, out, in_, scale, bin_, bout, eps=EPS):
    P.op("act", lambda e: e.activation(out=out, in_=in_, func=AF.Sqrt, bias=P.eps_ap(out, eps), scale=scale),
         reads=[bin_], writes=[bout])
    P.op("dve", lambda e: e.reciprocal(out=out, in_=out), reads=[bout], writes=[bout])


def ffn_stage(K, x_src, x_dst, normg, experts, router, final_g):
    P, nc, T = K.P, K.nc, x_src.T
    TB = min(T, 2048)
    NT = TB // 128
    NSB = TB // 512
    moe = router is not None
    with P.scope():
        xacc = P.sb([128, NT, D]); bx = bufs(NT)
        hT = P.sb([128, KD, TB], BF16); bh = bufs(NT)
        xs = [P.sb([128, D]) for _ in range(2)]; bxs = bufs(2)
        h32 = [P.sb([128, D]) for _ in range(2)]; bh32 = bufs(2)
        junk = P.sb([128, D], BF16); bjunk = Buf()
        ss = P.sb([128, NT]); bss = bufs(NT)
        rs = P.sb([128, NT]); brs = bufs(NT)
        gate = P.sb([128, NT, 8]); bgate = bufs(NT)
        sm = P.sb([128, 64]); bsm = Buf()
        wg = [P.sb([128, KD, 512], BF16) for _ in range(2)]; bwg = bufs(2)
        wu = [P.sb([128, KD, 512], BF16) for _ in range(2)]; bwu = bufs(2)
        wd = [P.sb([128, 4, D], BF16) for _ in range(2)]; bwd = bufs(2)
        act = [P.sb([128, 4, 512], BF16) for _ in range(2)]; bact = bufs(2)
        sg = [P.sb([128, 512]) for _ in range(2)]; bsg = bufs(2)
        pg = [P.ps([128, 512]) for _ in range(2)]; bpg = [Buf(True), Buf(True)]
        pu = [P.ps([128, 512]) for _ in range(2)]; bpu = [Buf(True), Buf(True)]
        pd = [P.ps([128, 512]) for _ in range(2)]; bpd = [Buf(True), Buf(True)]
        ptr = P.ps([128, 1024]); bptr = Buf(True)
        if moe:
            wr = P.sb([128, KD, 8]); bwr = Buf()
            P.dma(wr[:], router.rearrange("(k p) e -> p k e", p=128), writes=[bwr])
            lg = P.sb([128, 8]); blg = Buf()
            m8 = P.sb([128, 8]); bm8 = Buf()
        if final_g is not None:
            fg = P.sb([128, D]); bfg = Buf()
            P.dma(fg[:], final_g.partition_broadcast(128), writes=[bfg])
        gcnt = 0
        piece_idx = 0
        for tb in range(T // TB):
            r0 = tb * TB
            for n0 in range(0, NT, 4):
                P.dma(xacc[:, n0:n0 + 4, :],
                      x_src.ap[r0 + n0 * 128: r0 + (n0 + 4) * 128, :].rearrange("(n p) d -> p n d", p=128),
                      reads=[x_src.b[(r0 + n0 * 128) // 512]], writes=bx[n0:n0 + 4])
            def prep_tile(n):
                s2 = n % 2
                P.op("act", lambda e, n=n: e.activation(out=junk[:], in_=xacc[:, n, :], func=AF.Square,
                                                        accum_out=ss[:, n:n + 1]),
                     reads=[bx[n]], writes=[bjunk, bss[n]])
                rsqrt_op(P, rs[:, n:n + 1], ss[:, n:n + 1], 1.0 / D, bss[n], brs[n])
                P.op("act", lambda e, n=n, s2=s2: e.activation(out=xs[s2][:], in_=xacc[:, n, :], func=AF.Copy,
                                                               scale=rs[:, n:n + 1]),
                     reads=[bx[n], brs[n]], writes=[bxs[s2]])
                for k in range(KD):
                    P.op("pe", lambda e, k=k, s2=s2: e.transpose(out=ptr[:, k * 128:(k + 1) * 128],
                                                                 in_=xs[s2][:, k * 128:(k + 1) * 128],
                                                                 identity=K.ident32[:]),
                         reads=[bxs[s2], K.bconst], writes=[bptr])
                P.op("dve", lambda e, s2=s2: e.scalar_tensor_tensor(
                    out=h32[s2][:].rearrange("p (k t) -> p k t", k=KD),
                    in0=ptr[:].rearrange("p (k t) -> p k t", k=KD), scalar=1.0,
                    in1=bcast(normg.unsqueeze(2), [128, KD, 128]), op0=ALU.mult, op1=ALU.mult),
                    reads=[bptr, K.bconst], writes=[bh32[s2]])
                P.op("pool", lambda e, n=n, s2=s2: e.tensor_copy(
                    out=hT[:, :, n * 128:(n + 1) * 128], in_=h32[s2][:].rearrange("p (k t) -> p k t", k=KD)),
                    reads=[bh32[s2]], writes=[bh[n]])
                if moe:
                    for k in range(KD):
                        P.op("pe", lambda e, k=k, s2=s2: e.matmul(pd[0][:, 0:8], lhsT=h32[s2][:, k * 128:(k + 1) * 128],
                                                                  rhs=wr[:, k, :], start=(k == 0), stop=(k == KD - 1)),
                             reads=[bh32[s2], bwr], writes=[bpd[0]])
                    P.op("dve", lambda e: e.tensor_copy(out=lg[:], in_=pd[0][:, 0:8]), reads=[bpd[0]], writes=[blg])
                    P.op("dve", lambda e: e.max(out=m8[:], in_=lg[:]), reads=[blg], writes=[bm8])
                    P.op("dve", lambda e: e.tensor_tensor(out=sm[:, 2:3], in0=m8[:, 0:1], in1=m8[:, 1:2], op=ALU.subtract),
                         reads=[bm8], writes=[bsm])
                    P.op("act", lambda e: e.activation(out=sm[:, 0:1], in_=sm[:, 2:3], func=AF.Sigmoid),
                         reads=[bsm], writes=[bsm])
                    P.op("act", lambda e: e.activation(out=sm[:, 1:2], in_=sm[:, 2:3], func=AF.Sigmoid, scale=-1.0),
                         reads=[bsm], writes=[bsm])
                    P.op("dve", lambda e: e.tensor_scalar(out=sm[:, 8:16], in0=lg[:], scalar1=m8[:, 0:1], scalar2=sm[:, 0:1],
                                                          op0=ALU.is_equal, op1=ALU.mult),
                         reads=[blg, bm8, bsm], writes=[bsm])
                    P.op("dve", lambda e: e.tensor_scalar(out=sm[:, 16:24], in0=lg[:], scalar1=m8[:, 1:2], scalar2=sm[:, 1:2],
                                                          op0=ALU.is_equal, op1=ALU.mult),
                         reads=[blg, bm8, bsm], writes=[bsm])
                    P.op("dve", lambda e, n=n: e.tensor_tensor(out=gate[:, n, :], in0=sm[:, 8:16], in1=sm[:, 16:24], op=ALU.add),
                         reads=[bsm], writes=[bgate[n]])
            first_piece = True
            pend = []
            for ei, ex in enumerate(experts):
                nch = ex["ff"] // 128
                for c0 in range(0, nch, 4):
                    ncp = min(4, nch - c0)
                    sl = piece_idx % 2
                    piece_idx += 1
                    P.dma(wg[sl][:, :, 0:ncp * 128], ex["wg"][:, c0 * 128:(c0 + ncp) * 128].rearrange("(k p) f -> p k f", p=128),
                          writes=[bwg[sl]], q="pool")
                    P.dma(wu[sl][:, :, 0:ncp * 128], ex["wu"][:, c0 * 128:(c0 + ncp) * 128].rearrange("(k p) f -> p k f", p=128),
                          writes=[bwu[sl]], q="pool")
                    P.dma(wd[sl][:, 0:ncp, :], ex["wd"][c0 * 128:(c0 + ncp) * 128, :].rearrange("(c p) d -> p c d", p=128),
                          writes=[bwd[sl]], q="pool")
                    for sbk in range(NSB):
                        if first_piece:
                            if sbk == 0:
                                for n_ in range(0, 4):
                                    prep_tile(n_)
                            if sbk + 1 < NSB:
                                for n_ in range((sbk + 1) * 4, (sbk + 2) * 4):
                                    prep_tile(n_)
                        asl = gcnt % 2
                        gcnt += 1
                        hb = bh[sbk * 4:(sbk + 1) * 4]
                        for c in range(ncp):
                            pp = c % 2
                            for k in range(KD):
                                P.op("pe", lambda e, k=k, c=c, pp=pp, sl=sl, sbk=sbk: e.matmul(
                                    pg[pp][:], lhsT=wg[sl][:, k, c * 128:(c + 1) * 128], rhs=hT[:, k, sbk * 512:(sbk + 1) * 512],
                                    start=(k == 0), stop=(k == KD - 1)), reads=[bwg[sl]] + hb, writes=[bpg[pp]])
                            for k in range(KD):
                                P.op("pe", lambda e, k=k, c=c, pp=pp, sl=sl, sbk=sbk: e.matmul(
                                    pu[pp][:], lhsT=wu[sl][:, k, c * 128:(c + 1) * 128], rhs=hT[:, k, sbk * 512:(sbk + 1) * 512],
                                    start=(k == 0), stop=(k == KD - 1)), reads=[bwu[sl]] + hb, writes=[bpu[pp]])
                            P.op("act", lambda e, pp=pp: e.activation(out=sg[pp][:], in_=pg[pp][:], func=AF.Silu),
                                 reads=[bpg[pp]], writes=[bsg[pp]])
                            P.op("dve", lambda e, pp=pp, asl=asl, c=c: e.tensor_tensor(out=act[asl][:, c, :], in0=sg[pp][:],
                                                                                      in1=pu[pp][:], op=ALU.mult),
                                 reads=[bsg[pp], bpu[pp]], writes=[bact[asl]])
                        for f in pend:
                            f()
                        pend = []

                        def down(asl=asl, sl=sl, sbk=sbk, ncp=ncp, ei=ei):
                            for n4 in range(4):
                                n = sbk * 4 + n4
                                for hf in range(2):
                                    dp = (n4 * 2 + hf) % 2
                                    for c in range(ncp):
                                        P.op("pe", lambda e, c=c, dp=dp, n4=n4, hf=hf: e.matmul(
                                            pd[dp][:], lhsT=act[asl][:, c, n4 * 128:(n4 + 1) * 128],
                                            rhs=wd[sl][:, c, hf * 512:(hf + 1) * 512], start=(c == 0), stop=(c == ncp - 1)),
                                            reads=[bact[asl], bwd[sl]], writes=[bpd[dp]])
                                    if moe:
                                        P.op("dve", lambda e, dp=dp, n=n, hf=hf: e.scalar_tensor_tensor(
                                            out=xacc[:, n, hf * 512:(hf + 1) * 512], in0=pd[dp][:], scalar=gate[:, n, ei:ei + 1],
                                            in1=xacc[:, n, hf * 512:(hf + 1) * 512], op0=ALU.mult, op1=ALU.add),
                                            reads=[bpd[dp], bgate[n], bx[n]], writes=[bx[n]])
                                    else:
                                        P.op("dve", lambda e, dp=dp, n=n, hf=hf: e.tensor_tensor(
                                            out=xacc[:, n, hf * 512:(hf + 1) * 512], in0=pd[dp][:],
                                            in1=xacc[:, n, hf * 512:(hf + 1) * 512], op=ALU.add),
                                            reads=[bpd[dp], bx[n]], writes=[bx[n]])
                        pend.append(down)
                    first_piece = False
            for f in pend:
                f()
            pend = []
            for n in range(NT):
                if final_g is not None:
                    s2 = n % 2
                    P.op("act", lambda e, n=n: e.activation(out=junk[:], in_=xacc[:, n, :], func=AF.Square,
                                                            accum_out=ss[:, n:n + 1]),
                         reads=[bx[n]], writes=[bjunk, bss[n]])
                    rsqrt_op(P, rs[:, n:n + 1], ss[:, n:n + 1], 1.0 / D, bss[n], brs[n])
                    P.op("dve", lambda e, n=n: e.scalar_tensor_tensor(out=xacc[:, n, :], in0=xacc[:, n, :], scalar=rs[:, n:n + 1],
                                                                      in1=fg[:], op0=ALU.mult, op1=ALU.mult),
                         reads=[bx[n], brs[n], bfg], writes=[bx[n]])
            for n0 in range(0, NT, 4):
                P.dma(x_dst.ap[r0 + n0 * 128: r0 + (n0 + 4) * 128, :].rearrange("(n p) d -> p n d", p=128),
                      xacc[:, n0:n0 + 4, :], reads=bx[n0:n0 + 4], writes=[x_dst.b[(r0 + n0 * 128) // 512]])


class KCtx:
    pass


class DBuf:
    def __init__(self, ap, T):
        self.ap = ap
        self.T = T
        self.b = bufs(max(1, T // 512))


def build_program(T, do_mixer=(True, True), do_ffn=(True, True), do_final=True, en=(1, 1, 1, 1), split=False):
    nc = bass.Bass("TRN2", target_bir_lowering=False)
    K = KCtx()
    K.nc, K.T = nc, T
    dr = {}

    def din(name, shape, dt=F32):
        dr[name] = nc.dram_tensor(name, list(shape), dt, kind="ExternalInput").ap()
        return dr[name]
    x = DBuf(din("x", [T, D]), T)
    TO = T // 2 if split else T
    out = DBuf(nc.dram_tensor("out", [TO, D], F32, kind="ExternalOutput").ap(), TO)
    xh = DBuf(nc.dram_tensor("xh", [TO, D], F32, kind="Internal").ap(), TO)
    din("wsel", [128, 2])
    xs_ = [DBuf(nc.dram_tensor("xs%d" % i, [T, D], F32, kind="Internal").ap(), T) for i in range(3)]
    yA = DBuf(nc.dram_tensor("yA", [128, 6, T], BF16, kind="Internal").ap(), T)
    hTd = DBuf(nc.dram_tensor("hTd", [128, KD, T], BF16, kind="Internal").ap(), T)
    din("consts", [128, NCONST])
    din("normg", [128, 4 * KD])
    din("final_norm", [1, D])
    din("w_in", [2, D, IN_COLS]); din("w_out", [2, D, D])
    din("lpa", [2, 128, NLPA]); din("lps", [2, 128, NLPS])
    din("ffn_w_gate", [D, D_FF]); din("ffn_w_up", [D, D_FF]); din("ffn_w_down", [D_FF, D])
    din("moe_router", [D, N_EXP])
    din("moe_w_gate", [N_EXP, D, D_FFE]); din("moe_w_up", [N_EXP, D, D_FFE]); din("moe_w_down", [N_EXP, D_FFE, D])
    with ExitStack() as es:
        P = Prog(nc, es)
        K.P = P
        K.consts = P.sb([128, NCONST]); K.bconst = Buf()
        lo, hi = CONST_COLS["ident"]
        K.ident32 = K.consts[:, lo:hi]
        normg = P.sb([128, 4 * KD])
        wsel = P.sb([128, 2])
        P.epst = P.sb([128, 1])
        beps = Buf()
        P.op("dve", lambda e: e.memset(P.epst[:], EPS), writes=[beps])
        K.ones_bf = P.sb([128, 128], BF16)
        P.op("dve", lambda e: e.memset(K.ones_bf[:], 1.0), writes=[beps])
        K.ident_bf = P.sb([128, 128], BF16)
        P.const_ap(1.0); P.const_ap(64 * EPS)
        P.dma(K.consts[:], dr["consts"], writes=[K.bconst])
        bng = Buf()
        P.dma(normg[:], dr["normg"], writes=[bng])
        P.dma(wsel[:], dr["wsel"], writes=[K.bconst])
        P.op("dve", lambda e: e.tensor_copy(out=K.ident_bf[:], in_=K.ident32), reads=[K.bconst], writes=[beps])
        for e_ in ("dve", "pe", "act", "pool"):
            P.wait_all(e_, [bng, K.bconst, beps])
        cur = x
        free = list(xs_)
        st = NS()
        st.s5_init = st.sc_init = st.gdn_s_init = st.gdn_raw_init = None
        st.s5_out = st.sc_out = st.gdn_s_out = st.gdn_raw_out = None
        for l in range(2):
            if do_mixer[l]:
                with P.scope():
                    lpa = P.sb([128, NLPA]); K.blpa = Buf()
                    P.dma(lpa[:], dr["lpa"][l], writes=[K.blpa])
                    with P.scope():
                        S5 = NS()
                        S5.Wt = P.sb([128, 2, 8, 2, 128], BF16)
                        S5.QT = P.sb([128, 8, 8, 2, 64], BF16)
                        S5.BDT = P.sb([128, 2, 8, 128], BF16)
                        S5.Hr = P.sb([128, 7, 8]); S5.Hi = P.sb([128, 7, 8])
                        S5.b = Buf()
                        if en[0]:
                            with P.scope():
                                lps = P.sb([128, NLPS]); K.blps = Buf()
                                P.dma(lps[:], dr["lps"][l], writes=[K.blps])
                                s5_tables(K, lps, S5)
                        mixer_pass_a(K, l, cur, yA, hTd, normg[:, (2 * l) * KD:(2 * l + 1) * KD], dr["w_in"][l], lpa, S5, st, en=en[:3])
                    sp_ = split and l == 1
                    dst = xh if sp_ else free.pop(0)
                    mixer_pass_b(K, l, cur, dst, yA, hTd, normg[:, (2 * l) * KD:(2 * l + 1) * KD], dr["w_in"][l], dr["w_out"][l], lpa, st,
                                 en_gdn=bool(en[3]), wsel=wsel if sp_ else None)
                    if cur is not x and cur is not xh:
                        free.append(cur)
                    cur = dst
            if do_ffn[l]:
                last = (l == 1) or not (do_ffn[1] or do_mixer[1])
                dst = out if last else free.pop(0)
                if l == 0:
                    experts = [dict(wg=dr["ffn_w_gate"], wu=dr["ffn_w_up"], wd=dr["ffn_w_down"], ff=D_FF)]
                    router = None
                else:
                    experts = [dict(wg=dr["moe_w_gate"][e], wu=dr["moe_w_up"][e], wd=dr["moe_w_down"][e], ff=D_FFE)
                               for e in range(N_EXP)]
                    router = dr["moe_router"]
                ffn_stage(K, cur, dst, normg[:, (2 * l + 1) * KD:(2 * l + 2) * KD], experts, router,
                          dr["final_norm"] if (last and do_final) else None)
                if cur is not x:
                    free.append(cur)
                cur = dst
        K.final = cur
        if cur is not out:
            with P.scope():
                t = P.sb([128, 4, D]); bt = Buf()
                for blk in range(TO // 512):
                    P.dma(t[:], cur.ap[blk * 512:(blk + 1) * 512, :].rearrange("(n p) d -> p n d", p=128), reads=[cur.b[blk]], writes=[bt])
                    P.dma(out.ap[blk * 512:(blk + 1) * 512, :].rearrange("(n p) d -> p n d", p=128), t[:], reads=[bt], writes=[out.b[blk]])
        P.wait_all("sp", out.b)
    K.ninst = P.ninst
    return nc, K


def mm(P, out, lhsT, rhs, start, stop, rd, wr):
    P.op("pe", lambda e: e.matmul(out, lhsT=lhsT, rhs=rhs, start=start, stop=stop), rd, wr)


def tt(P, eng, out, in0, in1, op, rd, wr):
    P.op(eng, lambda e: e.tensor_tensor(out=out, in0=in0, in1=in1, op=op), rd, wr)


def ts(P, eng, out, in0, s1, s2, op0, op1, rd, wr):
    if s2 is None:
        P.op(eng, lambda e: e.tensor_scalar(out=out, in0=in0, scalar1=s1, scalar2=None, op0=op0), rd, wr)
    else:
        P.op(eng, lambda e: e.tensor_scalar(out=out, in0=in0, scalar1=s1, scalar2=s2, op0=op0, op1=op1), rd, wr)


def stt(P, eng, out, in0, scalar, in1, op0, op1, rd, wr):
    P.op(eng, lambda e: e.scalar_tensor_tensor(out=out, in0=in0, scalar=scalar, in1=in1, op0=op0, op1=op1), rd, wr)


def actf(P, out, in_, func, rd, wr, **kw):
    P.op("act", lambda e: e.activation(out=out, in_=in_, func=func, **kw), rd, wr)


def cp(P, eng, out, in_, rd, wr):
    if eng == "act":
        P.op(eng, lambda e: e.activation(out=out, in_=in_, func=AF.Copy), rd, wr)
    else:
        P.op(eng, lambda e: e.tensor_copy(out=out, in_=in_), rd, wr)


MUL, ADD, SUB = ALU.mult, ALU.add, ALU.subtract
MAGIC = 12582912.0
TWO_PI = 2.0 * np.pi

CONST_COLS = {}


def _const_layout():
    o = 0
    for name, n in [("ident", 128), ("triu", 128), ("U1", 128), ("U2", 128), ("maskS", 128), ("maskI", 128),
                    ("onesblk", 128), ("chunk0", 128), ("chunk1", 128), ("headsel", 2), ("mq", 4), ("mask8", 8), ("par", 2), ("pairm", 4)]:
        CONST_COLS[name] = (o, o + n)
        o += n
    return o


NCONST = _const_layout()


def make_consts():
    p = np.arange(128)
    ch = p // 64
    same = (ch[:, None] == ch[None, :])
    c = np.zeros((128, NCONST), np.float32)

    def put(name, a):
        lo, hi = CONST_COLS[name]
        c[:, lo:hi] = a
    put("ident", np.eye(128))
    put("triu", (p[None, :] >= p[:, None]))
    put("U1", same & (p[:, None] <= p[None, :]))
    put("U2", same & (p[:, None] > p[None, :]))
    put("maskS", same & (p[:, None] < p[None, :]))
    put("maskI", same & (p[:, None] <= p[None, :]))
    put("onesblk", same)
    put("chunk0", np.repeat((p < 64)[:, None], 128, 1))
    put("chunk1", np.repeat((p >= 64)[:, None], 128, 1))
    put("headsel", np.stack([p < 64, p >= 64], 1))
    put("mq", np.stack([(p // 16) % 4 == q for q in range(4)], 1))
    put("mask8", np.stack([(p // 16) == g for g in range(8)], 1))
    put("par", np.stack([(p // 16) % 2 == q for q in range(2)], 1))
    put("pairm", np.stack([(p // 32) == q for q in range(4)], 1))
    return c


LPA = {}
LPS = {}


def _lp_layout():
    o = 0
    for name, n in [("s5_d", 2), ("s5_bglu", 2), ("s5_on", 2), ("sc_on", 2), ("sc_conv", 6), ("gdn_conv", 24),
                    ("wglu", 512), ("ln_g", 256), ("ln_b", 256), ("gmn", 256), ("gdnn", 256), ("a_log", 4), ("dtb", 4),
                    ("bsp", 4), ("wsp", 512)]:
        LPA[name] = (o, o + n)
        o += n
    na = o
    o = 0
    for name, n in [("LRp", 8), ("LIp", 8), ("STp", 8), ("CRp", 128), ("CIp", 128), ("LRu", 128), ("LIu", 128), ("STu", 2),
                    ("BRu", 128), ("BIu", 128), ("CRu", 2048), ("CIu", 2048)]:
        LPS[name] = (o, o + n)
        o += n
    return na, o


NLPA, NLPS = _lp_layout()


def pack_layer_params(inp, l):
    a = np.zeros((128, NLPA), np.float32)
    s = np.zeros((128, NLPS), np.float32)

    def pa(name, v):
        lo, hi = LPA[name]
        a[:, lo:hi] = np.asarray(v, np.float32).reshape(128, hi - lo)

    def ps_(name, v):
        lo, hi = LPS[name]
        s[:, lo:hi] = np.asarray(v, np.float32).reshape(128, hi - lo)

    def fm(v, nt):
        return np.asarray(v).reshape(nt, 128).T
    pa("s5_d", fm(inp["s5_d"][l], 2)); pa("s5_bglu", fm(inp["s5_b_glu"][l], 2)); pa("s5_on", fm(inp["s5_out_norm"][l], 2))
    pa("sc_on", fm(inp["sc_out_norm"][l], 2))
    pa("sc_conv", inp["sc_conv"][l].reshape(3, 2, 128).transpose(2, 1, 0))
    pa("gdn_conv", inp["gdn_conv"][l].reshape(4, 6, 128).transpose(2, 1, 0))
    pa("wglu", inp["s5_w_glu"][l].reshape(2, 128, 256).transpose(1, 0, 2))
    rep = lambda v: np.broadcast_to(np.asarray(v)[None, :], (128, len(v)))
    pa("ln_g", rep(inp["sgu_ln_g"][l])); pa("ln_b", rep(inp["sgu_ln_b"][l])); pa("gmn", rep(inp["gmlp_out_norm"][l]))
    pa("gdnn", rep(np.tile(inp["gdn_norm"][l], 4))); pa("a_log", rep(inp["gdn_a_log"][l])); pa("dtb", rep(inp["gdn_dt_bias"][l]))
    pa("bsp", inp["sgu_b"][l].T)
    pa("wsp", inp["sgu_w"][l].transpose(2, 0, 1))
    lam_re, lam_im, st = inp["s5_lam_re"][l], inp["s5_lam_im"][l], inp["s5_log_step"][l]
    pm = lambda v: v.reshape(8, 2, 64).transpose(1, 2, 0).reshape(128, 8)
    ps_("LRp", pm(lam_re)); ps_("LIp", pm(lam_im)); ps_("STp", pm(np.repeat(st[:, None], 64, 1)))
    cpm = lambda c: c.reshape(8, 2, 16, 64).transpose(1, 3, 0, 2).reshape(128, 128)
    ps_("CRp", cpm(inp["s5_c_re"][l])); ps_("CIp", cpm(inp["s5_c_im"][l]))
    um = lambda v: np.repeat(v.reshape(2, 8, 1, 64), 16, 2).transpose(1, 2, 0, 3).reshape(128, 128)
    ps_("LRu", um(lam_re)); ps_("LIu", um(lam_im))
    ps_("STu", np.repeat(st.reshape(2, 8, 1), 16, 2).transpose(1, 2, 0).reshape(128, 2))
    bum = lambda b: b.reshape(2, 8, 64, 16).transpose(1, 3, 0, 2).reshape(128, 128)
    ps_("BRu", bum(inp["s5_b_re"][l])); ps_("BIu", bum(inp["s5_b_im"][l]))
    cum = lambda c: np.repeat(c.reshape(2, 8, 1, 16, 64), 16, 2).transpose(1, 2, 0, 3, 4).reshape(128, 2048)
    ps_("CRu", cum(inp["s5_c_re"][l])); ps_("CIu", cum(inp["s5_c_im"][l]))
    return a, s


def cview(K, name):
    lo, hi = CONST_COLS[name]
    return K.consts[:, lo:hi]


def s5_tables(K, lps, S5):
    P = K.P
    B = S5.b
    R = [B]

    def L(name, shape=None):
        lo, hi = LPS[name]
        v = lps[:, lo:hi]
        return v

    def cexp(dst_r, dst_i, lrdt, lidt, t0, t1, shape_n):
        actf(P, t0, lrdt, AF.Exp, R, R)
        ts(P, "dve", t1, lidt, 1.0 / TWO_PI, MAGIC, MUL, ADD, R, R)
        ts(P, "dve", t1, t1, MAGIC, -TWO_PI, SUB, MUL, R, R)
        tt(P, "dve", t1, t1, lidt, ADD, R, R)
        actf(P, dst_i, t1, AF.Sin, R, R)
        ts(P, "dve", dst_r, lidt, np.pi / 2, None, ADD, None, R, R)
        ts(P, "dve", t1, dst_r, 1.0 / TWO_PI, MAGIC, MUL, ADD, R, R)
        ts(P, "dve", t1, t1, MAGIC, -TWO_PI, SUB, MUL, R, R)
        tt(P, "dve", t1, t1, dst_r, ADD, R, R)
        actf(P, dst_r, t1, AF.Sin, R, R)
        tt(P, "dve", dst_r, dst_r, t0, MUL, R, R)
        tt(P, "dve", dst_i, dst_i, t0, MUL, R, R)

    def cmul(or_, oi, ar, ai, br, bi, t0):
        tt(P, "dve", t0, ai, bi, MUL, R, R)
        tt(P, "dve", or_, ar, br, MUL, R, R)
        tt(P, "dve", or_, or_, t0, SUB, R, R)
        tt(P, "dve", t0, ai, br, MUL, R, R)
        tt(P, "dve", oi, ar, bi, MUL, R, R)
        tt(P, "dve", oi, oi, t0, ADD, R, R)

    with P.scope():
        B.lw = K.blps.lw
        T = [P.sb([128, 128]) for _ in range(10)]
        dtu = P.sb([128, 2])
        Xr = P.sb([128, 8, 128]); Xi = P.sb([128, 8, 128])
        big0 = P.sb([128, 1024]); big1 = P.sb([128, 1024]); val = P.sb([128, 16])
        v3 = lambda t: t[:].rearrange("p (c n) -> p c n", c=2)
        actf(P, dtu[:], L("STu"), AF.Exp, R, R)
        dtb = bcast(dtu[:].unsqueeze(2), [128, 2, 64])
        lrdt, lidt, ar, ai, t0, t1 = T[0], T[1], T[2], T[3], T[4], T[5]
        tt(P, "dve", v3(lrdt), L("LRu").rearrange("p (c n) -> p c n", c=2), dtb, MUL, R, R)
        tt(P, "dve", v3(lidt), L("LIu").rearrange("p (c n) -> p c n", c=2), dtb, MUL, R, R)
        cexp(ar[:], ai[:], lrdt[:], lidt[:], t0[:], t1[:], 128)
        den, am1, fr, fi = T[6], T[7], T[8], T[9]
        tt(P, "dve", den[:], L("LRu"), L("LRu"), MUL, R, R)
        tt(P, "dve", t0[:], L("LIu"), L("LIu"), MUL, R, R)
        tt(P, "dve", den[:], den[:], t0[:], ADD, R, R)
        P.op("dve", lambda e: e.reciprocal(out=den[:], in_=den[:]), R, R)
        ts(P, "dve", am1[:], ar[:], -1.0, None, ADD, None, R, R)
        tt(P, "dve", fr[:], am1[:], L("LRu"), MUL, R, R)
        tt(P, "dve", t0[:], ai[:], L("LIu"), MUL, R, R)
        tt(P, "dve", fr[:], fr[:], t0[:], ADD, R, R)
        tt(P, "dve", fr[:], fr[:], den[:], MUL, R, R)
        tt(P, "dve", fi[:], ai[:], L("LRu"), MUL, R, R)
        tt(P, "dve", t0[:], am1[:], L("LIu"), MUL, R, R)
        tt(P, "dve", fi[:], fi[:], t0[:], SUB, R, R)
        tt(P, "dve", fi[:], fi[:], den[:], MUL, R, R)
        cmul(Xr[:, 0, :], Xi[:, 0, :], fr[:], fi[:], L("BRu"), L("BIu"), t0[:])
        for k in range(7):
            cmul(Xr[:, k + 1, :], Xi[:, k + 1, :], Xr[:, k, :], Xi[:, k, :], ar[:], ai[:], t0[:])
        par = cview(K, "par")
        for jp in range(8):
            for part, X in enumerate((Xr, Xi)):
                src = X[:, 7 - jp, :].rearrange("p (c n) -> p c n", c=2)
                for half in range(2):
                    ts(P, "dve", S5.Wt[:, :, jp, part, half * 64:(half + 1) * 64], src, par[:, half:half + 1], None, MUL, None,
                       R + [K.bconst], [S5.b])
        m8 = cview(K, "mask8")
        for k in range(8):
            for ct in range(2):
                xr = bcast(Xr[:, k, ct * 64:(ct + 1) * 64].unsqueeze(1), [128, 16, 64])
                xi = bcast(Xi[:, k, ct * 64:(ct + 1) * 64].unsqueeze(1), [128, 16, 64])
                lo = LPS["CRu"][0] + ct * 1024
                cr = lps[:, lo:lo + 1024].rearrange("p (q n) -> p q n", q=16)
                lo = LPS["CIu"][0] + ct * 1024
                ci = lps[:, lo:lo + 1024].rearrange("p (q n) -> p q n", q=16)
                b0 = big0[:].rearrange("p (q n) -> p q n", q=16)
                b1 = big1[:].rearrange("p (q n) -> p q n", q=16)
                tt(P, "dve", b0, cr, xr, MUL, R, R)
                tt(P, "dve", b1, ci, xi, MUL, R, R)
                tt(P, "dve", b0, b0, b1, SUB, R, R)
                P.op("dve", lambda e, b0=b0: e.tensor_reduce(out=val[:], in_=b0, axis=AX.X, op=ADD), R, R)
                tt(P, "dve", S5.BDT[:, ct, k, :].rearrange("p (g q) -> p g q", g=8), bcast(val[:].unsqueeze(1), [128, 8, 16]),
                   bcast(m8.unsqueeze(2), [128, 8, 16]), MUL, R + [K.bconst], [S5.b])
        Q = [P.sb([128, 8]) for _ in range(6)]
        Pr = P.sb([128, 9, 8]); Pi = P.sb([128, 9, 8])
        dtp, lrp, lip, t0, t1 = Q[0], Q[1], Q[2], Q[3], Q[4]
        actf(P, dtp[:], L("STp"), AF.Exp, R, R)
        tt(P, "dve", lrp[:], L("LRp"), dtp[:], MUL, R, R)
        tt(P, "dve", lip[:], L("LIp"), dtp[:], MUL, R, R)
        cexp(Pr[:, 1, :], Pi[:, 1, :], lrp[:], lip[:], t0[:], t1[:], 8)
        for k in range(1, 8):
            cmul(Pr[:, k + 1, :], Pi[:, k + 1, :], Pr[:, k, :], Pi[:, k, :], Pr[:, 1, :], Pi[:, 1, :], t0[:])
        cp(P, "dve", S5.Hr[:, 0, :], Pr[:, 8, :], R, [S5.b])
        cp(P, "dve", S5.Hi[:, 0, :], Pi[:, 8, :], R, [S5.b])
        for s in range(6):
            cmul(S5.Hr[:, s + 1, :], S5.Hi[:, s + 1, :], S5.Hr[:, s, :], S5.Hi[:, s, :], S5.Hr[:, s, :], S5.Hi[:, s, :], t0[:])
        P.op("dve", lambda e: e.memset(S5.QT[:], 0.0), R, [S5.b])
        c0 = P.sb([128, 8, 16]); c1 = P.sb([128, 8, 16])
        CR = L("CRp").rearrange("p (r q) -> p r q", r=8)
        CI = L("CIp").rearrange("p (r q) -> p r q", r=8)
        for j in range(8):
            pr_b = bcast(Pr[:, j + 1, :].unsqueeze(2), [128, 8, 16])
            pi_b = bcast(Pi[:, j + 1, :].unsqueeze(2), [128, 8, 16])
            for part in range(2):
                if part == 0:
                    tt(P, "dve", c0[:], CR, pr_b, MUL, R, R)
                    tt(P, "dve", c1[:], CI, pi_b, MUL, R, R)
                    tt(P, "dve", c0[:], c0[:], c1[:], SUB, R, R)
                else:
                    tt(P, "dve", c0[:], CR, pi_b, MUL, R, R)
                    tt(P, "dve", c1[:], CI, pr_b, MUL, R, R)
                    stt(P, "dve", c0[:], c0[:], -1.0, c1[:], MUL, SUB, R, R)
                for gl in range(2):
                    for lp in range(2):
                        cp(P, "dve", S5.QT[gl * 64:(gl + 1) * 64, lp::2, j, part, 32 * lp + 16 * gl:32 * lp + 16 * gl + 16],
                           c0[gl * 64:(gl + 1) * 64, lp::2, :], R, [S5.b])


class NS:
    pass


def load_x_block_hT(K, x_src, blk, xr, bxr, xs2, bxs2, hT, bhT, ptrs, bptrs, normg, rs, brs, ss, bss, junk, bjunk):
    P = K.P
    r0 = blk * 512
    for n in range(4):
        i = n % 2
        xs, bxs = xs2[i], bxs2[i]
        ptr, bptr = ptrs[i], bptrs[i]
        P.dma(xr[i][:], x_src.ap[r0 + n * 128:r0 + (n + 1) * 128, :], reads=[x_src.b[blk]], writes=[bxr[i]])
        actf(P, junk[:], xr[i][:], AF.Square, [bxr[i]], [bjunk, bss[i]], accum_out=ss[:, n:n + 1])
        rsqrt_op(P, rs[:, n:n + 1], ss[:, n:n + 1], 1.0 / D, bss[i], brs[i])
        actf(P, xs[:], xr[i][:], AF.Copy, [bxr[i], brs[i]], [bxs], scale=rs[:, n:n + 1])
        for k in range(KD):
            P.op("pe", lambda e, k=k, xs=xs, ptr=ptr: e.transpose(out=ptr[:, k * 128:(k + 1) * 128], in_=xs[:, k * 128:(k + 1) * 128],
                                                                  identity=K.ident32[:]), [bxs, K.bconst], bptr)
        tt(P, "dve", hT[:, :, n * 128:(n + 1) * 128], ptr.rearrange("p (k t) -> p k t", k=KD),
           bcast(normg.unsqueeze(2), [128, KD, 128]), MUL, bptr + [K.bconst], [bhT])


def fm_norm(K, y, by, gain, out_fn, W, bout, perm=False):
    P = K.P
    actf(P, W.sq[:], y[:], AF.Square, [by], [W.bsq])
    for ct in range(2):
        mm(P, W.pn[:, 0:512], K.ones_bf[:], W.sq[:, ct, :], ct == 0, ct == 1, [W.bsq, K.bconst], [W.bpn])
    actf(P, W.rstd[:], W.pn[:, 0:512], AF.Sqrt, [W.bpn], [W.brstd], bias=P.epst[:, 0:1], scale=1.0 / 256)
    P.op("dve", lambda e: e.reciprocal(out=W.rstd[:], in_=W.rstd[:]), [W.brstd], [W.brstd])
    for ct in range(2):
        if perm:
            yi = y[:, ct, :].rearrange("p (j c) -> p j c", j=8)
            ri = W.rstd[:].rearrange("p (j c) -> p j c", j=8)
        else:
            yi, ri = y[:, ct, :], W.rstd[:]
        stt(P, "dve", out_fn(ct), yi, gain[:, ct:ct + 1], ri, MUL, MUL, [by, W.brstd], [bout])


def mixer_pass_a(K, l, x_src, yA, hTd, normg, w_in, lpa, S5, st, en=(1, 1, 1)):
    P, T = K.P, K.T
    NB = T // 512
    PAD = 64
    with P.scope():
        wA = P.sb([128, KD, 1536], BF16); bwA = Buf(); bwA2 = Buf()
        P.dma(wA[:, :, 0:768], w_in[:, 0:768].rearrange("(k p) c -> p k c", p=128), writes=[bwA], q="pool")
        P.dma(wA[:, :, 768:1536], w_in[:, 1800:2568].rearrange("(k p) c -> p k c", p=128), writes=[bwA2], q="pool")
        bW = [bwA, bwA2]
        xr = [P.sb([128, D]) for _ in range(2)]; bxr = bufs(2)
        xs2 = [P.sb([128, D]) for _ in range(2)]; bxs2 = bufs(2)
        hT = P.sb([128, KD, 512], BF16); bhT = Buf()
        junk = P.sb([128, D], BF16); bjunk = Buf()
        ss = P.sb([128, 4]); bss = bufs(2); rs = P.sb([128, 4]); brs = bufs(2)
        ptr = P.ps([128, 1024]); bptr = Buf(True); bptrB = Buf(True)
        pp = [P.ps([128, 512]) for _ in range(2)]; bpp = [Buf(True), Buf(True)]
        pg = [P.ps([128, 512]) for _ in range(2)]; bpg = [Buf(True), Buf(True)]
        pv = P.ps([128, 1024]); bpv = Buf(True)
        W = NS()
        W.sq = P.sb([128, 2, 512], BF16); W.bsq = Buf()
        W.rstd = P.sb([128, 512]); W.brstd = Buf()
        W.pn = pg[1]; W.bpn = bpg[1]
        W2 = NS()
        W2.sq = P.sb([128, 2, 512], BF16); W2.bsq = Buf()
        W2.rstd = P.sb([128, 512]); W2.brstd = Buf()
        W2.pn = pv[:, 512:1024]; W2.bpn = bpv
        ya_sc = P.sb([128, 2, 512]); bya_sc = Buf()
        junk_placeholder = None
        yst = P.sb([128, 6, 512], BF16); byst = bufs(3)
        ya = P.sb([128, 2, 512]); bya = Buf()
        yb = P.sb([128, 2, 512]); byb = Buf()
        P.op("dve", lambda e: e.memset(yst[:], 0.0), [], byst)
        uT = P.sb([128, 2, 512], BF16); buT = Buf()
        uTm = P.sb([128, 4, 2, 512], BF16); buTm = Buf()
        SA = P.sb([128, 8, 2, PAD + 65]); SB = P.sb([128, 8, 2, PAD + 65]); bSA = bufs(2); bSB = bufs(2)
        Sb16 = P.sb([128, 8, 2, 64], BF16); bS16 = Buf()
        hsT = [P.sb([128, 8, 65]) for _ in range(4)]; bhs = bufs(4); bdr, bdi = Buf(), Buf()
        yg = P.sb([128, 2, 512], BF16); byg = Buf()
        sig = P.sb([128, 512]); bsig = Buf()
        wglu = P.sb([128, 2, 256], BF16); bwglu = Buf()
        cp(P, "dve", wglu[:], lpa[:, LPA["wglu"][0]:LPA["wglu"][1]].rearrange("p (k c) -> p k c", k=2), [K.blpa], [bwglu])
        P.op("dve", lambda e: e.memset(SA[:], 0.0), [], bSA)
        P.op("dve", lambda e: e.memset(SB[:], 0.0), [], bSB)
        if st.s5_init is not None:
            P.dma(SA[:, :, :, PAD:PAD + 1], st.s5_init.rearrange("p (r c o) -> p r c o", r=8, c=2), reads=[], writes=bSA)
        Bsb = P.sb([128, 2, 512]); bB = Buf()
        Csb = P.sb([128, 2, 512]); bC = Buf()
        z = P.sb([128, 2, 2 + 512], BF16); bz = Buf()
        dsc = P.sb([128, 2, 3, 128], BF16); bdsc = Buf()
        lo = LPA["sc_conv"][0]
        for ct in range(2):
            for j in range(3):
                ts(P, "dve", dsc[:, ct, j, :], K.ident32[:], lpa[:, lo + ct * 3 + j:lo + ct * 3 + j + 1], None, MUL, None,
                   [K.blpa, K.bconst], [bdsc])
        P.op("dve", lambda e: e.memset(z[:], 0.0), [], [bz])
        if st.sc_init is not None:
            P.dma(z[:, :, 0:2], st.sc_init.rearrange("p (c o) -> p c o", c=2), reads=[], writes=[bz], q="pool")
        wsp = P.sb([128, 4, 128], BF16); bwsp = Buf()
        lo = LPA["wsp"][0]
        tt(P, "dve", wsp[:], lpa[:, lo:lo + 512].rearrange("p (h t) -> p h t", h=4),
           bcast(cview(K, "triu").unsqueeze(1), [128, 4, 128]), MUL, [K.blpa, K.bconst], [bwsp])
        def gm_set(pg_, bpg_):
            return (P.sb([128, 512]), Buf(), P.sb([128, 256]), Buf(), P.sb([128, 256], BF16), Buf(), P.sb([128, 256]), Buf(),
                    P.sb([128, 6]), P.sb([128, 2]), Buf(), P.sb([128, 2]), Buf(), P.sb([128, 256], BF16), Buf(), pg_, bpg_)
        gmA = gm_set(pg, bpg)
        gmB = gm_set(pp, bpp)

        def LA(name):
            lo, hi = LPA[name]
            return lpa[:, lo:hi]

        for blk in range(NB):
            load_x_block_hT(K, x_src, blk, xr, bxr, xs2, bxs2, hT, bhT, [ptr[:], pv[:]], [[bptr, bptrB], [bpv]], normg, rs, brs, ss, bss,
                            junk, bjunk)
            P.dma(hTd.ap[:, :, blk * 512:(blk + 1) * 512], hT[:], reads=[bhT], writes=[hTd.b[blk]])

            def proj_fm(col0, ps_ap, pbuf):
                wi, c = (0, col0) if col0 < 768 else (1, col0 - 1800 + 768)
                for k in range(KD):
                    mm(P, ps_ap, wA[:, k, c:c + 128], hT[:, k, :], k == 0, k == KD - 1, [bW[wi], bhT], [pbuf])
            def g_sc():
                if not en[2]:
                    return
                cp(P, "pool", z[:, :, 0:2], z[:, :, 512:514], [bz], [bz])
                for ct in range(2):
                    proj_fm(1800 + ct * 128, pp[0][:], bpp[0])
                    yield
                    cp(P, "act", Bsb[:, ct, :], pp[0][:], [bpp[0]], [bB])
                    proj_fm(2056 + ct * 128, pp[1][:], bpp[1])
                    yield
                    cp(P, "act", Csb[:, ct, :], pp[1][:], [bpp[1]], [bC])
                    proj_fm(2312 + ct * 128, pp[0][:], bpp[0])
                    yield
                    tt(P, "dve", z[:, ct, 2:514], Csb[:, ct, :], pp[0][:], MUL, [bC, bpp[0]], [bz])
                    yield
                for ct in range(2):
                    for j in range(3):
                        mm(P, pg[0][:], dsc[:, ct, j, :], z[:, ct, j:j + 512], j == 0, j == 2, [bdsc, bz], [bpg[0]])
                    yield
                    tt(P, "dve", ya_sc[:, ct, :], pg[0][:], Bsb[:, ct, :], MUL, [bpg[0], bB], [bya_sc])
                    yield
                fm_norm(K, ya_sc, bya_sc, LA("sc_on"), lambda ct: yst[:, 4 + ct, :], W, byst[2])
                yield
            def g_gm_tile(n, B_):
                gm, bgm, vn, bvn, vnb, bvnb, og, bog, stt6, mv, bmv, gs, bgs, junk2, bjunk2, pg, bpg = B_
                if True:
                    for k in range(KD):
                        mm(P, pg[0][:], hT[:, k, n * 128:(n + 1) * 128], wA[:, k, 256:768], k == 0, k == KD - 1, [bwA, bhT], [bpg[0]])
                    yield
                    actf(P, gm[:], pg[0][:], AF.Gelu_apprx_tanh, [bpg[0]], [bgm])
                    yield
                    P.op("dve", lambda e: e.bn_stats(out=stt6[:], in_=gm[:, 256:512]), [bgm], [bmv])
                    P.op("dve", lambda e: e.bn_aggr(out=mv[:], in_=stt6[:]), [bmv], [bmv])
                    rsqrt_op(P, mv[:, 1:2], mv[:, 1:2], 1.0, bmv, bmv)
                    ts(P, "dve", vn[:], gm[:, 256:512], mv[:, 0:1], mv[:, 1:2], SUB, MUL, [bgm, bmv], [bvn])
                    tt(P, "dve", vn[:], vn[:], LA("ln_g"), MUL, [bvn, K.blpa], [bvn])
                    tt(P, "dve", vnb[:], vn[:], LA("ln_b"), ADD, [bvn, K.blpa], [bvnb])
                    yield
                    for h in range(4):
                        mm(P, pg[1][:, h * 64:(h + 1) * 64], wsp[:, h, :], vnb[:, h * 64:(h + 1) * 64], True, True, [bwsp, bvnb], [bpg[1]])
                    lo = LPA["bsp"][0]
                    for h in range(4):
                        stt(P, "dve", og[:, h * 64:(h + 1) * 64], pg[1][:, h * 64:(h + 1) * 64], lpa[:, lo + h:lo + h + 1],
                            gm[:, h * 64:(h + 1) * 64], ADD, MUL, [bpg[1], bgm, K.blpa], [bog])
                    yield
                    actf(P, junk2[:], og[:], AF.Square, [bog], [bjunk2, bgs], accum_out=gs[:, 0:1])
                    yield
                    rsqrt_op(P, gs[:, 1:2], gs[:, 0:1], 1.0 / 256, bgs, bgs)
                    stt(P, "dve", og[:], og[:], gs[:, 1:2], LA("gmn"), MUL, MUL, [bog, bgs, K.blpa], [bog])
                    for ct in range(2):
                        P.op("pe", lambda e, ct=ct: e.transpose(out=pg[1][:, 256 + ct * 128:256 + (ct + 1) * 128],
                                                                in_=og[:, ct * 128:(ct + 1) * 128], identity=K.ident32[:]),
                             [bog, K.bconst], [bpg[1]])
                    yield
                    cp(P, "act", yst[:, 2:4, n * 128:(n + 1) * 128], pg[1][:, 256:512].rearrange("p (c t) -> p c t", c=2),
                       [bpg[1]], [byst[1]])
                    yield
            def g_gm():
                if not en[1]:
                    return
                for n0 in (0, 2):
                    live_ = [g_gm_tile(n0, gmA), g_gm_tile(n0 + 1, gmB)]
                    while live_:
                        for g_ in list(live_):
                            try:
                                next(g_)
                                yield
                            except StopIteration:
                                live_.remove(g_)

            DBG = 9

            def g_s5():
                if not en[0]:
                    return
                pvh = [pv[:, 0:512], pv[:, 512:1024]]
                for ct in range(2):
                    proj_fm(ct * 128, pvh[ct], bpv)
                    yield
                    cp(P, "act", uT[:, ct, :], pvh[ct], [bpv], [buT])
                    yield
                pm = cview(K, "pairm")
                for q4 in range(4):
                    actf(P, uTm[:, q4, :, :], uT[:], AF.Copy, [buT, K.bconst], [buTm], scale=pm[:, q4:q4 + 1])
                for pr in range(8):
                    ct, q4 = pr // 4, pr % 4
                    for part in range(2):
                        for jp in range(8):
                            mm(P, pv[:, (pr * 2 + part) * 64:(pr * 2 + part + 1) * 64],
                               S5.Wt[:, ct, jp, part, :], uTm[:, q4, ct, jp::8], jp == 0, jp == 7, [S5.b, buTm], [bpv])
                    if pr % 2 == 1:
                        yield
                cp(P, "dve", SA[:, :, :, PAD + 1:PAD + 65], pv[:].rearrange("p (r c n) -> p r c n", r=8, c=2), [bpv], bSA)
                src, dst, bs, bd = SA, SB, bSA, bSB
                for s in range(7 if DBG >= 2 else 0):
                    d = 1 << s
                    L = 65 - d
                    hr = bcast(S5.Hr[:, s, :].unsqueeze(2), [128, 8, L])
                    hi = bcast(S5.Hi[:, s, :].unsqueeze(2), [128, 8, L])
                    sr, si = src[:, :, 0, PAD + d:PAD + 65], src[:, :, 1, PAD + d:PAD + 65]
                    shr, shi = src[:, :, 0, PAD:PAD + L], src[:, :, 1, PAD:PAD + L]
                    dr_, di_ = dst[:, :, 0, PAD + d:PAD + 65], dst[:, :, 1, PAD + d:PAD + 65]
                    cp(P, "act", dst[:, :, :, PAD:PAD + d], src[:, :, :, PAD:PAD + d], bs, bd)
                    tt(P, "dve", hsT[0][:, :, 0:L], shr, hr, MUL, [bs[0], S5.b], [bhs[0]])
                    tt(P, "dve", hsT[1][:, :, 0:L], shi, hi, MUL, [bs[1], S5.b], [bhs[1]])
                    tt(P, "dve", hsT[2][:, :, 0:L], shi, hr, MUL, [bs[1], S5.b], [bhs[2]])
                    tt(P, "dve", hsT[3][:, :, 0:L], shr, hi, MUL, [bs[0], S5.b], [bhs[3]])
                    tt(P, "dve", dr_, sr, hsT[0][:, :, 0:L], ADD, [bs[0], bhs[0]], [bd[0]])
                    tt(P, "dve", di_, si, hsT[2][:, :, 0:L], ADD, [bs[1], bhs[2]], [bd[1]])
                    tt(P, "dve", dr_, dr_, hsT[1][:, :, 0:L], SUB, [bd[0], bhs[1]], [bd[0]])
                    tt(P, "dve", di_, di_, hsT[3][:, :, 0:L], ADD, [bd[1], bhs[3]], [bd[1]])
                    src, dst, bs, bd = dst, src, bd, bs
                    yield
                cp(P, "act", Sb16[:], src[:, :, :, PAD:PAD + 64], bs, [bS16])
                cp(P, "pool", SA[:, :, :, PAD:PAD + 1], src[:, :, :, PAD + 64:PAD + 65], bs, bSA)
                yield
                for ct in range(2 if DBG >= 3 else 0):
                    pa, pb, bpa, bpb = ptr[:, 0:512], ptr[:, 512:1024], bptr, bptrB
                    for j in range(8):
                        for jp in range(j + 1):
                            mm(P, pa[:, j * 64:(j + 1) * 64], S5.BDT[:, ct, j - jp, :], uT[:, ct, jp::8], jp == 0, jp == j,
                               [S5.b, buT], [bpa])
                        if j % 3 == 2:
                            yield
                    for j in range(8):
                        for hh in range(2):
                            i = 0
                            for lp in range(2):
                                pr = 4 * ct + 2 * hh + lp
                                for part in range(2):
                                    mm(P, pb[hh * 64:(hh + 1) * 64, j * 64:(j + 1) * 64], S5.QT[:, pr, j, part, :],
                                       Sb16[:, pr, part, :], i == 0, i == 3, [S5.b, bS16], [bpb])
                                    i += 1
                        if j % 2 == 1:
                            yield
                    lo = LPA["s5_d"][0]
                    stt(P, "dve", ya[:, ct, :].rearrange("p (j c) -> p j c", j=8), uT[:, ct, :].rearrange("p (c j) -> p j c", j=8),
                        lpa[:, lo + ct:lo + ct + 1], pa.rearrange("p (j c) -> p j c", j=8), MUL, ADD, [buT, bpa, K.blpa], [bya])
                    yield
                    tt(P, "dve", ya[:, ct, :], ya[:, ct, :], pb, ADD, [bya, bpb], [bya])
                    yield
                if DBG < 3:
                    P.op("dve", lambda e: e.memset(ya[:], 0.5), [], [bya])
                actf(P, yg[:], ya[:], AF.Gelu_apprx_tanh, [bya], [byg])
                lo = LPA["s5_bglu"][0]
                for mc in range(2 if DBG >= 4 else 0):
                    for kc in range(2):
                        mm(P, pvh[0], wglu[:, kc, mc * 128:(mc + 1) * 128], yg[:, kc, :], kc == 0, kc == 1, [bwglu, byg], [bpv])
                    yield
                    actf(P, sig[:], pvh[0], AF.Sigmoid, [bpv, K.blpa], [bsig], bias=lpa[:, lo + mc:lo + mc + 1])
                    yield
                    tt(P, "dve", yb[:, mc, :], yg[:, mc, :], sig[:], MUL, [byg, bsig], [byb])
                    yield
                if DBG < 4:
                    P.op("dve", lambda e: e.memset(yb[:], 0.5), [], [byb])
                if DBG >= 5:
                    fm_norm(K, yb, byb, LA("s5_on"), lambda ct: yst[:, ct, :].rearrange("p (c j) -> p j c", j=8), W2, byst[0], perm=True)
                yield

            def seq_(*gs):
                for g in gs:
                    yield from g
            live = [g_s5(), seq_(g_sc(), g_gm())]
            while live:
                for g in list(live):
                    try:
                        next(g)
                    except StopIteration:
                        live.remove(g)
            P.dma(yA.ap[:, :, blk * 512:(blk + 1) * 512], yst[:], reads=byst, writes=[yA.b[blk]])
        if st.s5_out is not None:
            P.dma(st.s5_out.rearrange("p (r c o) -> p r c o", r=8, c=2), SA[:, :, :, PAD:PAD + 1], reads=bSA, writes=[st.bout])
            P.dma(st.sc_out.rearrange("p (c o) -> p c o", c=2), z[:, :, 512:514], reads=[bz], writes=[st.bout2])


def mixer_pass_b(K, l, x_src, x_dst, yA, hTd, normg, w_in, w_out, lpa, st, en_gdn=True, wsel=None):
    P, T = K.P, K.T
    NB = T // 512
    with P.scope():
        wB = P.sb([128, KD, 1032], BF16); bwB = Buf()
        P.dma(wB[:], w_in[:, 768:1800].rearrange("(k p) c -> p k c", p=128), writes=[bwB], q="pool")
        wo = P.sb([128, KD, D], BF16); bwo = Buf()
        P.dma(wo[:], w_out.rearrange("(k p) c -> p k c", p=128), writes=[bwo], q="pool")
        xr = [P.sb([128, D]) for _ in range(2)]; bxr = bufs(2)
        xs = P.sb([128, D]); bxs = Buf()
        hT = P.sb([128, KD, 512], BF16); bhT = Buf()
        junk = P.sb([128, D], BF16); bjunk = Buf()
        ss = P.sb([128, 4]); bss = Buf(); rs = P.sb([128, 4]); brs = Buf()
        ptr = P.ps([128, 1024]); bptr = Buf(True); bptrB = Buf(True)
        pq = P.ps([128, 512]); bpq = Buf(True)
        pX = [P.ps([128, 512]) for _ in range(3)]; bpX = [Buf(True) for _ in range(3)]
        psA = P.ps([128, 512]); psB = P.ps([128, 512]); bpsA, bpsB = Buf(True), Buf(True)
        yAt = P.sb([128, 6, 512], BF16); byA = Buf()
        ygT = P.sb([128, 2, 512], BF16); byg = Buf()
        bdst = bufs(4)

        def LA(name):
            lo, hi = LPA[name]
            return lpa[:, lo:hi]
        if en_gdn:
            rawT = P.sb([128, 6, 515], BF16); braw = Buf()
            dgd = P.sb([128, 6, 4, 128], BF16); bdgd = Buf()
            lo = LPA["gdn_conv"][0]
            for ct in range(6):
                for j in range(4):
                    ts(P, "dve", dgd[:, ct, j, :], K.ident32[:], lpa[:, lo + ct * 4 + j:lo + ct * 4 + j + 1], None, MUL, None,
                       [K.blpa, K.bconst], [bdgd])
            P.op("dve", lambda e: e.memset(rawT[:], 0.0), [], [braw])
            qT = P.sb([128, 2, 512]); kT = P.sb([128, 2, 512]); vT = P.sb([128, 2, 512]); bq, bk, bv = Buf(), Buf(), Buf()
            sq = xs[:].rearrange("p (c t) -> p c t", c=2); bsq = bxs
            rk = P.sb([128, 512]); brk = Buf()
            kTm = P.sb([128, 2, 2, 512], BF16); qTm = P.sb([128, 2, 2, 512], BF16); bkm, bqm = Buf(), Buf()
            kTb = P.sb([128, 2, 512], BF16); qTb = P.sb([128, 2, 512], BF16); bkb, bqb = Buf(), Buf()
            kdm = P.sb([128, 2, 256], BF16); bkdm = Buf()
            ktm = P.sb([128, 4, 256]); vtm = P.sb([128, 4, 256]); bktm, bvtm = Buf(), Buf()
            zs = P.sb([128, 4, 256]); bzs = Buf()
            otm1 = P.sb([128, 256]); botm1 = Buf()
            ab4 = P.sb([128, 4, 8]); bab = Buf()
            sc = NS()
            for nm in ("beta", "nbeta", "g", "eG", "neG", "ekd", "rq", "eGrq"):
                setattr(sc, nm, P.sb([128, 4, 4]))
            sc.gtot = P.sb([128, 4, 2, 4]); bsc = bufs(4)
            negA = P.sb([128, 4]); bnegA = Buf()
            actf(P, negA[:], LA("a_log"), AF.Exp, [K.blpa], [bnegA])
            ts(P, "dve", negA[:], negA[:], -1.0, None, MUL, None, [bnegA], [bnegA])
            tmp4 = P.sb([128, 16]); btmp4 = Buf()
            gU2 = P.sb([128, 512]); bgU2 = Buf()
            expD = P.sb([128, 512]); DTs = P.sb([128, 512]); DTi = P.sb([128, 512]); bD = Buf()
            Ma = P.sb([128, 512], BF16); Mb = P.sb([128, 512], BF16); MTa = P.sb([128, 512], BF16); MTb = P.sb([128, 512], BF16)
            bM = Buf(); bMx = {}
            Z = [P.sb([128, 512], BF16) for _ in range(2)]; bZ = bufs(2)
            QKT = [P.sb([128, 512], BF16) for _ in range(2)]; bQK = bufs(2)
            S = P.sb([128, 2, 64]); bS = bufs(4)
            Sb = P.sb([128, 2, 64], BF16); bSb = bufs(2)
            P.op("dve", lambda e: e.memset(S[:], 0.0), [], bS)
            P.op("dve", lambda e: e.memset(Sb[:], 0.0), [], bSb)
            rp = P.sb([128, 4, 64], BF16); vnew = P.sb([128, 4, 64], BF16); tmpo = P.sb([128, 4, 64]); brp, bvn, bto = bufs(4), bufs(4), bufs(4)
            P.op("dve", lambda e: e.memset(rp[:], 0.0), [], brp)
            P.op("dve", lambda e: e.memset(vnew[:], 0.0), [], bvn)
            ygn = P.sb([128, 256]); bygn = Buf()
            on4 = P.sb([128, 8]); bon4 = Buf()
            c1 = P.const_ap(1.0)
            c64e = P.const_ap(64 * EPS)
            U1, U2 = cview(K, "U1"), cview(K, "U2")
            rep4 = lambda name: bcast(cview(K, name).unsqueeze(1), [128, 4, 128])
            v4 = lambda t: t[:].rearrange("p (h c) -> p h c", h=4)
            if st.gdn_s_init is not None:
                P.dma(S[:], st.gdn_s_init.rearrange("p (a b) -> p a b", a=2), reads=[], writes=bS)
                P.dma(rawT[:, :, 0:3], st.gdn_raw_init.rearrange("p (a b) -> p a b", a=6), reads=[], writes=[braw])
        else:
            P.op("dve", lambda e: e.memset(ygT[:], 0.0), [], [byg])

        def block_gen(blk):
            P.dma(hT[:], hTd.ap[:, :, blk * 512:(blk + 1) * 512], reads=[hTd.b[blk]], writes=[bhT])
            GD = 9
            if en_gdn and GD < 9:
                P.op("dve", lambda e: e.memset(ygT[:], 0.0), [], [byg])
            if en_gdn:
                cp(P, "pool", rawT[:, :, 0:3], rawT[:, :, 512:515], [braw], [braw])
                pqs = [(pq, bpq), (psA, bpsA)]
                for ct in range(6):
                    pq_, bpq_ = pqs[ct % 2]
                    for k in range(KD):
                        mm(P, pq_[:], wB[:, k, ct * 128:(ct + 1) * 128], hT[:, k, :], k == 0, k == KD - 1, [bwB, bhT], [bpq_])
                    cp(P, "act", rawT[:, ct, 3:515], pq_[:], [bpq_], [braw])
                    yield
                for ct in range(6):
                    pq_, bpq_ = pqs[ct % 2]
                    for j in range(4):
                        mm(P, pq_[:], dgd[:, ct, j, :], rawT[:, ct, j:j + 512], j == 0, j == 3, [bdgd, braw], [bpq_])
                    dst, bd = [(qT, bq), (kT, bk), (vT, bv)][ct // 2]
                    actf(P, dst[:, ct % 2, :], pq_[:], AF.Silu, [bpq_], [bd])
                    yield
                actf(P, sq, kT[:], AF.Square, [bk], [bsq])
                for ct in range(2 if GD >= 2 else 0):
                    mm(P, pq[:], cview(K, "onesblk"), sq[:, ct, :], True, True, [bsq, K.bconst], [bpq])
                    actf(P, rk[:], pq[:], AF.Sqrt, [bpq], [brk], bias=P.epst[:, 0:1], scale=1.0)
                    P.op("dve", lambda e: e.reciprocal(out=rk[:], in_=rk[:]), [brk], [brk])
                    tt(P, "dve", kT[:, ct, :], kT[:, ct, :], rk[:], MUL, [bk, brk], [bk])
                    yield
                cp(P, "act", kTb[:], kT[:], [bk], [bkb])
                cp(P, "act", qTb[:], qT[:], [bq], [bqb])
                hsel = cview(K, "headsel")
                for hp in range(2 if GD >= 2 else 0):
                    actf(P, kTm[:, hp, :, :], kT[:], AF.Copy, [bk, K.bconst], [bkm], scale=hsel[:, hp:hp + 1])
                    actf(P, qTm[:, hp, :, :], qT[:], AF.Copy, [bq, K.bconst], [bqm], scale=hsel[:, hp:hp + 1])
                actf(P, sq, qT[:], AF.Square, [bq], [bsq])
                for n in range(4 if GD >= 2 else 0):
                    for ct in range(2):
                        mm(P, pX[0][:, n * 4 + 2 * ct:n * 4 + 2 * ct + 2], sq[:, ct, n * 128:(n + 1) * 128], cview(K, "headsel"),
                           True, True, [bsq, K.bconst], [bpX[0]])
                actf(P, tmp4[:], pX[0][:, 0:16], AF.Sqrt, [bpX[0]], [btmp4], bias=c64e, scale=64.0)
                P.op("dve", lambda e: e.reciprocal(out=sc.rq[:].rearrange("p a b -> p (a b)"), in_=tmp4[:]), [btmp4], bsc)
                for n in range(4):
                    pt_, bpt_ = (ptr[:, 0:512], bptr) if n % 2 == 0 else (ptr[:, 512:1024], bptrB)
                    for ct in range(2):
                        P.op("pe", lambda e, n=n, ct=ct, pt_=pt_: e.transpose(out=pt_[:, ct * 128:(ct + 1) * 128],
                                                                             in_=kT[:, ct, n * 128:(n + 1) * 128], identity=K.ident32[:]),
                             [bk, K.bconst], [bpt_])
                        P.op("pe", lambda e, n=n, ct=ct, pt_=pt_: e.transpose(out=pt_[:, 256 + ct * 128:256 + (ct + 1) * 128],
                                                                             in_=vT[:, ct, n * 128:(n + 1) * 128], identity=K.ident32[:]),
                             [bv, K.bconst], [bpt_])
                    cp(P, "act", ktm[:, n, :], pt_[:, 0:256], [bpt_], [bktm])
                    cp(P, "dve", vtm[:, n, :], pt_[:, 256:512], [bpt_], [bvtm])
                    yield
                for n in range(4):
                    pq_, bpq_ = pqs[n % 2]
                    for k in range(KD):
                        mm(P, pq_[:, 0:264], hT[:, k, n * 128:(n + 1) * 128], wB[:, k, 768:1032], k == 0, k == KD - 1, [bwB, bhT], [bpq_])
                    actf(P, zs[:, n, :], pq_[:, 0:256], AF.Silu, [bpq_], [bzs])
                    cp(P, "dve", ab4[:, n, :], pq_[:, 256:264], [bpq_], [bab])
                    yield
                f16 = lambda t: t[:].rearrange("p a b -> p (a b)")
                B_ = list(bsc)
                actf(P, sc.beta[:], ab4[:, :, 4:8], AF.Sigmoid, [bab], B_)
                ts(P, "dve", f16(sc.nbeta), f16(sc.beta), -1.0, None, MUL, None, B_, B_)
                tt(P, "dve", sc.g[:], ab4[:, :, 0:4], bcast(LA("dtb").unsqueeze(1), [128, 4, 4]), ADD, [bab, K.blpa], B_)
                actf(P, f16(sc.g), f16(sc.g), AF.Exp, B_, B_)
                actf(P, f16(sc.g), f16(sc.g), AF.Ln, B_, B_, bias=c1, scale=1.0)
                tt(P, "dve", sc.g[:], sc.g[:], bcast(negA[:].unsqueeze(1), [128, 4, 4]), MUL, B_ + [bnegA], B_)
                mm(P, pX[1][:, 0:16], U1, f16(sc.g), True, True, B_ + [K.bconst], [bpX[1]])
                mm(P, pX[1][:, 16:32], U2, f16(sc.g), True, True, B_ + [K.bconst], [bpX[1]])
                mm(P, pX[1][:, 32:48], cview(K, "chunk0"), f16(sc.g), True, True, B_ + [K.bconst], [bpX[1]])
                mm(P, pX[1][:, 48:64], cview(K, "chunk1"), f16(sc.g), True, True, B_ + [K.bconst], [bpX[1]])
                actf(P, f16(sc.eG), pX[1][:, 0:16], AF.Exp, [bpX[1]], B_)
                actf(P, f16(sc.ekd), pX[1][:, 16:32], AF.Exp, [bpX[1]], B_)
                for cc in range(2):
                    actf(P, sc.gtot[:, :, cc, :], pX[1][:, 32 + 16 * cc:48 + 16 * cc].rearrange("p (a b) -> p a b", a=4), AF.Exp, [bpX[1]], B_)
                ts(P, "dve", f16(sc.neG), f16(sc.eG), -1.0, None, MUL, None, B_, B_)
                tt(P, "dve", f16(sc.eGrq), f16(sc.eG), f16(sc.rq), MUL, B_, B_)
                def prep(n):
                    nb = n % 2
                    tk = slice(n * 128, (n + 1) * 128)
                    for h in range(4):
                        ts(P, "dve", gU2[:, h * 128:(h + 1) * 128], U2, sc.g[:, n, h:h + 1], None, MUL, None, [bsc[n], K.bconst], [bgU2])
                    yield
                    for h in range(4):
                        mm(P, pX[0][:, h * 128:(h + 1) * 128], gU2[:, h * 128:(h + 1) * 128], U1, True, True, [bgU2, K.bconst], [bpX[0]])
                    for h in range(4):
                        pair, hp = h // 2, h % 2
                        mm(P, pX[1][:, h * 128:(h + 1) * 128], kTm[:, hp, pair, tk], kTb[:, pair, tk], True, True, [bkb, bkm], [bpX[1]])
                    for h in range(4):
                        pair, hp = h // 2, h % 2
                        mm(P, pX[2][:, h * 128:(h + 1) * 128], kTm[:, hp, pair, tk], qTb[:, pair, tk], True, True, [bkm, bqb], [bpX[2]])
                    yield
                    actf(P, expD[:], pX[0][:], AF.Exp, [bpX[0]], [bD])
                    yield
                    tt(P, "dve", v4(DTs), v4(expD), rep4("maskS"), MUL, [bD, K.bconst], [bD])
                    tt(P, "pool", v4(DTi), v4(expD), rep4("maskI"), MUL, [bD, K.bconst], [bD])
                    yield
                    for h in range(4):
                        stt(P, "dve", Ma[:, h * 128:(h + 1) * 128], pX[1][:, h * 128:(h + 1) * 128], sc.nbeta[:, n, h:h + 1],
                            DTs[:, h * 128:(h + 1) * 128], MUL, MUL, [bpX[1], bsc[n], bD], [bM])
                    yield
                    tt(P, "dve", QKT[nb][:], pX[2][:], DTi[:], MUL, [bpX[2], bD], [bQK[nb]])
                    tt(P, "dve", v4(Z[nb]), v4(Ma), rep4("ident"), ADD, [bM, K.bconst], [bZ[nb]])
                    pX0b = pX[0][:].bitcast(BF16)
                    for h in range(4):
                        P.op("pe", lambda e, h=h: e.transpose(out=pX0b[:, h * 128:(h + 1) * 128], in_=Ma[:, h * 128:(h + 1) * 128],
                                                              identity=K.ident_bf[:]), [bM, K.bconst], [bpX[0]])
                    yield
                    cp(P, "act", MTa[:], pX0b[:, 0:512], [bpX[0]], [bM])
                    yield
                    for t_ in (Ma, Mb, MTa, MTb):
                        bMx.setdefault(id(t_), Buf())
                    bMx[id(Ma)].lw = bM.lw; bMx[id(MTa)].lw = bM.lw
                    B_ = lambda t_: bMx[id(t_)]
                    Mc, MTc, Mn, MTn = Ma, MTa, Mb, MTb
                    pend = None
                    for lv in range(1, 6):
                        for h in range(4):
                            hs = slice(h * 128, (h + 1) * 128)
                            mm(P, pX[1][:, hs], Mc[:, hs], MTc[:, hs], True, True, [B_(Mc), B_(MTc)], [bpX[1]])
                        if lv < 5:
                            for h in range(4):
                                hs = slice(h * 128, (h + 1) * 128)
                                mm(P, pX[0][:, hs], MTc[:, hs], Mc[:, hs], True, True, [B_(Mc), B_(MTc)], [bpX[0]])
                        if pend is not None:
                            pend()
                        yield
                        cp(P, "act", MTn[:], pX[1][:], [bpX[1]], [B_(MTn)])
                        if lv < 5:
                            cp(P, "dve", Mn[:], pX[0][:], [bpX[0]], [B_(Mn)])
                        yield

                        def zupd(MTn=MTn):
                            for h in range(4):
                                hs = slice(h * 128, (h + 1) * 128)
                                mm(P, pX[2][:, hs], MTn[:, hs], Z[nb][:, hs], True, True, [B_(MTn), bZ[nb]], [bpX[2]])
                            tt(P, "dve", Z[nb][:], Z[nb][:], pX[2][:], ADD, [bZ[nb], bpX[2]], [bZ[nb]])
                        pend = zupd
                        Mc, MTc, Mn, MTn = Mn, MTn, Mc, MTc
                    pend()
                    P.join([bM], [bMx[id(t_)] for t_ in (Ma, Mb, MTa, MTb)] + [bM])

                def rec(n):
                    nb = n % 2
                    tk = slice(n * 128, (n + 1) * 128)
                    bankA, bankB, bankC = psA, psB, ptr[:, 512:1024]
                    bA, bB, bC = bpsA, bpsB, bptrB
                    H = [(h, h // 2, h % 2, slice((h % 2) * 64, (h % 2 + 1) * 64), slice(h * 64, (h + 1) * 64),
                          slice(h * 128, (h + 1) * 128)) for h in range(4)]
                    for cc in range(2):
                        cm = cview(K, "chunk%d" % cc)
                        stt(P, "dve", kdm[:, cc, :].rearrange("p (h c) -> p h c", h=4), ktm[:, n, :].rearrange("p (h c) -> p h c", h=4),
                            cm[:, 0:1], bcast(sc.ekd[:, n, :].unsqueeze(2), [128, 4, 64]), MUL, MUL, [bktm, bsc[n], K.bconst], [bkdm])
                    yield
                    for cc in range(2):
                        tp = slice(cc * 64, (cc + 1) * 64)
                        for h, pair, hp, kp, hc, hs in H:
                            mm(P, bankA[:, hc], kTm[:, hp, pair, tk], Sb[:, pair, :], True, True, [bkm, bSb[pair]], [bA])
                        for h, pair, hp, kp, hc, hs in H:
                            mm(P, bankB[:, hc], qTm[:, hp, pair, tk], Sb[:, pair, :], True, True, [bqm, bSb[pair]], [bB])
                        yield
                        for h, pair, hp, kp, hc, hs in H:
                            stt(P, "dve", rp[tp, h, :], bankA[tp, hc], sc.neG[tp, n, h:h + 1], vtm[tp, n, hc], MUL, ADD,
                                [bA, bsc[n], bvtm], [brp[h]])
                        for h, pair, hp, kp, hc, hs in H:
                            actf(P, tmpo[tp, h, :], bankB[tp, hc], AF.Copy, [bB, bsc[n]], [bto[h]], scale=sc.eGrq[tp, n, h:h + 1])
                        yield
                        for h, pair, hp, kp, hc, hs in H:
                            mm(P, bankC[:, hc], Z[nb][:, hs], rp[:, h, :], True, True, [bZ[nb], brp[h]], [bC])
                        yield
                        for h, pair, hp, kp, hc, hs in H:
                            ts(P, "dve", vnew[tp, h, :], bankC[tp, hc], sc.beta[tp, n, h:h + 1], None, MUL, None, [bC, bsc[n]], [bvn[h]])
                        yield
                        for h, pair, hp, kp, hc, hs in H:
                            mm(P, bankA[:, hc], QKT[nb][:, hs], vnew[:, h, :], True, True, [bQK[nb], bvn[h]], [bA])
                        for h, pair, hp, kp, hc, hs in H:
                            mm(P, bankB[:, hc], kdm[:, cc, pair * 128:(pair + 1) * 128], vnew[:, h, :], True, True, [bkdm, bvn[h]], [bB])
                        yield
                        for h, pair, hp, kp, hc, hs in H:
                            stt(P, "dve", otm1[tp, hc], bankA[tp, hc], sc.rq[tp, n, h:h + 1], tmpo[tp, h, :], MUL, ADD,
                                [bA, bsc[n], bto[h]], [botm1])
                        for h, pair, hp, kp, hc, hs in H:
                            stt(P, "dve", S[kp, pair, :], S[kp, pair, :], sc.gtot[kp, n, cc, h:h + 1], bankB[kp, hc], MUL, ADD,
                                [bS[h], bB, bsc[n]], [bS[h]])
                        for pair in range(2):
                            cp(P, "act", Sb[:, pair, :], S[:, pair, :], [bS[2 * pair], bS[2 * pair + 1]], [bSb[pair]])
                        yield
                    tt(P, "pool", ygn[:], otm1[:], otm1[:], MUL, [botm1], [bygn])
                    P.op("dve", lambda e: e.tensor_reduce(out=on4[:, 0:4], in_=ygn[:].rearrange("p (h c) -> p h c", h=4),
                                                          axis=AX.X, op=ADD), [bygn], [bon4])
                    yield
                    rsqrt_op(P, on4[:, 4:8], on4[:, 0:4], 1.0 / 64, bon4, bon4)
                    yield
                    tt(P, "dve", v4(ygn), otm1[:].rearrange("p (h c) -> p h c", h=4), bcast(on4[:, 4:8].unsqueeze(2), [128, 4, 64]),
                       MUL, [botm1, bon4, bygn], [bygn])
                    tt(P, "dve", ygn[:], ygn[:], LA("gdnn"), MUL, [bygn, K.blpa], [bygn])
                    tt(P, "dve", ygn[:], ygn[:], zs[:, n, :], MUL, [bygn, bzs], [bygn])
                    yield
                    for ct in range(2):
                        P.op("pe", lambda e, ct=ct: e.transpose(out=ptr[:, ct * 128:(ct + 1) * 128], in_=ygn[:, ct * 128:(ct + 1) * 128],
                                                                identity=K.ident32[:]), [bygn, K.bconst], [bptr])
                    yield
                    cp(P, "act", ygT[:, :, n * 128:(n + 1) * 128], ptr[:, 0:256].rearrange("p (c t) -> p c t", c=2), [bptr], [byg])

                def interleave(*gens):
                    live = [g for g in gens if g is not None]
                    while live:
                        for g in list(live):
                            try:
                                next(g)
                            except StopIteration:
                                live.remove(g)
                yield "front_done"
                P.dma(yAt[:], yA.ap[:, :, blk * 512:(blk + 1) * 512], reads=[yA.b[blk]], writes=[byA])
                interleave(prep(0))
                for n in range(4):
                    interleave(rec(n), prep(n + 1) if n < 3 else None)
            else:
                yield "front_done"
                P.dma(yAt[:], yA.ap[:, :, blk * 512:(blk + 1) * 512], reads=[yA.b[blk]], writes=[byA])
            yield "tiles_done"
            ysrc = [yAt[:, 0], yAt[:, 1], yAt[:, 2], yAt[:, 3], ygT[:, 0], ygT[:, 1], yAt[:, 4], yAt[:, 5]]
            def ld_x(n_):
                P.dma(xr[n_ % 2][:], x_src.ap[blk * 512 + n_ * 128:blk * 512 + (n_ + 1) * 128, :], reads=[x_src.b[blk]], writes=[bxr[n_ % 2]])
            ld_x(0)
            ld_x(1)
            for n in range(4):
                i = n % 2
                r0 = blk * 512 + n * 128
                for hf in range(2):
                    pw_, bpw_ = (pq, bpq) if hf == 0 else (psB, bpsB)
                    for k in range(KD):
                        mm(P, pw_[:], ysrc[k][:, n * 128:(n + 1) * 128], wo[:, k, hf * 512:(hf + 1) * 512], k == 0, k == KD - 1,
                           [byA, byg, bwo], [bpw_])
                    tt(P, "dve", xr[i][:, hf * 512:(hf + 1) * 512], xr[i][:, hf * 512:(hf + 1) * 512], pw_[:], ADD, [bxr[i], bpw_], [bxr[i]])
                if wsel is None:
                    P.dma(x_dst.ap[r0:r0 + 128, :], xr[i][:], reads=[bxr[i]], writes=[bdst[n]])
                else:
                    half, g = blk // (NB // 2), blk % (NB // 2)
                    rr = g * 512 + n * 128
                    ts(P, "dve", xr[i][:], xr[i][:], wsel[:, half:half + 1], None, MUL, None, [bxr[i], K.bconst], [bxr[i]])
                    if half == 0:
                        P.dma(x_dst.ap[rr:rr + 128, :], xr[i][:], reads=[bxr[i]], writes=[bdst[n]])
                    else:
                        P.dma(x_dst.ap[rr:rr + 128, :], xr[i][:], reads=[bxr[i], x_dst.b[g]], writes=[bdst[n]], q="pool", accum_op=ADD)
                if n + 2 < 4:
                    ld_x(n + 2)
                yield
            gi = blk if wsel is None else blk % (NB // 2)
            x_dst.b[gi] = Buf()
            P.join([x_dst.b[gi]], bdst)

        def run_until(g, marker):
            for v in g:
                if v == marker:
                    return

        gens = [block_gen(b_) for b_ in range(NB)]
        run_until(gens[0], "front_done")
        for b_ in range(NB):
            run_until(gens[b_], "tiles_done")
            nxt_ = gens[b_ + 1] if b_ + 1 < NB else None
            tail_done = False
            front_done = nxt_ is None
            while not (tail_done and front_done):
                if not tail_done:
                    try:
                        next(gens[b_])
                    except StopIteration:
                        tail_done = True
                if not front_done:
                    if next(nxt_) == "front_done":
                        front_done = True
        if en_gdn and st.gdn_s_out is not None:
            P.dma(st.gdn_s_out.rearrange("p (a b) -> p a b", a=2), S[:], reads=bS, writes=[st.bout3])
            P.dma(st.gdn_raw_out.rearrange("p (a b) -> p a b", a=6), rawT[:, :, 512:515], reads=[braw], writes=[st.bout4])


def host_inputs(inp):
    def ng(v):
        return np.ascontiguousarray(np.asarray(v, np.float32).reshape(KD, 128).T)
    normg = np.concatenate([ng(inp['mix_norm'][0]), ng(inp['ffn_norm'][0]), ng(inp['mix_norm'][1]), ng(inp['ffn_norm'][1])], axis=1)
    lp = [pack_layer_params(inp, l) for l in range(2)]
    f32 = lambda a: np.ascontiguousarray(np.asarray(a, np.float32))
    return dict(consts=make_consts(), normg=normg, final_norm=f32(inp['final_norm'])[None, :],
                w_in=f32(inp['w_in']), w_out=f32(inp['w_out']),
                lpa=np.stack([lp[0][0], lp[1][0]]), lps=np.stack([lp[0][1], lp[1][1]]),
                ffn_w_gate=f32(inp['ffn_w_gate'][0]), ffn_w_up=f32(inp['ffn_w_up'][0]), ffn_w_down=f32(inp['ffn_w_down'][0]),
                moe_router=f32(inp['moe_router'][0]), moe_w_gate=f32(inp['moe_w_gate'][0]), moe_w_up=f32(inp['moe_w_up'][0]),
                moe_w_down=f32(inp['moe_w_down'][0]))


_PROG_CACHE = {}


def kernel(**inputs):
    inp = {k: np.asarray(v) for k, v in inputs.items()}
    x = np.ascontiguousarray(inp["x"], dtype=np.float32)
    B, L, _ = x.shape
    T = L
    if T not in _PROG_CACHE:
        _PROG_CACHE[T] = build_program(T, split=True)[0]
    nc = _PROG_CACHE[T]
    shared = host_inputs(inp)
    n_cores = 2 * B
    in_maps = []
    for c in range(n_cores):
        m = dict(shared)
        m["x"] = x[c // 2]
        w = np.zeros((128, 2), np.float32)
        w[:, c % 2] = 1.0
        m["wsel"] = w
        in_maps.append(m)
    res = run_bass_kernel_spmd(nc, in_maps, core_ids=list(range(n_cores)))
    out = np.zeros((B, L, D), np.float32)
    for c in range(n_cores):
        out[c // 2, (c % 2) * (L // 2):(c % 2 + 1) * (L // 2)] = np.asarray(res.results[c]["out"], dtype=np.float32)
    return out
```

```python
import numpy as np
import ml_dtypes
import concourse.bass as bass
import concourse.mybir as mybir
from concourse.bass_utils import run_bass_kernel_spmd
from contextlib import ExitStack, contextmanager

F32 = mybir.dt.float32
BF16 = mybir.dt.bfloat16
AF = mybir.ActivationFunctionType
ALU = mybir.AluOpType
AX = mybir.AxisListType

D = 1024
KD = 8
EPS = 1e-6
N_EXP = 8
D_FF = 2816
D_FFE = 3584
IN_COLS = 2568


class Buf:
    __slots__ = ("lw", "rd", "x")

    def __init__(self, x=False):
        self.lw = None
        self.rd = []
        self.x = x


def bufs(n):
    return [Buf() for _ in range(n)]


class Prog:
    NDMA = 16

    def __init__(self, nc, es):
        self.nc = nc
        self.es = es
        self.stack = [es]
        self.cnt = {}
        self.sem = {}
        self.known = {}
        self.eng = {"pe": nc.tensor, "act": nc.scalar, "dve": nc.vector, "pool": nc.gpsimd, "sp": nc.sync}
        for e in self.eng:
            self.cnt[e] = 0
            self.sem[e] = es.enter_context(nc.semaphore("s_" + e))
            self.known[e] = {}
        self.dsem, self.dcnt, self.drot = {}, {}, {}
        for e in ("sp", "pool", "act"):
            self.dsem[e] = [es.enter_context(nc.semaphore("d_%s%d" % (e, i))) for i in range(self.NDMA)]
            self.dcnt[e] = [0] * self.NDMA
            self.drot[e] = 0
        self.nid = 0
        self.ninst = 0
        self.consts_ = {}

    @contextmanager
    def scope(self):
        with ExitStack() as es:
            self.stack.append(es)
            try:
                yield
            finally:
                self.barrier()
                self.stack.pop()

    def barrier(self):
        for e in self.eng:
            waits = []
            kn = self.known[e]
            for f in self.eng:
                if self.cnt[f] and kn.get(f, 0) < self.cnt[f]:
                    kn[f] = self.cnt[f]
                    waits.append((f, self.cnt[f]))
            for q in self.dsem:
                for i in range(self.NDMA):
                    key, v = (q, i), self.dcnt[q][i]
                    if v and kn.get(key, 0) < v:
                        kn[key] = v
                        waits.append((key, v))
            self._emit(e, waits, None, None)

    def sb(self, shape, dt=F32):
        self.nid += 1
        return self.stack[-1].enter_context(self.nc.sbuf_tensor("sb%d" % self.nid, list(shape), dt))

    def ps(self, shape, dt=F32):
        self.nid += 1
        return self.stack[-1].enter_context(self.nc.psum_tensor("ps%d" % self.nid, list(shape), dt))

    def _semobj(self, key):
        if isinstance(key, str):
            return self.sem[key]
        return self.dsem[key[0]][key[1]]

    def _deps(self, eng, reads, writes):
        need = {}
        for b in reads:
            if b.lw is not None:
                k, v = b.lw
                if need.get(k, 0) < v:
                    need[k] = v
        for b in writes:
            if b.lw is not None and b.lw[0] != eng:
                k, v = b.lw
                if need.get(k, 0) < v:
                    need[k] = v
            for k, v in b.rd:
                if k != eng and need.get(k, 0) < v:
                    need[k] = v
        kn = self.known[eng]
        out = []
        for k, v in need.items():
            if kn.get(k, 0) < v:
                kn[k] = v
                out.append((k, v))
        return out

    def _emit(self, engname, waits, fn, inc):
        e = self.eng[engname]
        for k, v in waits:
            e.wait_ge(self._semobj(k), v)
        if fn is None:
            return
        ins = fn(e)
        ins.then_inc(self._semobj(inc[0]), inc[1])
        self.ninst += 1

    def op(self, eng, fn, reads=(), writes=()):
        if any(b.x for b in reads):
            writes = list(writes) + [b for b in reads if b.x and b not in writes]
            reads = [b for b in reads if not b.x]
        waits = self._deps(eng, reads, writes)
        self.cnt[eng] += 1
        tag = (eng, self.cnt[eng])
        self._emit(eng, waits, fn, (eng, 1))
        for b in reads:
            b.rd.append(tag)
        for b in writes:
            b.lw = tag
            b.rd = []

    def dma(self, out, in_, reads=(), writes=(), q="sp", **kw):
        i = self.drot[q]
        self.drot[q] = (i + 1) % self.NDMA
        key = (q, i)
        waits = self._deps(q, reads, writes)
        prev = self.dcnt[q][i]
        if prev and self.known[q].get(key, 0) < prev:
            self.known[q][key] = prev
            waits.append((key, prev))
        self.dcnt[q][i] = prev + 16
        tag = (key, prev + 16)
        self._emit(q, waits, lambda e: e.dma_start(out=out, in_=in_, **kw), (key, 16))
        for b in reads:
            b.rd.append(tag)
        for b in writes:
            b.lw = tag
            b.rd = []

    def eps_ap(self, like, eps):
        return self.epst[0:like.shape[0], 0:1]

    def const_ap(self, val):
        if val not in self.consts_:
            t = self.es.enter_context(self.nc.sbuf_tensor("cst%d" % len(self.consts_), [128, 1], F32))
            b = Buf()
            self.op("dve", lambda e: e.memset(t[:], float(val)), [], [b])
            for e_ in ("act", "pool", "pe"):
                self.wait_all(e_, [b])
            self.consts_[val] = t
        return self.consts_[val][:, 0:1]

    def join(self, dst, srcs):
        self.op("sp", lambda e: e.nop(), reads=list(srcs), writes=list(dst))

    def wait_all(self, eng, bl):
        self._emit(eng, self._deps(eng, bl, ()), None, None)


def bcast(ap, shape):
    return ap.broadcast_to(list(shape))


def rsqrt_op(P, out, in_, scale, bin_, bout, eps=EPS):
    P.op("act", lambda e: e.activation(out=out, in_=in_, func=AF.Sqrt, bias=P.eps_ap(out, eps), scale=scale),
         reads=[bin_], writes=[bout])
    P.op("dve", lambda e: e.reciprocal(out=out, in_=out), reads=[bout], writes=[bout])


def ffn_stage(K, x_src, x_dst, normg, experts, router, final_g):
    P, nc, T = K.P, K.nc, x_src.T
    TB = min(T, 2048)
    NT = TB // 128
    NSB = TB // 512
    moe = router is not None
    with P.scope():
        xacc = P.sb([128, NT, D]); bx = bufs(NT)
        hT = P.sb([128, KD, TB], BF16); bh = bufs(NT)
        xs = [P.sb([128, D]) for _ in range(2)]; bxs = bufs(2)
        h32 = [P.sb([128, D]) for _ in range(2)]; bh32 = bufs(2)
        junk = P.sb([128, D], BF16); bjunk = Buf()
        ss = P.sb([128, NT]); bss = bufs(NT)
        rs = P.sb([128, NT]); brs = bufs(NT)
        gate = P.sb([128, NT, 8]); bgate = bufs(NT)
        sm = P.sb([128, 64]); bsm = Buf()
        wg = [P.sb([128, KD, 512], BF16) for _ in range(2)]; bwg = bufs(2)
        wu = [P.sb([128, KD, 512], BF16) for _ in range(2)]; bwu = bufs(2)
        wd = [P.sb([128, 4, D], BF16) for _ in range(2)]; bwd = bufs(2)
        act = [P.sb([128, 4, 512], BF16) for _ in range(2)]; bact = bufs(2)
        sg = [P.sb([128, 512]) for _ in range(2)]; bsg = bufs(2)
        pg = [P.ps([128, 512]) for _ in range(2)]; bpg = [Buf(True), Buf(True)]
        pu = [P.ps([128, 512]) for _ in range(2)]; bpu = [Buf(True), Buf(True)]
        pd = [P.ps([128, 512]) for _ in range(2)]; bpd = [Buf(True), Buf(True)]
        ptr = P.ps([128, 1024]); bptr = Buf(True)
        if moe:
            wr = P.sb([128, KD, 8]); bwr = Buf()
            P.dma(wr[:], router.rearrange("(k p) e -> p k e", p=128), writes=[bwr])
            lg = P.sb([128, 8]); blg = Buf()
            m8 = P.sb([128, 8]); bm8 = Buf()
        if final_g is not None:
            fg = P.sb([128, D]); bfg = Buf()
            P.dma(fg[:], final_g.partition_broadcast(128), writes=[bfg])
        gcnt = 0
        piece_idx = 0
        for tb in range(T // TB):
            r0 = tb * TB
            for n0 in range(0, NT, 4):
                P.dma(xacc[:, n0:n0 + 4, :],
                      x_src.ap[r0 + n0 * 128: r0 + (n0 + 4) * 128, :].rearrange("(n p) d -> p n d", p=128),
                      reads=[x_src.b[(r0 + n0 * 128) // 512]], writes=bx[n0:n0 + 4])
            def prep_tile(n):
                s2 = n % 2
                P.op("act", lambda e, n=n: e.activation(out=junk[:], in_=xacc[:, n, :], func=AF.Square,
                                                        accum_out=ss[:, n:n + 1]),
                     reads=[bx[n]], writes=[bjunk, bss[n]])
                rsqrt_op(P, rs[:, n:n + 1], ss[:, n:n + 1], 1.0 / D, bss[n], brs[n])
                P.op("act", lambda e, n=n, s2=s2: e.activation(out=xs[s2][:], in_=xacc[:, n, :], func=AF.Copy,
                                                               scale=rs[:, n:n + 1]),
                     reads=[bx[n], brs[n]], writes=[bxs[s2]])
                for k in range(KD):
                    P.op("pe", lambda e, k=k, s2=s2: e.transpose(out=ptr[:, k * 128:(k + 1) * 128],
                                                                 in_=xs[s2][:, k * 128:(k + 1) * 128],
                                                                 identity=K.ident32[:]),
                         reads=[bxs[s2], K.bconst], writes=[bptr])
                P.op("dve", lambda e, s2=s2: e.scalar_tensor_tensor(
                    out=h32[s2][:].rearrange("p (k t) -> p k t", k=KD),
                    in0=ptr[:].rearrange("p (k t) -> p k t", k=KD), scalar=1.0,
                    in1=bcast(normg.unsqueeze(2), [128, KD, 128]), op0=ALU.mult, op1=ALU.mult),
                    reads=[bptr, K.bconst], writes=[bh32[s2]])
                P.op("pool", lambda e, n=n, s2=s2: e.tensor_copy(
                    out=hT[:, :, n * 128:(n + 1) * 128], in_=h32[s2][:].rearrange("p (k t) -> p k t", k=KD)),
                    reads=[bh32[s2]], writes=[bh[n]])
                if moe:
                    for k in range(KD):
                        P.op("pe", lambda e, k=k, s2=s2: e.matmul(pd[0][:, 0:8], lhsT=h32[s2][:, k * 128:(k + 1) * 128],
                                                                  rhs=wr[:, k, :], start=(k == 0), stop=(k == KD - 1)),
                             reads=[bh32[s2], bwr], writes=[bpd[0]])
                    P.op("dve", lambda e: e.tensor_copy(out=lg[:], in_=pd[0][:, 0:8]), reads=[bpd[0]], writes=[blg])
                    P.op("dve", lambda e: e.max(out=m8[:], in_=lg[:]), reads=[blg], writes=[bm8])
                    P.op("dve", lambda e: e.tensor_tensor(out=sm[:, 2:3], in0=m8[:, 0:1], in1=m8[:, 1:2], op=ALU.subtract),
                         reads=[bm8], writes=[bsm])
                    P.op("act", lambda e: e.activation(out=sm[:, 0:1], in_=sm[:, 2:3], func=AF.Sigmoid),
                         reads=[bsm], writes=[bsm])
                    P.op("act", lambda e: e.activation(out=sm[:, 1:2], in_=sm[:, 2:3], func=AF.Sigmoid, scale=-1.0),
                         reads=[bsm], writes=[bsm])
                    P.op("dve", lambda e: e.tensor_scalar(out=sm[:, 8:16], in0=lg[:], scalar1=m8[:, 0:1], scalar2=sm[:, 0:1],
                                                          op0=ALU.is_equal, op1=ALU.mult),
                         reads=[blg, bm8, bsm], writes=[bsm])
                    P.op("dve", lambda e: e.tensor_scalar(out=sm[:, 16:24], in0=lg[:], scalar1=m8[:, 1:2], scalar2=sm[:, 1:2],
                                                          op0=ALU.is_equal, op1=ALU.mult),
                         reads=[blg, bm8, bsm], writes=[bsm])
                    P.op("dve", lambda e, n=n: e.tensor_tensor(out=gate[:, n, :], in0=sm[:, 8:16], in1=sm[:, 16:24], op=ALU.add),
                         reads=[bsm], writes=[bgate[n]])
            first_piece = True
            pend = []
            for ei, ex in enumerate(experts):
                nch = ex["ff"] // 128
                for c0 in range(0, nch, 4):
                    ncp = min(4, nch - c0)
                    sl = piece_idx % 2
                    piece_idx += 1
                    P.dma(wg[sl][:, :, 0:ncp * 128], ex["wg"][:, c0 * 128:(c0 + ncp) * 128].rearrange("(k p) f -> p k f", p=128),
                          writes=[bwg[sl]], q="pool")
                    P.dma(wu[sl][:, :, 0:ncp * 128], ex["wu"][:, c0 * 128:(c0 + ncp) * 128].rearrange("(k p) f -> p k f", p=128),
                          writes=[bwu[sl]], q="pool")
                    P.dma(wd[sl][:, 0:ncp, :], ex["wd"][c0 * 128:(c0 + ncp) * 128, :].rearrange("(c p) d -> p c d", p=128),
                          writes=[bwd[sl]], q="pool")
                    for sbk in range(NSB):
                        if first_piece:
                            if sbk == 0:
                                for n_ in range(0, 4):
                                    prep_tile(n_)
                            if sbk + 1 < NSB:
                                for n_ in range((sbk + 1) * 4, (sbk + 2) * 4):
                                    prep_tile(n_)
                        asl = gcnt % 2
                        gcnt += 1
                        hb = bh[sbk * 4:(sbk + 1) * 4]
                        for c in range(ncp):
                            pp = c % 2
                            for k in range(KD):
                                P.op("pe", lambda e, k=k, c=c, pp=pp, sl=sl, sbk=sbk: e.matmul(
                                    pg[pp][:], lhsT=wg[sl][:, k, c * 128:(c + 1) * 128], rhs=hT[:, k, sbk * 512:(sbk + 1) * 512],
                                    start=(k == 0), stop=(k == KD - 1)), reads=[bwg[sl]] + hb, writes=[bpg[pp]])
                            for k in range(KD):
                                P.op("pe", lambda e, k=k, c=c, pp=pp, sl=sl, sbk=sbk: e.matmul(
                                    pu[pp][:], lhsT=wu[sl][:, k, c * 128:(c + 1) * 128], rhs=hT[:, k, sbk * 512:(sbk + 1) * 512],
                                    start=(k == 0), stop=(k == KD - 1)), reads=[bwu[sl]] + hb, writes=[bpu[pp]])
                            P.op("act", lambda e, pp=pp: e.activation(out=sg[pp][:], in_=pg[pp][:], func=AF.Silu),
                                 reads=[bpg[pp]], writes=[bsg[pp]])
                            P.op("dve", lambda e, pp=pp, asl=asl, c=c: e.tensor_tensor(out=act[asl][:, c, :], in0=sg[pp][:],
                                                                                      in1=pu[pp][:], op=ALU.mult),
                                 reads=[bsg[pp], bpu[pp]], writes=[bact[asl]])
                        for f in pend:
                            f()
                        pend = []

                        def down(asl=asl, sl=sl, sbk=sbk, ncp=ncp, ei=ei):
                            for n4 in range(4):
                                n = sbk * 4 + n4
                                for hf in range(2):
                                    dp = (n4 * 2 + hf) % 2
                                    for c in range(ncp):
                                        P.op("pe", lambda e, c=c, dp=dp, n4=n4, hf=hf: e.matmul(
                                            pd[dp][:], lhsT=act[asl][:, c, n4 * 128:(n4 + 1) * 128],
                                            rhs=wd[sl][:, c, hf * 512:(hf + 1) * 512], start=(c == 0), stop=(c == ncp - 1)),
                                            reads=[bact[asl], bwd[sl]], writes=[bpd[dp]])
                                    if moe:
                                        P.op("dve", lambda e, dp=dp, n=n, hf=hf: e.scalar_tensor_tensor(
                                            out=xacc[:, n, hf * 512:(hf + 1) * 512], in0=pd[dp][:], scalar=gate[:, n, ei:ei + 1],
                                            in1=xacc[:, n, hf * 512:(hf + 1) * 512], op0=ALU.mult, op1=ALU.add),
                                            reads=[bpd[dp], bgate[n], bx[n]], writes=[bx[n]])
                                    else:
                                        P.op("dve", lambda e, dp=dp, n=n, hf=hf: e.tensor_tensor(
                                            out=xacc[:, n, hf * 512:(hf + 1) * 512], in0=pd[dp][:],
                                            in1=xacc[:, n, hf * 512:(hf + 1) * 512], op=ALU.add),
                                            reads=[bpd[dp], bx[n]], writes=[bx[n]])
                        pend.append(down)
                    first_piece = False
            for f in pend:
                f()
            pend = []
            for n in range(NT):
                if final_g is not None:
                    s2 = n % 2
                    P.op("act", lambda e, n=n: e.activation(out=junk[:], in_=xacc[:, n, :], func=AF.Square,
                                                            accum_out=ss[:, n:n + 1]),
                         reads=[bx[n]], writes=[bjunk, bss[n]])
                    rsqrt_op(P, rs[:, n:n + 1], ss[:, n:n + 1], 1.0 / D, bss[n], brs[n])
                    P.op("dve", lambda e, n=n: e.scalar_tensor_tensor(out=xacc[:, n, :], in0=xacc[:, n, :], scalar=rs[:, n:n + 1],
                                                                      in1=fg[:], op0=ALU.mult, op1=ALU.mult),
                         reads=[bx[n], brs[n], bfg], writes=[bx[n]])
            for n0 in range(0, NT, 4):
                P.dma(x_dst.ap[r0 + n0 * 128: r0 + (n0 + 4) * 128, :].rearrange("(n p) d -> p n d", p=128),
                      xacc[:, n0:n0 + 4, :], reads=bx[n0:n0 + 4], writes=[x_dst.b[(r0 + n0 * 128) // 512]])


class KCtx:
    pass


class DBuf:
    def __init__(self, ap, T):
        self.ap = ap
        self.T = T
        self.b = bufs(max(1, T // 512))


def build_program(T, do_mixer=(True, True), do_ffn=(True, True), do_final=True, en=(1, 1, 1, 1), split=False):
    nc = bass.Bass("TRN2", target_bir_lowering=False)
    K = KCtx()
    K.nc, K.T = nc, T
    dr = {}

    def din(name, shape, dt=F32):
        dr[name] = nc.dram_tensor(name, list(shape), dt, kind="ExternalInput").ap()
        return dr[name]
    x = DBuf(din("x", [T, D]), T)
    TO = T // 2 if split else T
    out = DBuf(nc.dram_tensor("out", [TO, D], F32, kind="ExternalOutput").ap(), TO)
    xh = DBuf(nc.dram_tensor("xh", [TO, D], F32, kind="Internal").ap(), TO)
    din("wsel", [128, 2])
    xs_ = [DBuf(nc.dram_tensor("xs%d" % i, [T, D], F32, kind="Internal").ap(), T) for i in range(3)]
    yA = DBuf(nc.dram_tensor("yA", [128, 6, T], BF16, kind="Internal").ap(), T)
    hTd = DBuf(nc.dram_tensor("hTd", [128, KD, T], BF16, kind="Internal").ap(), T)
    din("consts", [128, NCONST])
    din("normg", [128, 4 * KD])
    din("final_norm", [1, D])
    din("w_in", [2, D, IN_COLS]); din("w_out", [2, D, D])
    din("lpa", [2, 128, NLPA]); din("lps", [2, 128, NLPS])
    din("ffn_w_gate", [D, D_FF]); din("ffn_w_up", [D, D_FF]); din("ffn_w_down", [D_FF, D])
    din("moe_router", [D, N_EXP])
    din("moe_w_gate", [N_EXP, D, D_FFE]); din("moe_w_up", [N_EXP, D, D_FFE]); din("moe_w_down", [N_EXP, D_FFE, D])
    with ExitStack() as es:
        P = Prog(nc, es)
        K.P = P
        K.consts = P.sb([128, NCONST]); K.bconst = Buf()
        lo, hi = CONST_COLS["ident"]
        K.ident32 = K.consts[:, lo:hi]
        normg = P.sb([128, 4 * KD])
        wsel = P.sb([128, 2])
        P.epst = P.sb([128, 1])
        beps = Buf()
        P.op("dve", lambda e: e.memset(P.epst[:], EPS), writes=[beps])
        K.ones_bf = P.sb([128, 128], BF16)
        P.op("dve", lambda e: e.memset(K.ones_bf[:], 1.0), writes=[beps])
        K.ident_bf = P.sb([128, 128], BF16)
        P.const_ap(1.0); P.const_ap(64 * EPS)
        P.dma(K.consts[:], dr["consts"], writes=[K.bconst])
        bng = Buf()
        P.dma(normg[:], dr["normg"], writes=[bng])
        P.dma(wsel[:], dr["wsel"], writes=[K.bconst])
        P.op("dve", lambda e: e.tensor_copy(out=K.ident_bf[:], in_=K.ident32), reads=[K.bconst], writes=[beps])
        for e_ in ("dve", "pe", "act", "pool"):
            P.wait_all(e_, [bng, K.bconst, beps])
        cur = x
        free = list(xs_)
        st = NS()
        st.s5_init = st.sc_init = st.gdn_s_init = st.gdn_raw_init = None
        st.s5_out = st.sc_out = st.gdn_s_out = st.gdn_raw_out = None
        for l in range(2):
            if do_mixer[l]:
                with P.scope():
                    lpa = P.sb([128, NLPA]); K.blpa = Buf()
                    P.dma(lpa[:], dr["lpa"][l], writes=[K.blpa])
                    with P.scope():
                        S5 = NS()
                        S5.Wt = P.sb([128, 2, 8, 2, 128], BF16)
                        S5.QT = P.sb([128, 8, 8, 2, 64], BF16)
                        S5.BDT = P.sb([128, 2, 8, 128], BF16)
                        S5.Hr = P.sb([128, 7, 8]); S5.Hi = P.sb([128, 7, 8])
                        S5.b = Buf()
                        if en[0]:
                            with P.scope():
                                lps = P.sb([128, NLPS]); K.blps = Buf()
                                P.dma(lps[:], dr["lps"][l], writes=[K.blps])
                                s5_tables(K, lps, S5)
                        mixer_pass_a(K, l, cur, yA, hTd, normg[:, (2 * l) * KD:(2 * l + 1) * KD], dr["w_in"][l], lpa, S5, st, en=en[:3])
                    sp_ = split and l == 1
                    dst = xh if sp_ else free.pop(0)
                    mixer_pass_b(K, l, cur, dst, yA, hTd, normg[:, (2 * l) * KD:(2 * l + 1) * KD], dr["w_in"][l], dr["w_out"][l], lpa, st,
                                 en_gdn=bool(en[3]), wsel=wsel if sp_ else None)
                    if cur is not x and cur is not xh:
                        free.append(cur)
                    cur = dst
            if do_ffn[l]:
                last = (l == 1) or not (do_ffn[1] or do_mixer[1])
                dst = out if last else free.pop(0)
                if l == 0:
                    experts = [dict(wg=dr["ffn_w_gate"], wu=dr["ffn_w_up"], wd=dr["ffn_w_down"], ff=D_FF)]
                    router = None
                else:
                    experts = [dict(wg=dr["moe_w_gate"][e], wu=dr["moe_w_up"][e], wd=dr["moe_w_down"][e], ff=D_FFE)
                               for e in range(N_EXP)]
                    router = dr["moe_router"]
                ffn_stage(K, cur, dst, normg[:, (2 * l + 1) * KD:(2 * l + 2) * KD], experts, router,
                          dr["final_norm"] if (last and do_final) else None)
                if cur is not x:
                    free.append(cur)
                cur = dst
        K.final = cur
        if cur is not out:
            with P.scope():
                t = P.sb([128, 4, D]); bt = Buf()
                for blk in range(TO // 512):
                    P.dma(t[:], cur.ap[blk * 512:(blk + 1) * 512, :].rearrange("(n p) d -> p n d", p=128), reads=[cur.b[blk]], writes=[bt])
                    P.dma(out.ap[blk * 512:(blk + 1) * 512, :].rearrange("(n p) d -> p n d", p=128), t[:], reads=[bt], writes=[out.b[blk]])
        P.wait_all("sp", out.b)
    K.ninst = P.ninst
    return nc, K


def mm(P, out, lhsT, rhs, start, stop, rd, wr):
    P.op("pe", lambda e: e.matmul(out, lhsT=lhsT, rhs=rhs, start=start, stop=stop), rd, wr)


def tt(P, eng, out, in0, in1, op, rd, wr):
    P.op(eng, lambda e: e.tensor_tensor(out=out, in0=in0, in1=in1, op=op), rd, wr)


def ts(P, eng, out, in0, s1, s2, op0, op1, rd, wr):
    if s2 is None:
        P.op(eng, lambda e: e.tensor_scalar(out=out, in0=in0, scalar1=s1, scalar2=None, op0=op0), rd, wr)
    else:
        P.op(eng, lambda e: e.tensor_scalar(out=out, in0=in0, scalar1=s1, scalar2=s2, op0=op0, op1=op1), rd, wr)


def stt(P, eng, out, in0, scalar, in1, op0, op1, rd, wr):
    P.op(eng, lambda e: e.scalar_tensor_tensor(out=out, in0=in0, scalar=scalar, in1=in1, op0=op0, op1=op1), rd, wr)


def actf(P, out, in_, func, rd, wr, **kw):
    P.op("act", lambda e: e.activation(out=out, in_=in_, func=func, **kw), rd, wr)


def cp(P, eng, out, in_, rd, wr):
    if eng == "act":
        P.op(eng, lambda e: e.activation(out=out, in_=in_, func=AF.Copy), rd, wr)
    else:
        P.op(eng, lambda e: e.tensor_copy(out=out, in_=in_), rd, wr)


MUL, ADD, SUB = ALU.mult, ALU.add, ALU.subtract
MAGIC = 12582912.0
TWO_PI = 2.0 * np.pi

CONST_COLS = {}


def _const_layout():
    o = 0
    for name, n in [("ident", 128), ("triu", 128), ("U1", 128), ("U2", 128), ("maskS", 128), ("maskI", 128),
                    ("onesblk", 128), ("chunk0", 128), ("chunk1", 128), ("headsel", 2), ("mq", 4), ("mask8", 8), ("par", 2), ("pairm", 4)]:
        CONST_COLS[name] = (o, o + n)
        o += n
    return o


NCONST = _const_layout()


def make_consts():
    p = np.arange(128)
    ch = p // 64
    same = (ch[:, None] == ch[None, :])
    c = np.zeros((128, NCONST), np.float32)

    def put(name, a):
        lo, hi = CONST_COLS[name]
        c[:, lo:hi] = a
    put("ident", np.eye(128))
    put("triu", (p[None, :] >= p[:, None]))
    put("U1", same & (p[:, None] <= p[None, :]))
    put("U2", same & (p[:, None] > p[None, :]))
    put("maskS", same & (p[:, None] < p[None, :]))
    put("maskI", same & (p[:, None] <= p[None, :]))
    put("onesblk", same)
    put("chunk0", np.repeat((p < 64)[:, None], 128, 1))
    put("chunk1", np.repeat((p >= 64)[:, None], 128, 1))
    put("headsel", np.stack([p < 64, p >= 64], 1))
    put("mq", np.stack([(p // 16) % 4 == q for q in range(4)], 1))
    put("mask8", np.stack([(p // 16) == g for g in range(8)], 1))
    put("par", np.stack([(p // 16) % 2 == q for q in range(2)], 1))
    put("pairm", np.stack([(p // 32) == q for q in range(4)], 1))
    return c


LPA = {}
LPS = {}


def _lp_layout():
    o = 0
    for name, n in [("s5_d", 2), ("s5_bglu", 2), ("s5_on", 2), ("sc_on", 2), ("sc_conv", 6), ("gdn_conv", 24),
                    ("wglu", 512), ("ln_g", 256), ("ln_b", 256), ("gmn", 256), ("gdnn", 256), ("a_log", 4), ("dtb", 4),
                    ("bsp", 4), ("wsp", 512)]:
        LPA[name] = (o, o + n)
        o += n
    na = o
    o = 0
    for name, n in [("LRp", 8), ("LIp", 8), ("STp", 8), ("CRp", 128), ("CIp", 128), ("LRu", 128), ("LIu", 128), ("STu", 2),
                    ("BRu", 128), ("BIu", 128), ("CRu", 2048), ("CIu", 2048)]:
        LPS[name] = (o, o + n)
        o += n
    return na, o


NLPA, NLPS = _lp_layout()


def pack_layer_params(inp, l):
    a = np.zeros((128, NLPA), np.float32)
    s = np.zeros((128, NLPS), np.float32)

    def pa(name, v):
        lo, hi = LPA[name]
        a[:, lo:hi] = np.asarray(v, np.float32).reshape(128, hi - lo)

    def ps_(name, v):
        lo, hi = LPS[name]
        s[:, lo:hi] = np.asarray(v, np.float32).reshape(128, hi - lo)

    def fm(v, nt):
        return np.asarray(v).reshape(nt, 128).T
    pa("s5_d", fm(inp["s5_d"][l], 2)); pa("s5_bglu", fm(inp["s5_b_glu"][l], 2)); pa("s5_on", fm(inp["s5_out_norm"][l], 2))
    pa("sc_on", fm(inp["sc_out_norm"][l], 2))
    pa("sc_conv", inp["sc_conv"][l].reshape(3, 2, 128).transpose(2, 1, 0))
    pa("gdn_conv", inp["gdn_conv"][l].reshape(4, 6, 128).transpose(2, 1, 0))
    pa("wglu", inp["s5_w_glu"][l].reshape(2, 128, 256).transpose(1, 0, 2))
    rep = lambda v: np.broadcast_to(np.asarray(v)[None, :], (128, len(v)))
    pa("ln_g", rep(inp["sgu_ln_g"][l])); pa("ln_b", rep(inp["sgu_ln_b"][l])); pa("gmn", rep(inp["gmlp_out_norm"][l]))
    pa("gdnn", rep(np.tile(inp["gdn_norm"][l], 4))); pa("a_log", rep(inp["gdn_a_log"][l])); pa("dtb", rep(inp["gdn_dt_bias"][l]))
    pa("bsp", inp["sgu_b"][l].T)
    pa("wsp", inp["sgu_w"][l].transpose(2, 0, 1))
    lam_re, lam_im, st = inp["s5_lam_re"][l], inp["s5_lam_im"][l], inp["s5_log_step"][l]
    pm = lambda v: v.reshape(8, 2, 64).transpose(1, 2, 0).reshape(128, 8)
    ps_("LRp", pm(lam_re)); ps_("LIp", pm(lam_im)); ps_("STp", pm(np.repeat(st[:, None], 64, 1)))
    cpm = lambda c: c.reshape(8, 2, 16, 64).transpose(1, 3, 0, 2).reshape(128, 128)
    ps_("CRp", cpm(inp["s5_c_re"][l])); ps_("CIp", cpm(inp["s5_c_im"][l]))
    um = lambda v: np.repeat(v.reshape(2, 8, 1, 64), 16, 2).transpose(1, 2, 0, 3).reshape(128, 128)
    ps_("LRu", um(lam_re)); ps_("LIu", um(lam_im))
    ps_("STu", np.repeat(st.reshape(2, 8, 1), 16, 2).transpose(1, 2, 0).reshape(128, 2))
    bum = lambda b: b.reshape(2, 8, 64, 16).transpose(1, 3, 0, 2).reshape(128, 128)
    ps_("BRu", bum(inp["s5_b_re"][l])); ps_("BIu", bum(inp["s5_b_im"][l]))
    cum = lambda c: np.repeat(c.reshape(2, 8, 1, 16, 64), 16, 2).transpose(1, 2, 0, 3, 4).reshape(128, 2048)
    ps_("CRu", cum(inp["s5_c_re"][l])); ps_("CIu", cum(inp["s5_c_im"][l]))
    return a, s


def cview(K, name):
    lo, hi = CONST_COLS[name]
    return K.consts[:, lo:hi]


def s5_tables(K, lps, S5):
    P = K.P
    B = S5.b
    R = [B]

    def L(name, shape=None):
        lo, hi = LPS[name]
        v = lps[:, lo:hi]
        return v

    def cexp(dst_r, dst_i, lrdt, lidt, t0, t1, shape_n):
        actf(P, t0, lrdt, AF.Exp, R, R)
        ts(P, "dve", t1, lidt, 1.0 / TWO_PI, MAGIC, MUL, ADD, R, R)
        ts(P, "dve", t1, t1, MAGIC, -TWO_PI, SUB, MUL, R, R)
        tt(P, "dve", t1, t1, lidt, ADD, R, R)
        actf(P, dst_i, t1, AF.Sin, R, R)
        ts(P, "dve", dst_r, lidt, np.pi / 2, None, ADD, None, R, R)
        ts(P, "dve", t1, dst_r, 1.0 / TWO_PI, MAGIC, MUL, ADD, R, R)
        ts(P, "dve", t1, t1, MAGIC, -TWO_PI, SUB, MUL, R, R)
        tt(P, "dve", t1, t1, dst_r, ADD, R, R)
        actf(P, dst_r, t1, AF.Sin, R, R)
        tt(P, "dve", dst_r, dst_r, t0, MUL, R, R)
        tt(P, "dve", dst_i, dst_i, t0, MUL, R, R)

    def cmul(or_, oi, ar, ai, br, bi, t0):
        tt(P, "dve", t0, ai, bi, MUL, R, R)
        tt(P, "dve", or_, ar, br, MUL, R, R)
        tt(P, "dve", or_, or_, t0, SUB, R, R)
        tt(P, "dve", t0, ai, br, MUL, R, R)
        tt(P, "dve", oi, ar, bi, MUL, R, R)
        tt(P, "dve", oi, oi, t0, ADD, R, R)

    with P.scope():
        B.lw = K.blps.lw
        T = [P.sb([128, 128]) for _ in range(10)]
        dtu = P.sb([128, 2])
        Xr = P.sb([128, 8, 128]); Xi = P.sb([128, 8, 128])
        big0 = P.sb([128, 1024]); big1 = P.sb([128, 1024]); val = P.sb([128, 16])
        v3 = lambda t: t[:].rearrange("p (c n) -> p c n", c=2)
        actf(P, dtu[:], L("STu"), AF.Exp, R, R)
        dtb = bcast(dtu[:].unsqueeze(2), [128, 2, 64])
        lrdt, lidt, ar, ai, t0, t1 = T[0], T[1], T[2], T[3], T[4], T[5]
        tt(P, "dve", v3(lrdt), L("LRu").rearrange("p (c n) -> p c n", c=2), dtb, MUL, R, R)
        tt(P, "dve", v3(lidt), L("LIu").rearrange("p (c n) -> p c n", c=2), dtb, MUL, R, R)
        cexp(ar[:], ai[:], lrdt[:], lidt[:], t0[:], t1[:], 128)
        den, am1, fr, fi = T[6], T[7], T[8], T[9]
        tt(P, "dve", den[:], L("LRu"), L("LRu"), MUL, R, R)
        tt(P, "dve", t0[:], L("LIu"), L("LIu"), MUL, R, R)
        tt(P, "dve", den[:], den[:], t0[:], ADD, R, R)
        P.op("dve", lambda e: e.reciprocal(out=den[:], in_=den[:]), R, R)
        ts(P, "dve", am1[:], ar[:], -1.0, None, ADD, None, R, R)
        tt(P, "dve", fr[:], am1[:], L("LRu"), MUL, R, R)
        tt(P, "dve", t0[:], ai[:], L("LIu"), MUL, R, R)
        tt(P, "dve", fr[:], fr[:], t0[:], ADD, R, R)
        tt(P, "dve", fr[:], fr[:], den[:], MUL, R, R)
        tt(P, "dve", fi[:], ai[:], L("LRu"), MUL, R, R)
        tt(P, "dve", t0[:], am1[:], L("LIu"), MUL, R, R)
        tt(P, "dve", fi[:], fi[:], t0[:], SUB, R, R)
        tt(P, "dve", fi[:], fi[:], den[:], MUL, R, R)
        cmul(Xr[:, 0, :], Xi[:, 0, :], fr[:], fi[:], L("BRu"), L("BIu"), t0[:])
        for k in range(7):
            cmul(Xr[:, k + 1, :], Xi[:, k + 1, :], Xr[:, k, :], Xi[:, k, :], ar[:], ai[:], t0[:])
        par = cview(K, "par")
        for jp in range(8):
            for part, X in enumerate((Xr, Xi)):
                src = X[:, 7 - jp, :].rearrange("p (c n) -> p c n", c=2)
                for half in range(2):
                    ts(P, "dve", S5.Wt[:, :, jp, part, half * 64:(half + 1) * 64], src, par[:, half:half + 1], None, MUL, None,
                       R + [K.bconst], [S5.b])
        m8 = cview(K, "mask8")
        for k in range(8):
            for ct in range(2):
                xr = bcast(Xr[:, k, ct * 64:(ct + 1) * 64].unsqueeze(1), [128, 16, 64])
                xi = bcast(Xi[:, k, ct * 64:(ct + 1) * 64].unsqueeze(1), [128, 16, 64])
                lo = LPS["CRu"][0] + ct * 1024
                cr = lps[:, lo:lo + 1024].rearrange("p (q n) -> p q n", q=16)
                lo = LPS["CIu"][0] + ct * 1024
                ci = lps[:, lo:lo + 1024].rearrange("p (q n) -> p q n", q=16)
                b0 = big0[:].rearrange("p (q n) -> p q n", q=16)
                b1 = big1[:].rearrange("p (q n) -> p q n", q=16)
                tt(P, "dve", b0, cr, xr, MUL, R, R)
                tt(P, "dve", b1, ci, xi, MUL, R, R)
                tt(P, "dve", b0, b0, b1, SUB, R, R)
                P.op("dve", lambda e, b0=b0: e.tensor_reduce(out=val[:], in_=b0, axis=AX.X, op=ADD), R, R)
                tt(P, "dve", S5.BDT[:, ct, k, :].rearrange("p (g q) -> p g q", g=8), bcast(val[:].unsqueeze(1), [128, 8, 16]),
                   bcast(m8.unsqueeze(2), [128, 8, 16]), MUL, R + [K.bconst], [S5.b])
        Q = [P.sb([128, 8]) for _ in range(6)]
        Pr = P.sb([128, 9, 8]); Pi = P.sb([128, 9, 8])
        dtp, lrp, lip, t0, t1 = Q[0], Q[1], Q[2], Q[3], Q[4]
        actf(P, dtp[:], L("STp"), AF.Exp, R, R)
        tt(P, "dve", lrp[:], L("LRp"), dtp[:], MUL, R, R)
        tt(P, "dve", lip[:], L("LIp"), dtp[:], MUL, R, R)
        cexp(Pr[:, 1, :], Pi[:, 1, :], lrp[:], lip[:], t0[:], t1[:], 8)
        for k in range(1, 8):
            cmul(Pr[:, k + 1, :], Pi[:, k + 1, :], Pr[:, k, :], Pi[:, k, :], Pr[:, 1, :], Pi[:, 1, :], t0[:])
        cp(P, "dve", S5.Hr[:, 0, :], Pr[:, 8, :], R, [S5.b])
        cp(P, "dve", S5.Hi[:, 0, :], Pi[:, 8, :], R, [S5.b])
        for s in range(6):
            cmul(S5.Hr[:, s + 1, :], S5.Hi[:, s + 1, :], S5.Hr[:, s, :], S5.Hi[:, s, :], S5.Hr[:, s, :], S5.Hi[:, s, :], t0[:])
        P.op("dve", lambda e: e.memset(S5.QT[:], 0.0), R, [S5.b])
        c0 = P.sb([128, 8, 16]); c1 = P.sb([128, 8, 16])
        CR = L("CRp").rearrange("p (r q) -> p r q", r=8)
        CI = L("CIp").rearrange("p (r q) -> p r q", r=8)
        for j in range(8):
            pr_b = bcast(Pr[:, j + 1, :].unsqueeze(2), [128, 8, 16])
            pi_b = bcast(Pi[:, j + 1, :].unsqueeze(2), [128, 8, 16])
            for part in range(2):
                if part == 0:
                    tt(P, "dve", c0[:], CR, pr_b, MUL, R, R)
                    tt(P, "dve", c1[:], CI, pi_b, MUL, R, R)
                    tt(P, "dve", c0[:], c0[:], c1[:], SUB, R, R)
                else:
                    tt(P, "dve", c0[:], CR, pi_b, MUL, R, R)
                    tt(P, "dve", c1[:], CI, pr_b, MUL, R, R)
                    stt(P, "dve", c0[:], c0[:], -1.0, c1[:], MUL, SUB, R, R)
                for gl in range(2):
                    for lp in range(2):
                        cp(P, "dve", S5.QT[gl * 64:(gl + 1) * 64, lp::2, j, part, 32 * lp + 16 * gl:32 * lp + 16 * gl + 16],
                           c0[gl * 64:(gl + 1) * 64, lp::2, :], R, [S5.b])


class NS:
    pass


def load_x_block_hT(K, x_src, blk, xr, bxr, xs2, bxs2, hT, bhT, ptrs, bptrs, normg, rs, brs, ss, bss, junk, bjunk):
    P = K.P
    r0 = blk * 512
    for n in range(4):
        i = n % 2
        xs, bxs = xs2[i], bxs2[i]
        ptr, bptr = ptrs[i], bptrs[i]
        P.dma(xr[i][:], x_src.ap[r0 + n * 128:r0 + (n + 1) * 128, :], reads=[x_src.b[blk]], writes=[bxr[i]])
        actf(P, junk[:], xr[i][:], AF.Square, [bxr[i]], [bjunk, bss[i]], accum_out=ss[:, n:n + 1])
        rsqrt_op(P, rs[:, n:n + 1], ss[:, n:n + 1], 1.0 / D, bss[i], brs[i])
        actf(P, xs[:], xr[i][:], AF.Copy, [bxr[i], brs[i]], [bxs], scale=rs[:, n:n + 1])
        ptb = ptr.bitcast(BF16)
        for k in range(KD):
            P.op("pe", lambda e, k=k, xs=xs, ptb=ptb: e.transpose(out=ptb[:, k * 128:(k + 1) * 128], in_=xs[:, k * 128:(k + 1) * 128],
                                                                  identity=K.ident_bf[:]), [bxs, K.bconst], bptr)
        tt(P, "dve", hT[:, :, n * 128:(n + 1) * 128], ptb[:, 0:1024].rearrange("p (k t) -> p k t", k=KD),
           bcast(normg.unsqueeze(2), [128, KD, 128]), MUL, bptr + [K.bconst], [bhT])


def fm_norm(K, y, by, gain, out_fn, W, bout, perm=False):
    P = K.P
    actf(P, W.sq[:], y[:], AF.Square, [by], [W.bsq])
    for ct in range(2):
        mm(P, W.pn[:, 0:512], K.ones_bf[:], W.sq[:, ct, :], ct == 0, ct == 1, [W.bsq, K.bconst], [W.bpn])
    actf(P, W.rstd[:], W.pn[:, 0:512], AF.Sqrt, [W.bpn], [W.brstd], bias=P.epst[:, 0:1], scale=1.0 / 256)
    P.op("dve", lambda e: e.reciprocal(out=W.rstd[:], in_=W.rstd[:]), [W.brstd], [W.brstd])
    for ct in range(2):
        if perm:
            yi = y[:, ct, :].rearrange("p (j c) -> p j c", j=8)
            ri = W.rstd[:].rearrange("p (j c) -> p j c", j=8)
        else:
            yi, ri = y[:, ct, :], W.rstd[:]
        stt(P, "dve", out_fn(ct), yi, gain[:, ct:ct + 1], ri, MUL, MUL, [by, W.brstd], [bout])


def mixer_pass_a(K, l, x_src, yA, hTd, normg, w_in, lpa, S5, st, en=(1, 1, 1)):
    P, T = K.P, K.T
    NB = T // 512
    PAD = 64
    with P.scope():
        wA = P.sb([128, KD, 1536], BF16); bwA = Buf(); bwA2 = Buf()
        P.dma(wA[:, :, 0:768], w_in[:, 0:768].rearrange("(k p) c -> p k c", p=128), writes=[bwA], q="pool")
        P.dma(wA[:, :, 768:1536], w_in[:, 1800:2568].rearrange("(k p) c -> p k c", p=128), writes=[bwA2], q="pool")
        bW = [bwA, bwA2]
        xr = [P.sb([128, D]) for _ in range(2)]; bxr = bufs(2)
        xs2 = [P.sb([128, D], BF16) for _ in range(2)]; bxs2 = bufs(2)
        hT = P.sb([128, KD, 512], BF16); bhT = Buf()
        junk = P.sb([128, D], BF16); bjunk = Buf()
        ss = P.sb([128, 4]); bss = bufs(2); rs = P.sb([128, 4]); brs = bufs(2)
        ptr = P.ps([128, 1024]); bptr = Buf(True); bptrB = Buf(True)
        pp = [P.ps([128, 512]) for _ in range(2)]; bpp = [Buf(True), Buf(True)]
        pg = [P.ps([128, 512]) for _ in range(2)]; bpg = [Buf(True), Buf(True)]
        pv = P.ps([128, 1024]); bpv = Buf(True)
        W = NS()
        W.sq = P.sb([128, 2, 512], BF16); W.bsq = Buf()
        W.rstd = P.sb([128, 512]); W.brstd = Buf()
        W.pn = pg[1]; W.bpn = bpg[1]
        W2 = NS()
        W2.sq = P.sb([128, 2, 512], BF16); W2.bsq = Buf()
        W2.rstd = P.sb([128, 512]); W2.brstd = Buf()
        W2.pn = pv[:, 512:1024]; W2.bpn = bpv
        ya_sc = P.sb([128, 2, 512]); bya_sc = Buf()
        junk_placeholder = None
        yst = P.sb([128, 6, 512], BF16); byst = bufs(3)
        ya = P.sb([128, 2, 512]); bya = Buf()
        yb = P.sb([128, 2, 512]); byb = Buf()
        P.op("dve", lambda e: e.memset(yst[:], 0.0), [], byst)
        uT = P.sb([128, 2, 512], BF16); buT = Buf()
        uTm = P.sb([128, 4, 2, 512], BF16); buTm = Buf()
        SA = P.sb([128, 8, 2, PAD + 65]); SB = P.sb([128, 8, 2, PAD + 65]); bSA = bufs(2); bSB = bufs(2)
        Sb16 = P.sb([128, 8, 2, 64], BF16); bS16 = Buf()
        hsT = [P.sb([128, 8, 65]) for _ in range(4)]; bhs = bufs(4); bdr, bdi = Buf(), Buf()
        yg = P.sb([128, 2, 512], BF16); byg = Buf()
        sig = P.sb([128, 512]); bsig = Buf()
        wglu = P.sb([128, 2, 256], BF16); bwglu = Buf()
        cp(P, "dve", wglu[:], lpa[:, LPA["wglu"][0]:LPA["wglu"][1]].rearrange("p (k c) -> p k c", k=2), [K.blpa], [bwglu])
        P.op("dve", lambda e: e.memset(SA[:], 0.0), [], bSA)
        P.op("dve", lambda e: e.memset(SB[:], 0.0), [], bSB)
        if st.s5_init is not None:
            P.dma(SA[:, :, :, PAD:PAD + 1], st.s5_init.rearrange("p (r c o) -> p r c o", r=8, c=2), reads=[], writes=bSA)
        Bsb = P.sb([128, 2, 512]); bB = Buf()
        Csb = P.sb([128, 2, 512]); bC = Buf()
        z = P.sb([128, 2, 2 + 512], BF16); bz = Buf()
        dsc = P.sb([128, 2, 3, 128], BF16); bdsc = Buf()
        lo = LPA["sc_conv"][0]
        for ct in range(2):
            for j in range(3):
                ts(P, "dve", dsc[:, ct, j, :], K.ident32[:], lpa[:, lo + ct * 3 + j:lo + ct * 3 + j + 1], None, MUL, None,
                   [K.blpa, K.bconst], [bdsc])
        P.op("dve", lambda e: e.memset(z[:], 0.0), [], [bz])
        if st.sc_init is not None:
            P.dma(z[:, :, 0:2], st.sc_init.rearrange("p (c o) -> p c o", c=2), reads=[], writes=[bz], q="pool")
        wsp = P.sb([128, 4, 128], BF16); bwsp = Buf()
        lo = LPA["wsp"][0]
        tt(P, "dve", wsp[:], lpa[:, lo:lo + 512].rearrange("p (h t) -> p h t", h=4),
           bcast(cview(K, "triu").unsqueeze(1), [128, 4, 128]), MUL, [K.blpa, K.bconst], [bwsp])
        def gm_set(pg_, bpg_):
            return (P.sb([128, 512]), Buf(), P.sb([128, 256]), Buf(), P.sb([128, 256], BF16), Buf(), P.sb([128, 256]), Buf(),
                    P.sb([128, 6]), P.sb([128, 2]), Buf(), P.sb([128, 2]), Buf(), P.sb([128, 256], BF16), Buf(), pg_, bpg_)
        gmA = gm_set(pg, bpg)
        gmB = gm_set(pp, bpp)

        def LA(name):
            lo, hi = LPA[name]
            return lpa[:, lo:hi]

        for blk in range(NB):
            load_x_block_hT(K, x_src, blk, xr, bxr, xs2, bxs2, hT, bhT, [ptr[:], pv[:]], [[bptr, bptrB], [bpv]], normg, rs, brs, ss, bss,
                            junk, bjunk)
            P.dma(hTd.ap[:, :, blk * 512:(blk + 1) * 512], hT[:], reads=[bhT], writes=[hTd.b[blk]])

            def proj_fm(col0, ps_ap, pbuf):
                wi, c = (0, col0) if col0 < 768 else (1, col0 - 1800 + 768)
                for k in range(KD):
                    mm(P, ps_ap, wA[:, k, c:c + 128], hT[:, k, :], k == 0, k == KD - 1, [bW[wi], bhT], [pbuf])
            def g_sc():
                if not en[2]:
                    return
                cp(P, "pool", z[:, :, 0:2], z[:, :, 512:514], [bz], [bz])
                for ct in range(2):
                    proj_fm(1800 + ct * 128, pp[0][:], bpp[0])
                    yield
                    cp(P, "act", Bsb[:, ct, :], pp[0][:], [bpp[0]], [bB])
                    proj_fm(2056 + ct * 128, pp[1][:], bpp[1])
                    yield
                    cp(P, "act", Csb[:, ct, :], pp[1][:], [bpp[1]], [bC])
                    proj_fm(2312 + ct * 128, pp[0][:], bpp[0])
                    yield
                    tt(P, "dve", z[:, ct, 2:514], Csb[:, ct, :], pp[0][:], MUL, [bC, bpp[0]], [bz])
                    yield
                for ct in range(2):
                    for j in range(3):
                        mm(P, pg[0][:], dsc[:, ct, j, :], z[:, ct, j:j + 512], j == 0, j == 2, [bdsc, bz], [bpg[0]])
                    yield
                    tt(P, "dve", ya_sc[:, ct, :], pg[0][:], Bsb[:, ct, :], MUL, [bpg[0], bB], [bya_sc])
                    yield
                fm_norm(K, ya_sc, bya_sc, LA("sc_on"), lambda ct: yst[:, 4 + ct, :], W, byst[2])
                yield
            def g_gm_tile(n, B_):
                gm, bgm, vn, bvn, vnb, bvnb, og, bog, stt6, mv, bmv, gs, bgs, junk2, bjunk2, pg, bpg = B_
                if True:
                    for k in range(KD):
                        mm(P, pg[0][:], hT[:, k, n * 128:(n + 1) * 128], wA[:, k, 256:768], k == 0, k == KD - 1, [bwA, bhT], [bpg[0]])
                    yield
                    actf(P, gm[:], pg[0][:], AF.Gelu_apprx_tanh, [bpg[0]], [bgm])
                    yield
                    P.op("dve", lambda e: e.bn_stats(out=stt6[:], in_=gm[:, 256:512]), [bgm], [bmv])
                    P.op("dve", lambda e: e.bn_aggr(out=mv[:], in_=stt6[:]), [bmv], [bmv])
                    rsqrt_op(P, mv[:, 1:2], mv[:, 1:2], 1.0, bmv, bmv)
                    ts(P, "dve", vn[:], gm[:, 256:512], mv[:, 0:1], mv[:, 1:2], SUB, MUL, [bgm, bmv], [bvn])
                    tt(P, "dve", vn[:], vn[:], LA("ln_g"), MUL, [bvn, K.blpa], [bvn])
                    tt(P, "dve", vnb[:], vn[:], LA("ln_b"), ADD, [bvn, K.blpa], [bvnb])
                    yield
                    for h in range(4):
                        mm(P, pg[1][:, h * 64:(h + 1) * 64], wsp[:, h, :], vnb[:, h * 64:(h + 1) * 64], True, True, [bwsp, bvnb], [bpg[1]])
                    lo = LPA["bsp"][0]
                    for h in range(4):
                        stt(P, "dve", og[:, h * 64:(h + 1) * 64], pg[1][:, h * 64:(h + 1) * 64], lpa[:, lo + h:lo + h + 1],
                            gm[:, h * 64:(h + 1) * 64], ADD, MUL, [bpg[1], bgm, K.blpa], [bog])
                    yield
                    actf(P, junk2[:], og[:], AF.Square, [bog], [bjunk2, bgs], accum_out=gs[:, 0:1])
                    yield
                    rsqrt_op(P, gs[:, 1:2], gs[:, 0:1], 1.0 / 256, bgs, bgs)
                    stt(P, "dve", og[:], og[:], gs[:, 1:2], LA("gmn"), MUL, MUL, [bog, bgs, K.blpa], [bog])
                    for ct in range(2):
                        P.op("pe", lambda e, ct=ct: e.transpose(out=pg[1][:, 256 + ct * 128:256 + (ct + 1) * 128],
                                                                in_=og[:, ct * 128:(ct + 1) * 128], identity=K.ident32[:]),
                             [bog, K.bconst], [bpg[1]])
                    yield
                    cp(P, "act", yst[:, 2:4, n * 128:(n + 1) * 128], pg[1][:, 256:512].rearrange("p (c t) -> p c t", c=2),
                       [bpg[1]], [byst[1]])
                    yield
            def g_gm():
                if not en[1]:
                    return
                for n0 in (0, 2):
                    live_ = [g_gm_tile(n0, gmA), g_gm_tile(n0 + 1, gmB)]
                    while live_:
                        for g_ in list(live_):
                            try:
                                next(g_)
                                yield
                            except StopIteration:
                                live_.remove(g_)

            DBG = 9

            def g_s5():
                if not en[0]:
                    return
                pvh = [pv[:, 0:512], pv[:, 512:1024]]
                for ct in range(2):
                    proj_fm(ct * 128, pvh[ct], bpv)
                    yield
                    cp(P, "act", uT[:, ct, :], pvh[ct], [bpv], [buT])
                    yield
                pm = cview(K, "pairm")
                for q4 in range(4):
                    actf(P, uTm[:, q4, :, :], uT[:], AF.Copy, [buT, K.bconst], [buTm], scale=pm[:, q4:q4 + 1])
                for pr in range(8):
                    ct, q4 = pr // 4, pr % 4
                    for part in range(2):
                        for jp in range(8):
                            mm(P, pv[:, (pr * 2 + part) * 64:(pr * 2 + part + 1) * 64],
                               S5.Wt[:, ct, jp, part, :], uTm[:, q4, ct, jp::8], jp == 0, jp == 7, [S5.b, buTm], [bpv])
                    if pr % 2 == 1:
                        yield
                cp(P, "dve", SA[:, :, :, PAD + 1:PAD + 65], pv[:].rearrange("p (r c n) -> p r c n", r=8, c=2), [bpv], bSA)
                src, dst, bs, bd = SA, SB, bSA, bSB
                for s in range(7 if DBG >= 2 else 0):
                    d = 1 << s
                    L = 65 - d
                    hr = bcast(S5.Hr[:, s, :].unsqueeze(2), [128, 8, L])
                    hi = bcast(S5.Hi[:, s, :].unsqueeze(2), [128, 8, L])
                    sr, si = src[:, :, 0, PAD + d:PAD + 65], src[:, :, 1, PAD + d:PAD + 65]
                    shr, shi = src[:, :, 0, PAD:PAD + L], src[:, :, 1, PAD:PAD + L]
                    dr_, di_ = dst[:, :, 0, PAD + d:PAD + 65], dst[:, :, 1, PAD + d:PAD + 65]
                    cp(P, "act", dst[:, :, :, PAD:PAD + d], src[:, :, :, PAD:PAD + d], bs, bd)
                    tt(P, "dve", hsT[0][:, :, 0:L], shr, hr, MUL, [bs[0], S5.b], [bhs[0]])
                    tt(P, "dve", hsT[1][:, :, 0:L], shi, hi, MUL, [bs[1], S5.b], [bhs[1]])
                    tt(P, "dve", hsT[2][:, :, 0:L], shi, hr, MUL, [bs[1], S5.b], [bhs[2]])
                    tt(P, "dve", hsT[3][:, :, 0:L], shr, hi, MUL, [bs[0], S5.b], [bhs[3]])
                    tt(P, "dve", dr_, sr, hsT[0][:, :, 0:L], ADD, [bs[0], bhs[0]], [bd[0]])
                    tt(P, "dve", di_, si, hsT[2][:, :, 0:L], ADD, [bs[1], bhs[2]], [bd[1]])
                    tt(P, "dve", dr_, dr_, hsT[1][:, :, 0:L], SUB, [bd[0], bhs[1]], [bd[0]])
                    tt(P, "dve", di_, di_, hsT[3][:, :, 0:L], ADD, [bd[1], bhs[3]], [bd[1]])
                    src, dst, bs, bd = dst, src, bd, bs
                    yield
                cp(P, "act", Sb16[:], src[:, :, :, PAD:PAD + 64], bs, [bS16])
                cp(P, "pool", SA[:, :, :, PAD:PAD + 1], src[:, :, :, PAD + 64:PAD + 65], bs, bSA)
                yield
                for ct in range(2 if DBG >= 3 else 0):
                    pa, pb, bpa, bpb = ptr[:, 0:512], ptr[:, 512:1024], bptr, bptrB
                    for j in range(8):
                        for jp in range(j + 1):
                            mm(P, pa[:, j * 64:(j + 1) * 64], S5.BDT[:, ct, j - jp, :], uT[:, ct, jp::8], jp == 0, jp == j,
                               [S5.b, buT], [bpa])
                        if j % 3 == 2:
                            yield
                    for j in range(8):
                        for hh in range(2):
                            i = 0
                            for lp in range(2):
                                pr = 4 * ct + 2 * hh + lp
                                for part in range(2):
                                    mm(P, pb[hh * 64:(hh + 1) * 64, j * 64:(j + 1) * 64], S5.QT[:, pr, j, part, :],
                                       Sb16[:, pr, part, :], i == 0, i == 3, [S5.b, bS16], [bpb])
                                    i += 1
                        if j % 2 == 1:
                            yield
                    lo = LPA["s5_d"][0]
                    stt(P, "dve", ya[:, ct, :].rearrange("p (j c) -> p j c", j=8), uT[:, ct, :].rearrange("p (c j) -> p j c", j=8),
                        lpa[:, lo + ct:lo + ct + 1], pa.rearrange("p (j c) -> p j c", j=8), MUL, ADD, [buT, bpa, K.blpa], [bya])
                    yield
                    tt(P, "dve", ya[:, ct, :], ya[:, ct, :], pb, ADD, [bya, bpb], [bya])
                    yield
                if DBG < 3:
                    P.op("dve", lambda e: e.memset(ya[:], 0.5), [], [bya])
                actf(P, yg[:], ya[:], AF.Gelu_apprx_tanh, [bya], [byg])
                lo = LPA["s5_bglu"][0]
                for mc in range(2 if DBG >= 4 else 0):
                    for kc in range(2):
                        mm(P, pvh[0], wglu[:, kc, mc * 128:(mc + 1) * 128], yg[:, kc, :], kc == 0, kc == 1, [bwglu, byg], [bpv])
                    yield
                    actf(P, sig[:], pvh[0], AF.Sigmoid, [bpv, K.blpa], [bsig], bias=lpa[:, lo + mc:lo + mc + 1])
                    yield
                    tt(P, "dve", yb[:, mc, :], yg[:, mc, :], sig[:], MUL, [byg, bsig], [byb])
                    yield
                if DBG < 4:
                    P.op("dve", lambda e: e.memset(yb[:], 0.5), [], [byb])
                if DBG >= 5:
                    fm_norm(K, yb, byb, LA("s5_on"), lambda ct: yst[:, ct, :].rearrange("p (c j) -> p j c", j=8), W2, byst[0], perm=True)
                yield

            def seq_(*gs):
                for g in gs:
                    yield from g
            live = [g_s5(), seq_(g_sc(), g_gm())]
            while live:
                for g in list(live):
                    try:
                        next(g)
                    except StopIteration:
                        live.remove(g)
            P.dma(yA.ap[:, :, blk * 512:(blk + 1) * 512], yst[:], reads=byst, writes=[yA.b[blk]])
        if st.s5_out is not None:
            P.dma(st.s5_out.rearrange("p (r c o) -> p r c o", r=8, c=2), SA[:, :, :, PAD:PAD + 1], reads=bSA, writes=[st.bout])
            P.dma(st.sc_out.rearrange("p (c o) -> p c o", c=2), z[:, :, 512:514], reads=[bz], writes=[st.bout2])


def mixer_pass_b(K, l, x_src, x_dst, yA, hTd, normg, w_in, w_out, lpa, st, en_gdn=True, wsel=None):
    P, T = K.P, K.T
    NB = T // 512
    with P.scope():
        wB = P.sb([128, KD, 1032], BF16); bwB = Buf()
        P.dma(wB[:], w_in[:, 768:1800].rearrange("(k p) c -> p k c", p=128), writes=[bwB], q="pool")
        wo = P.sb([128, KD, D], BF16); bwo = Buf()
        P.dma(wo[:], w_out.rearrange("(k p) c -> p k c", p=128), writes=[bwo], q="pool")
        xr = [P.sb([128, D]) for _ in range(2)]; bxr = bufs(2)
        xs = P.sb([128, D]); bxs = Buf()
        hT = P.sb([128, KD, 512], BF16); bhT = Buf()
        junk = P.sb([128, D], BF16); bjunk = Buf()
        ss = P.sb([128, 4]); bss = Buf(); rs = P.sb([128, 4]); brs = Buf()
        ptr = P.ps([128, 1024]); bptr = Buf(True); bptrB = Buf(True)
        pq = P.ps([128, 512]); bpq = Buf(True)
        pX = [P.ps([128, 512]) for _ in range(3)]; bpX = [Buf(True) for _ in range(3)]
        psA = P.ps([128, 512]); psB = P.ps([128, 512]); bpsA, bpsB = Buf(True), Buf(True)
        yAt = P.sb([128, 6, 512], BF16); byA = Buf()
        ygT = P.sb([128, 2, 512], BF16); byg = Buf()
        bdst = bufs(4)

        def LA(name):
            lo, hi = LPA[name]
            return lpa[:, lo:hi]
        if en_gdn:
            rawT = P.sb([128, 6, 515], BF16); braw = Buf()
            dgd = P.sb([128, 6, 4, 128], BF16); bdgd = Buf()
            lo = LPA["gdn_conv"][0]
            for ct in range(6):
                for j in range(4):
                    ts(P, "dve", dgd[:, ct, j, :], K.ident32[:], lpa[:, lo + ct * 4 + j:lo + ct * 4 + j + 1], None, MUL, None,
                       [K.blpa, K.bconst], [bdgd])
            P.op("dve", lambda e: e.memset(rawT[:], 0.0), [], [braw])
            qT = P.sb([128, 2, 512]); kT = P.sb([128, 2, 512]); vT = P.sb([128, 2, 512]); bq, bk, bv = Buf(), Buf(), Buf()
            sq = xs[:].rearrange("p (c t) -> p c t", c=2); bsq = bxs
            rk = P.sb([128, 512]); brk = Buf()
            kTm = P.sb([128, 2, 2, 512], BF16); qTm = P.sb([128, 2, 2, 512], BF16); bkm, bqm = Buf(), Buf()
            kTb = P.sb([128, 2, 512], BF16); qTb = P.sb([128, 2, 512], BF16); bkb, bqb = Buf(), Buf()
            kdm = P.sb([128, 2, 256], BF16); bkdm = Buf()
            ktm = P.sb([128, 4, 256]); vtm = P.sb([128, 4, 256]); bktm, bvtm = Buf(), Buf()
            zs = P.sb([128, 4, 256]); bzs = Buf()
            otm1 = P.sb([128, 256]); botm1 = Buf()
            ab4 = P.sb([128, 4, 8]); bab = Buf()
            sc = NS()
            for nm in ("beta", "nbeta", "g", "eG", "neG", "ekd", "rq", "eGrq"):
                setattr(sc, nm, P.sb([128, 4, 4]))
            sc.gtot = P.sb([128, 4, 2, 4]); bsc = bufs(4)
            negA = P.sb([128, 4]); bnegA = Buf()
            actf(P, negA[:], LA("a_log"), AF.Exp, [K.blpa], [bnegA])
            ts(P, "dve", negA[:], negA[:], -1.0, None, MUL, None, [bnegA], [bnegA])
            tmp4 = P.sb([128, 16]); btmp4 = Buf()
            gU2 = P.sb([128, 512]); bgU2 = Buf()
            expD = P.sb([128, 512]); DTs = P.sb([128, 512]); DTi = P.sb([128, 512]); bD = Buf()
            Ma = P.sb([128, 512], BF16); Mb = P.sb([128, 512], BF16); MTa = P.sb([128, 512], BF16); MTb = P.sb([128, 512], BF16)
            bM = Buf(); bMx = {}
            Z = [P.sb([128, 512], BF16) for _ in range(2)]; bZ = bufs(2)
            QKT = [P.sb([128, 512], BF16) for _ in range(2)]; bQK = bufs(2)
            S = P.sb([128, 2, 64]); bS = bufs(4)
            Sb = P.sb([128, 2, 64], BF16); bSb = bufs(2)
            P.op("dve", lambda e: e.memset(S[:], 0.0), [], bS)
            P.op("dve", lambda e: e.memset(Sb[:], 0.0), [], bSb)
            rp = P.sb([128, 4, 64], BF16); vnew = P.sb([128, 4, 64], BF16); tmpo = P.sb([128, 4, 64]); brp, bvn, bto = bufs(4), bufs(4), bufs(4)
            P.op("dve", lambda e: e.memset(rp[:], 0.0), [], brp)
            P.op("dve", lambda e: e.memset(vnew[:], 0.0), [], bvn)
            ygn = P.sb([128, 256]); bygn = Buf()
            on4 = P.sb([128, 8]); bon4 = Buf()
            c1 = P.const_ap(1.0)
            c64e = P.const_ap(64 * EPS)
            U1, U2 = cview(K, "U1"), cview(K, "U2")
            rep4 = lambda name: bcast(cview(K, name).unsqueeze(1), [128, 4, 128])
            v4 = lambda t: t[:].rearrange("p (h c) -> p h c", h=4)
            if st.gdn_s_init is not None:
                P.dma(S[:], st.gdn_s_init.rearrange("p (a b) -> p a b", a=2), reads=[], writes=bS)
                P.dma(rawT[:, :, 0:3], st.gdn_raw_init.rearrange("p (a b) -> p a b", a=6), reads=[], writes=[braw])
        else:
            P.op("dve", lambda e: e.memset(ygT[:], 0.0), [], [byg])

        def block_gen(blk):
            P.dma(hT[:], hTd.ap[:, :, blk * 512:(blk + 1) * 512], reads=[hTd.b[blk]], writes=[bhT])
            GD = 9
            if en_gdn and GD < 9:
                P.op("dve", lambda e: e.memset(ygT[:], 0.0), [], [byg])
            if en_gdn:
                cp(P, "pool", rawT[:, :, 0:3], rawT[:, :, 512:515], [braw], [braw])
                pqs = [(pq, bpq), (psA, bpsA)]
                for ct in range(6):
                    pq_, bpq_ = pqs[ct % 2]
                    for k in range(KD):
                        mm(P, pq_[:], wB[:, k, ct * 128:(ct + 1) * 128], hT[:, k, :], k == 0, k == KD - 1, [bwB, bhT], [bpq_])
                    cp(P, "act", rawT[:, ct, 3:515], pq_[:], [bpq_], [braw])
                    yield
                for ct in range(6):
                    pq_, bpq_ = pqs[ct % 2]
                    for j in range(4):
                        mm(P, pq_[:], dgd[:, ct, j, :], rawT[:, ct, j:j + 512], j == 0, j == 3, [bdgd, braw], [bpq_])
                    dst, bd = [(qT, bq), (kT, bk), (vT, bv)][ct // 2]
                    actf(P, dst[:, ct % 2, :], pq_[:], AF.Silu, [bpq_], [bd])
                    yield
                actf(P, sq, kT[:], AF.Square, [bk], [bsq])
                for ct in range(2 if GD >= 2 else 0):
                    mm(P, pq[:], cview(K, "onesblk"), sq[:, ct, :], True, True, [bsq, K.bconst], [bpq])
                    actf(P, rk[:], pq[:], AF.Sqrt, [bpq], [brk], bias=P.epst[:, 0:1], scale=1.0)
                    P.op("dve", lambda e: e.reciprocal(out=rk[:], in_=rk[:]), [brk], [brk])
                    tt(P, "dve", kT[:, ct, :], kT[:, ct, :], rk[:], MUL, [bk, brk], [bk])
                    yield
                cp(P, "act", kTb[:], kT[:], [bk], [bkb])
                cp(P, "act", qTb[:], qT[:], [bq], [bqb])
                hsel = cview(K, "headsel")
                for hp in range(2 if GD >= 2 else 0):
                    actf(P, kTm[:, hp, :, :], kT[:], AF.Copy, [bk, K.bconst], [bkm], scale=hsel[:, hp:hp + 1])
                    actf(P, qTm[:, hp, :, :], qT[:], AF.Copy, [bq, K.bconst], [bqm], scale=hsel[:, hp:hp + 1])
                actf(P, sq, qT[:], AF.Square, [bq], [bsq])
                for n in range(4 if GD >= 2 else 0):
                    for ct in range(2):
                        mm(P, pX[0][:, n * 4 + 2 * ct:n * 4 + 2 * ct + 2], sq[:, ct, n * 128:(n + 1) * 128], cview(K, "headsel"),
                           True, True, [bsq, K.bconst], [bpX[0]])
                actf(P, tmp4[:], pX[0][:, 0:16], AF.Sqrt, [bpX[0]], [btmp4], bias=c64e, scale=64.0)
                P.op("dve", lambda e: e.reciprocal(out=sc.rq[:].rearrange("p a b -> p (a b)"), in_=tmp4[:]), [btmp4], bsc)
                for n in range(4):
                    pt_, bpt_ = (ptr[:, 0:512], bptr) if n % 2 == 0 else (ptr[:, 512:1024], bptrB)
                    for ct in range(2):
                        P.op("pe", lambda e, n=n, ct=ct, pt_=pt_: e.transpose(out=pt_[:, ct * 128:(ct + 1) * 128],
                                                                             in_=kT[:, ct, n * 128:(n + 1) * 128], identity=K.ident32[:]),
                             [bk, K.bconst], [bpt_])
                        P.op("pe", lambda e, n=n, ct=ct, pt_=pt_: e.transpose(out=pt_[:, 256 + ct * 128:256 + (ct + 1) * 128],
                                                                             in_=vT[:, ct, n * 128:(n + 1) * 128], identity=K.ident32[:]),
                             [bv, K.bconst], [bpt_])
                    cp(P, "act", ktm[:, n, :], pt_[:, 0:256], [bpt_], [bktm])
                    cp(P, "dve", vtm[:, n, :], pt_[:, 256:512], [bpt_], [bvtm])
                    yield
                for n in range(4):
                    pq_, bpq_ = pqs[n % 2]
                    for k in range(KD):
                        mm(P, pq_[:, 0:264], hT[:, k, n * 128:(n + 1) * 128], wB[:, k, 768:1032], k == 0, k == KD - 1, [bwB, bhT], [bpq_])
                    actf(P, zs[:, n, :], pq_[:, 0:256], AF.Silu, [bpq_], [bzs])
                    cp(P, "dve", ab4[:, n, :], pq_[:, 256:264], [bpq_], [bab])
                    yield
                f16 = lambda t: t[:].rearrange("p a b -> p (a b)")
                B_ = list(bsc)
                actf(P, sc.beta[:], ab4[:, :, 4:8], AF.Sigmoid, [bab], B_)
                ts(P, "dve", f16(sc.nbeta), f16(sc.beta), -1.0, None, MUL, None, B_, B_)
                tt(P, "dve", sc.g[:], ab4[:, :, 0:4], bcast(LA("dtb").unsqueeze(1), [128, 4, 4]), ADD, [bab, K.blpa], B_)
                actf(P, f16(sc.g), f16(sc.g), AF.Exp, B_, B_)
                actf(P, f16(sc.g), f16(sc.g), AF.Ln, B_, B_, bias=c1, scale=1.0)
                tt(P, "dve", sc.g[:], sc.g[:], bcast(negA[:].unsqueeze(1), [128, 4, 4]), MUL, B_ + [bnegA], B_)
                mm(P, pX[1][:, 0:16], U1, f16(sc.g), True, True, B_ + [K.bconst], [bpX[1]])
                mm(P, pX[1][:, 16:32], U2, f16(sc.g), True, True, B_ + [K.bconst], [bpX[1]])
                mm(P, pX[1][:, 32:48], cview(K, "chunk0"), f16(sc.g), True, True, B_ + [K.bconst], [bpX[1]])
                mm(P, pX[1][:, 48:64], cview(K, "chunk1"), f16(sc.g), True, True, B_ + [K.bconst], [bpX[1]])
                actf(P, f16(sc.eG), pX[1][:, 0:16], AF.Exp, [bpX[1]], B_)
                actf(P, f16(sc.ekd), pX[1][:, 16:32], AF.Exp, [bpX[1]], B_)
                for cc in range(2):
                    actf(P, sc.gtot[:, :, cc, :], pX[1][:, 32 + 16 * cc:48 + 16 * cc].rearrange("p (a b) -> p a b", a=4), AF.Exp, [bpX[1]], B_)
                ts(P, "dve", f16(sc.neG), f16(sc.eG), -1.0, None, MUL, None, B_, B_)
                tt(P, "dve", f16(sc.eGrq), f16(sc.eG), f16(sc.rq), MUL, B_, B_)
                def prep(n):
                    nb = n % 2
                    tk = slice(n * 128, (n + 1) * 128)
                    for h in range(4):
                        ts(P, "dve", gU2[:, h * 128:(h + 1) * 128], U2, sc.g[:, n, h:h + 1], None, MUL, None, [bsc[n], K.bconst], [bgU2])
                    yield
                    for h in range(4):
                        mm(P, pX[0][:, h * 128:(h + 1) * 128], gU2[:, h * 128:(h + 1) * 128], U1, True, True, [bgU2, K.bconst], [bpX[0]])
                    for h in range(4):
                        pair, hp = h // 2, h % 2
                        mm(P, pX[1][:, h * 128:(h + 1) * 128], kTm[:, hp, pair, tk], kTb[:, pair, tk], True, True, [bkb, bkm], [bpX[1]])
                    for h in range(4):
                        pair, hp = h // 2, h % 2
                        mm(P, pX[2][:, h * 128:(h + 1) * 128], kTm[:, hp, pair, tk], qTb[:, pair, tk], True, True, [bkm, bqb], [bpX[2]])
                    yield
                    actf(P, expD[:], pX[0][:], AF.Exp, [bpX[0]], [bD])
                    yield
                    tt(P, "dve", v4(DTs), v4(expD), rep4("maskS"), MUL, [bD, K.bconst], [bD])
                    tt(P, "pool", v4(DTi), v4(expD), rep4("maskI"), MUL, [bD, K.bconst], [bD])
                    yield
                    for h in range(4):
                        stt(P, "dve", Ma[:, h * 128:(h + 1) * 128], pX[1][:, h * 128:(h + 1) * 128], sc.nbeta[:, n, h:h + 1],
                            DTs[:, h * 128:(h + 1) * 128], MUL, MUL, [bpX[1], bsc[n], bD], [bM])
                    yield
                    tt(P, "dve", QKT[nb][:], pX[2][:], DTi[:], MUL, [bpX[2], bD], [bQK[nb]])
                    tt(P, "dve", v4(Z[nb]), v4(Ma), rep4("ident"), ADD, [bM, K.bconst], [bZ[nb]])
                    pX0b = pX[0][:].bitcast(BF16)
                    for h in range(4):
                        P.op("pe", lambda e, h=h: e.transpose(out=pX0b[:, h * 128:(h + 1) * 128], in_=Ma[:, h * 128:(h + 1) * 128],
                                                              identity=K.ident_bf[:]), [bM, K.bconst], [bpX[0]])
                    yield
                    cp(P, "act", MTa[:], pX0b[:, 0:512], [bpX[0]], [bM])
                    yield
                    for t_ in (Ma, Mb, MTa, MTb):
                        bMx.setdefault(id(t_), Buf())
                    bMx[id(Ma)].lw = bM.lw; bMx[id(MTa)].lw = bM.lw
                    B_ = lambda t_: bMx[id(t_)]
                    Mc, MTc, Mn, MTn = Ma, MTa, Mb, MTb
                    pend = None
                    for lv in range(1, 6):
                        for h in range(4):
                            hs = slice(h * 128, (h + 1) * 128)
                            mm(P, pX[1][:, hs], Mc[:, hs], MTc[:, hs], True, True, [B_(Mc), B_(MTc)], [bpX[1]])
                        if lv < 5:
                            for h in range(4):
                                hs = slice(h * 128, (h + 1) * 128)
                                mm(P, pX[0][:, hs], MTc[:, hs], Mc[:, hs], True, True, [B_(Mc), B_(MTc)], [bpX[0]])
                        if pend is not None:
                            pend()
                        yield
                        cp(P, "act", MTn[:], pX[1][:], [bpX[1]], [B_(MTn)])
                        if lv < 5:
                            cp(P, "dve", Mn[:], pX[0][:], [bpX[0]], [B_(Mn)])
                        yield

                        def zupd(MTn=MTn):
                            for h in range(4):
                                hs = slice(h * 128, (h + 1) * 128)
                                mm(P, pX[2][:, hs], MTn[:, hs], Z[nb][:, hs], True, True, [B_(MTn), bZ[nb]], [bpX[2]])
                            tt(P, "dve", Z[nb][:], Z[nb][:], pX[2][:], ADD, [bZ[nb], bpX[2]], [bZ[nb]])
                        pend = zupd
                        Mc, MTc, Mn, MTn = Mn, MTn, Mc, MTc
                    pend()
                    P.join([bM], [bMx[id(t_)] for t_ in (Ma, Mb, MTa, MTb)] + [bM])

                def rec(n):
                    nb = n % 2
                    tk = slice(n * 128, (n + 1) * 128)
                    bankA, bankB, bankC = psA, psB, ptr[:, 512:1024]
                    bA, bB, bC = bpsA, bpsB, bptrB
                    H = [(h, h // 2, h % 2, slice((h % 2) * 64, (h % 2 + 1) * 64), slice(h * 64, (h + 1) * 64),
                          slice(h * 128, (h + 1) * 128)) for h in range(4)]
                    for cc in range(2):
                        cm = cview(K, "chunk%d" % cc)
                        stt(P, "dve", kdm[:, cc, :].rearrange("p (h c) -> p h c", h=4), ktm[:, n, :].rearrange("p (h c) -> p h c", h=4),
                            cm[:, 0:1], bcast(sc.ekd[:, n, :].unsqueeze(2), [128, 4, 64]), MUL, MUL, [bktm, bsc[n], K.bconst], [bkdm])
                    yield
                    for cc in range(2):
                        tp = slice(cc * 64, (cc + 1) * 64)
                        for h, pair, hp, kp, hc, hs in H:
                            mm(P, bankA[:, hc], kTm[:, hp, pair, tk], Sb[:, pair, :], True, True, [bkm, bSb[pair]], [bA])
                        for h, pair, hp, kp, hc, hs in H:
                            mm(P, bankB[:, hc], qTm[:, hp, pair, tk], Sb[:, pair, :], True, True, [bqm, bSb[pair]], [bB])
                        yield
                        for h, pair, hp, kp, hc, hs in H:
                            stt(P, "dve", rp[tp, h, :], bankA[tp, hc], sc.neG[tp, n, h:h + 1], vtm[tp, n, hc], MUL, ADD,
                                [bA, bsc[n], bvtm], [brp[h]])
                        for h, pair, hp, kp, hc, hs in H:
                            actf(P, tmpo[tp, h, :], bankB[tp, hc], AF.Copy, [bB, bsc[n]], [bto[h]], scale=sc.eGrq[tp, n, h:h + 1])
                        yield
                        for h, pair, hp, kp, hc, hs in H:
                            mm(P, bankC[:, hc], Z[nb][:, hs], rp[:, h, :], True, True, [bZ[nb], brp[h]], [bC])
                        yield
                        for h, pair, hp, kp, hc, hs in H:
                            ts(P, "dve", vnew[tp, h, :], bankC[tp, hc], sc.beta[tp, n, h:h + 1], None, MUL, None, [bC, bsc[n]], [bvn[h]])
                        yield
                        for h, pair, hp, kp, hc, hs in H:
                            mm(P, bankA[:, hc], QKT[nb][:, hs], vnew[:, h, :], True, True, [bQK[nb], bvn[h]], [bA])
                        for h, pair, hp, kp, hc, hs in H:
                            mm(P, bankB[:, hc], kdm[:, cc, pair * 128:(pair + 1) * 128], vnew[:, h, :], True, True, [bkdm, bvn[h]], [bB])
                        yield
                        for h, pair, hp, kp, hc, hs in H:
                            stt(P, "dve", otm1[tp, hc], bankA[tp, hc], sc.rq[tp, n, h:h + 1], tmpo[tp, h, :], MUL, ADD,
                                [bA, bsc[n], bto[h]], [botm1])
                        for h, pair, hp, kp, hc, hs in H:
                            stt(P, "dve", S[kp, pair, :], S[kp, pair, :], sc.gtot[kp, n, cc, h:h + 1], bankB[kp, hc], MUL, ADD,
                                [bS[h], bB, bsc[n]], [bS[h]])
                        for pair in range(2):
                            cp(P, "act", Sb[:, pair, :], S[:, pair, :], [bS[2 * pair], bS[2 * pair + 1]], [bSb[pair]])
                        yield
                    tt(P, "pool", ygn[:], otm1[:], otm1[:], MUL, [botm1], [bygn])
                    P.op("dve", lambda e: e.tensor_reduce(out=on4[:, 0:4], in_=ygn[:].rearrange("p (h c) -> p h c", h=4),
                                                          axis=AX.X, op=ADD), [bygn], [bon4])
                    yield
                    rsqrt_op(P, on4[:, 4:8], on4[:, 0:4], 1.0 / 64, bon4, bon4)
                    yield
                    tt(P, "dve", v4(ygn), otm1[:].rearrange("p (h c) -> p h c", h=4), bcast(on4[:, 4:8].unsqueeze(2), [128, 4, 64]),
                       MUL, [botm1, bon4, bygn], [bygn])
                    tt(P, "dve", ygn[:], ygn[:], LA("gdnn"), MUL, [bygn, K.blpa], [bygn])
                    tt(P, "dve", ygn[:], ygn[:], zs[:, n, :], MUL, [bygn, bzs], [bygn])
                    yield
                    for ct in range(2):
                        P.op("pe", lambda e, ct=ct: e.transpose(out=ptr[:, ct * 128:(ct + 1) * 128], in_=ygn[:, ct * 128:(ct + 1) * 128],
                                                                identity=K.ident32[:]), [bygn, K.bconst], [bptr])
                    yield
                    cp(P, "act", ygT[:, :, n * 128:(n + 1) * 128], ptr[:, 0:256].rearrange("p (c t) -> p c t", c=2), [bptr], [byg])

                def interleave(*gens):
                    live = [g for g in gens if g is not None]
                    while live:
                        for g in list(live):
                            try:
                                next(g)
                            except StopIteration:
                                live.remove(g)
                yield "front_done"
                P.dma(yAt[:], yA.ap[:, :, blk * 512:(blk + 1) * 512], reads=[yA.b[blk]], writes=[byA])
                interleave(prep(0))
                for n in range(4):
                    interleave(rec(n), prep(n + 1) if n < 3 else None)
            else:
                yield "front_done"
                P.dma(yAt[:], yA.ap[:, :, blk * 512:(blk + 1) * 512], reads=[yA.b[blk]], writes=[byA])
            yield "tiles_done"
            ysrc = [yAt[:, 0], yAt[:, 1], yAt[:, 2], yAt[:, 3], ygT[:, 0], ygT[:, 1], yAt[:, 4], yAt[:, 5]]
            def ld_x(n_):
                P.dma(xr[n_ % 2][:], x_src.ap[blk * 512 + n_ * 128:blk * 512 + (n_ + 1) * 128, :], reads=[x_src.b[blk]], writes=[bxr[n_ % 2]])
            ld_x(0)
            ld_x(1)
            for n in range(4):
                i = n % 2
                r0 = blk * 512 + n * 128
                for hf in range(2):
                    pw_, bpw_ = (pq, bpq) if hf == 0 else (psB, bpsB)
                    for k in range(KD):
                        mm(P, pw_[:], ysrc[k][:, n * 128:(n + 1) * 128], wo[:, k, hf * 512:(hf + 1) * 512], k == 0, k == KD - 1,
                           [byA, byg, bwo], [bpw_])
                    tt(P, "dve", xr[i][:, hf * 512:(hf + 1) * 512], xr[i][:, hf * 512:(hf + 1) * 512], pw_[:], ADD, [bxr[i], bpw_], [bxr[i]])
                if wsel is None:
                    P.dma(x_dst.ap[r0:r0 + 128, :], xr[i][:], reads=[bxr[i]], writes=[bdst[n]])
                else:
                    half, g = blk // (NB // 2), blk % (NB // 2)
                    rr = g * 512 + n * 128
                    ts(P, "dve", xr[i][:], xr[i][:], wsel[:, half:half + 1], None, MUL, None, [bxr[i], K.bconst], [bxr[i]])
                    if half == 0:
                        P.dma(x_dst.ap[rr:rr + 128, :], xr[i][:], reads=[bxr[i]], writes=[bdst[n]])
                    else:
                        P.dma(x_dst.ap[rr:rr + 128, :], xr[i][:], reads=[bxr[i], x_dst.b[g]], writes=[bdst[n]], q="pool", accum_op=ADD)
                if n + 2 < 4:
                    ld_x(n + 2)
                yield
            gi = blk if wsel is None else blk % (NB // 2)
            x_dst.b[gi] = Buf()
            P.join([x_dst.b[gi]], bdst)

        def run_until(g, marker):
            for v in g:
                if v == marker:
                    return

        gens = [block_gen(b_) for b_ in range(NB)]
        run_until(gens[0], "front_done")
        for b_ in range(NB):
            run_until(gens[b_], "tiles_done")
            nxt_ = gens[b_ + 1] if b_ + 1 < NB else None
            tail_done = False
            front_done = nxt_ is None
            while not (tail_done and front_done):
                if not tail_done:
                    try:
                        next(gens[b_])
                    except StopIteration:
                        tail_done = True
                if not front_done:
                    if next(nxt_) == "front_done":
                        front_done = True
        if en_gdn and st.gdn_s_out is not None:
            P.dma(st.gdn_s_out.rearrange("p (a b) -> p a b", a=2), S[:], reads=bS, writes=[st.bout3])
            P.dma(st.gdn_raw_out.rearrange("p (a b) -> p a b", a=6), rawT[:, :, 512:515], reads=[braw], writes=[st.bout4])


def host_inputs(inp):
    def ng(v):
        return np.ascontiguousarray(np.asarray(v, np.float32).reshape(KD, 128).T)
    normg = np.concatenate([ng(inp['mix_norm'][0]), ng(inp['ffn_norm'][0]), ng(inp['mix_norm'][1]), ng(inp['ffn_norm'][1])], axis=1)
    lp = [pack_layer_params(inp, l) for l in range(2)]
    f32 = lambda a: np.ascontiguousarray(np.asarray(a, np.float32))
    return dict(consts=make_consts(), normg=normg, final_norm=f32(inp['final_norm'])[None, :],
                w_in=f32(inp['w_in']), w_out=f32(inp['w_out']),
                lpa=np.stack([lp[0][0], lp[1][0]]), lps=np.stack([lp[0][1], lp[1][1]]),
                ffn_w_gate=f32(inp['ffn_w_gate'][0]), ffn_w_up=f32(inp['ffn_w_up'][0]), ffn_w_down=f32(inp['ffn_w_down'][0]),
                moe_router=f32(inp['moe_router'][0]), moe_w_gate=f32(inp['moe_w_gate'][0]), moe_w_up=f32(inp['moe_w_up'][0]),
                moe_w_down=f32(inp['moe_w_down'][0]))


_PROG_CACHE = {}


def kernel(**inputs):
    inp = {k: np.asarray(v) for k, v in inputs.items()}
    x = np.ascontiguousarray(inp["x"], dtype=np.float32)
    B, L, _ = x.shape
    T = L
    if T not in _PROG_CACHE:
        _PROG_CACHE[T] = build_program(T, split=True)[0]
    nc = _PROG_CACHE[T]
    shared = host_inputs(inp)
    n_cores = 2 * B
    in_maps = []
    for c in range(n_cores):
        m = dict(shared)
        m["x"] = x[c // 2]
        w = np.zeros((128, 2), np.float32)
        w[:, c % 2] = 1.0
        m["wsel"] = w
        in_maps.append(m)
    res = run_bass_kernel_spmd(nc, in_maps, core_ids=list(range(n_cores)))
    out = np.zeros((B, L, D), np.float32)
    for c in range(n_cores):
        out[c // 2, (c % 2) * (L // 2):(c % 2 + 1) * (L // 2)] = np.asarray(res.results[c]["out"], dtype=np.float32)
    return out
```

```python
import numpy as np
import ml_dtypes
import concourse.bass as bass
import concourse.mybir as mybir
from concourse.bass_utils import run_bass_kernel_spmd
from contextlib import ExitStack, contextmanager

F32 = mybir.dt.float32
BF16 = mybir.dt.bfloat16
AF = mybir.ActivationFunctionType
ALU = mybir.AluOpType
AX = mybir.AxisListType

D = 1024
KD = 8
EPS = 1e-6
N_EXP = 8
D_FF = 2816
D_FFE = 3584
IN_COLS = 2568


class Buf:
    __slots__ = ("lw", "rd", "x")

    def __init__(self, x=False):
        self.lw = None
        self.rd = []
        self.x = x


def bufs(n):
    return [Buf() for _ in range(n)]


class Prog:
    NDMA = 16

    def __init__(self, nc, es):
        self.nc = nc
        self.es = es
        self.stack = [es]
        self.cnt = {}
        self.sem = {}
        self.known = {}
        self.eng = {"pe": nc.tensor, "act": nc.scalar, "dve": nc.vector, "pool": nc.gpsimd, "sp": nc.sync}
        for e in self.eng:
            self.cnt[e] = 0
            self.sem[e] = es.enter_context(nc.semaphore("s_" + e))
            self.known[e] = {}
        self.dsem, self.dcnt, self.drot = {}, {}, {}
        for e in ("sp", "pool", "act"):
            self.dsem[e] = [es.enter_context(nc.semaphore("d_%s%d" % (e, i))) for i in range(self.NDMA)]
            self.dcnt[e] = [0] * self.NDMA
            self.drot[e] = 0
        self.nid = 0
        self.ninst = 0
        self.consts_ = {}

    @contextmanager
    def scope(self):
        with ExitStack() as es:
            self.stack.append(es)
            try:
                yield
            finally:
                self.barrier()
                self.stack.pop()

    def barrier(self):
        for e in self.eng:
            waits = []
            kn = self.known[e]
            for f in self.eng:
                if self.cnt[f] and kn.get(f, 0) < self.cnt[f]:
                    kn[f] = self.cnt[f]
                    waits.append((f, self.cnt[f]))
            for q in self.dsem:
                for i in range(self.NDMA):
                    key, v = (q, i), self.dcnt[q][i]
                    if v and kn.get(key, 0) < v:
                        kn[key] = v
                        waits.append((key, v))
            self._emit(e, waits, None, None)

    def sb(self, shape, dt=F32):
        self.nid += 1
        return self.stack[-1].enter_context(self.nc.sbuf_tensor("sb%d" % self.nid, list(shape), dt))

    def ps(self, shape, dt=F32):
        self.nid += 1
        return self.stack[-1].enter_context(self.nc.psum_tensor("ps%d" % self.nid, list(shape), dt))

    def _semobj(self, key):
        if isinstance(key, str):
            return self.sem[key]
        return self.dsem[key[0]][key[1]]

    def _deps(self, eng, reads, writes):
        need = {}
        for b in reads:
            if b.lw is not None:
                k, v = b.lw
                if need.get(k, 0) < v:
                    need[k] = v
        for b in writes:
            if b.lw is not None and b.lw[0] != eng:
                k, v = b.lw
                if need.get(k, 0) < v:
                    need[k] = v
            for k, v in b.rd:
                if k != eng and need.get(k, 0) < v:
                    need[k] = v
        kn = self.known[eng]
        out = []
        for k, v in need.items():
            if kn.get(k, 0) < v:
                kn[k] = v
                out.append((k, v))
        return out

    def _emit(self, engname, waits, fn, inc):
        e = self.eng[engname]
        for k, v in waits:
            e.wait_ge(self._semobj(k), v)
        if fn is None:
            return
        ins = fn(e)
        ins.then_inc(self._semobj(inc[0]), inc[1])
        self.ninst += 1

    def op(self, eng, fn, reads=(), writes=()):
        if any(b.x for b in reads):
            writes = list(writes) + [b for b in reads if b.x and b not in writes]
            reads = [b for b in reads if not b.x]
        waits = self._deps(eng, reads, writes)
        self.cnt[eng] += 1
        tag = (eng, self.cnt[eng])
        self._emit(eng, waits, fn, (eng, 1))
        for b in reads:
            b.rd.append(tag)
        for b in writes:
            b.lw = tag
            b.rd = []

    def dma(self, out, in_, reads=(), writes=(), q="sp", **kw):
        i = self.drot[q]
        self.drot[q] = (i + 1) % self.NDMA
        key = (q, i)
        waits = self._deps(q, reads, writes)
        prev = self.dcnt[q][i]
        if prev and self.known[q].get(key, 0) < prev:
            self.known[q][key] = prev
            waits.append((key, prev))
        self.dcnt[q][i] = prev + 16
        tag = (key, prev + 16)
        self._emit(q, waits, lambda e: e.dma_start(out=out, in_=in_, **kw), (key, 16))
        for b in reads:
            b.rd.append(tag)
        for b in writes:
            b.lw = tag
            b.rd = []

    def eps_ap(self, like, eps):
        return self.epst[0:like.shape[0], 0:1]

    def const_ap(self, val):
        if val not in self.consts_:
            t = self.es.enter_context(self.nc.sbuf_tensor("cst%d" % len(self.consts_), [128, 1], F32))
            b = Buf()
            self.op("dve", lambda e: e.memset(t[:], float(val)), [], [b])
            for e_ in ("act", "pool", "pe"):
                self.wait_all(e_, [b])
            self.consts_[val] = t
        return self.consts_[val][:, 0:1]

    def join(self, dst, srcs):
        self.op("sp", lambda e: e.nop(), reads=list(srcs), writes=list(dst))

    def wait_all(self, eng, bl):
        self._emit(eng, self._deps(eng, bl, ()), None, None)


def bcast(ap, shape):
    return ap.broadcast_to(list(shape))


def rsqrt_op(P, out, in_, scale, bin_, bout, eps=EPS):
    P.op("act", lambda e: e.activation(out=out, in_=in_, func=AF.Sqrt, bias=P.eps_ap(out, eps), scale=scale),
         reads=[bin_], writes=[bout])
    P.op("dve", lambda e: e.reciprocal(out=out, in_=out), reads=[bout], writes=[bout])


def ffn_stage(K, x_src, x_dst, normg, experts, router, final_g):
    P, nc, T = K.P, K.nc, x_src.T
    TB = min(T, 2048)
    NT = TB // 128
    NSB = TB // 512
    moe = router is not None
    with P.scope():
        xacc = P.sb([128, NT, D]); bx = bufs(NT)
        hT = P.sb([128, KD, TB], BF16); bh = bufs(NT)
        xs = [P.sb([128, D]) for _ in range(2)]; bxs = bufs(2)
        h32 = [P.sb([128, D]) for _ in range(2)]; bh32 = bufs(2)
        junk = P.sb([128, D], BF16); bjunk = Buf()
        ss = P.sb([128, NT]); bss = bufs(NT)
        rs = P.sb([128, NT]); brs = bufs(NT)
        gate = P.sb([128, NT, 8]); bgate = bufs(NT)
        sm = P.sb([128, 64]); bsm = Buf()
        wg = [P.sb([128, KD, 512], BF16) for _ in range(2)]; bwg = bufs(2)
        wu = [P.sb([128, KD, 512], BF16) for _ in range(2)]; bwu = bufs(2)
        wd = [P.sb([128, 4, D], BF16) for _ in range(2)]; bwd = bufs(2)
        act = [P.sb([128, 4, 512], BF16) for _ in range(2)]; bact = bufs(2)
        sg = [P.sb([128, 512]) for _ in range(2)]; bsg = bufs(2)
        pg = [P.ps([128, 512]) for _ in range(2)]; bpg = [Buf(True), Buf(True)]
        pu = [P.ps([128, 512]) for _ in range(2)]; bpu = [Buf(True), Buf(True)]
        pd = [P.ps([128, 512]) for _ in range(2)]; bpd = [Buf(True), Buf(True)]
        ptr = P.ps([128, 1024]); bptr = Buf(True)
        if moe:
            wr = P.sb([128, KD, 8]); bwr = Buf()
            P.dma(wr[:], router.rearrange("(k p) e -> p k e", p=128), writes=[bwr])
            lg = P.sb([128, 8]); blg = Buf()
            m8 = P.sb([128, 8]); bm8 = Buf()
        if final_g is not None:
            fg = P.sb([128, D]); bfg = Buf()
            P.dma(fg[:], final_g.partition_broadcast(128), writes=[bfg])
        gcnt = 0
        piece_idx = 0
        for tb in range(T // TB):
            r0 = tb * TB
            for n0 in range(0, NT, 4):
                P.dma(xacc[:, n0:n0 + 4, :],
                      x_src.ap[r0 + n0 * 128: r0 + (n0 + 4) * 128, :].rearrange("(n p) d -> p n d", p=128),
                      reads=[x_src.b[(r0 + n0 * 128) // 512]], writes=bx[n0:n0 + 4])
            def prep_tile(n):
                s2 = n % 2
                P.op("act", lambda e, n=n: e.activation(out=junk[:], in_=xacc[:, n, :], func=AF.Square,
                                                        accum_out=ss[:, n:n + 1]),
                     reads=[bx[n]], writes=[bjunk, bss[n]])
                rsqrt_op(P, rs[:, n:n + 1], ss[:, n:n + 1], 1.0 / D, bss[n], brs[n])
                P.op("act", lambda e, n=n, s2=s2: e.activation(out=xs[s2][:], in_=xacc[:, n, :], func=AF.Copy,
                                                               scale=rs[:, n:n + 1]),
                     reads=[bx[n], brs[n]], writes=[bxs[s2]])
                for k in range(KD):
                    P.op("pe", lambda e, k=k, s2=s2: e.transpose(out=ptr[:, k * 128:(k + 1) * 128],
                                                                 in_=xs[s2][:, k * 128:(k + 1) * 128],
                                                                 identity=K.ident32[:]),
                         reads=[bxs[s2], K.bconst], writes=[bptr])
                P.op("dve", lambda e, s2=s2: e.scalar_tensor_tensor(
                    out=h32[s2][:].rearrange("p (k t) -> p k t", k=KD),
                    in0=ptr[:].rearrange("p (k t) -> p k t", k=KD), scalar=1.0,
                    in1=bcast(normg.unsqueeze(2), [128, KD, 128]), op0=ALU.mult, op1=ALU.mult),
                    reads=[bptr, K.bconst], writes=[bh32[s2]])
                P.op("pool", lambda e, n=n, s2=s2: e.tensor_copy(
                    out=hT[:, :, n * 128:(n + 1) * 128], in_=h32[s2][:].rearrange("p (k t) -> p k t", k=KD)),
                    reads=[bh32[s2]], writes=[bh[n]])
                if moe:
                    for k in range(KD):
                        P.op("pe", lambda e, k=k, s2=s2: e.matmul(pd[0][:, 0:8], lhsT=h32[s2][:, k * 128:(k + 1) * 128],
                                                                  rhs=wr[:, k, :], start=(k == 0), stop=(k == KD - 1)),
                             reads=[bh32[s2], bwr], writes=[bpd[0]])
                    P.op("dve", lambda e: e.tensor_copy(out=lg[:], in_=pd[0][:, 0:8]), reads=[bpd[0]], writes=[blg])
                    P.op("dve", lambda e: e.max(out=m8[:], in_=lg[:]), reads=[blg], writes=[bm8])
                    P.op("dve", lambda e: e.tensor_tensor(out=sm[:, 2:3], in0=m8[:, 0:1], in1=m8[:, 1:2], op=ALU.subtract),
                         reads=[bm8], writes=[bsm])
                    P.op("act", lambda e: e.activation(out=sm[:, 0:1], in_=sm[:, 2:3], func=AF.Sigmoid),
                         reads=[bsm], writes=[bsm])
                    P.op("act", lambda e: e.activation(out=sm[:, 1:2], in_=sm[:, 2:3], func=AF.Sigmoid, scale=-1.0),
                         reads=[bsm], writes=[bsm])
                    P.op("dve", lambda e: e.tensor_scalar(out=sm[:, 8:16], in0=lg[:], scalar1=m8[:, 0:1], scalar2=sm[:, 0:1],
                                                          op0=ALU.is_equal, op1=ALU.mult),
                         reads=[blg, bm8, bsm], writes=[bsm])
                    P.op("dve", lambda e: e.tensor_scalar(out=sm[:, 16:24], in0=lg[:], scalar1=m8[:, 1:2], scalar2=sm[:, 1:2],
                                                          op0=ALU.is_equal, op1=ALU.mult),
                         reads=[blg, bm8, bsm], writes=[bsm])
                    P.op("dve", lambda e, n=n: e.tensor_tensor(out=gate[:, n, :], in0=sm[:, 8:16], in1=sm[:, 16:24], op=ALU.add),
                         reads=[bsm], writes=[bgate[n]])
            first_piece = True
            pend = []
            for ei, ex in enumerate(experts):
                nch = ex["ff"] // 128
                for c0 in range(0, nch, 4):
                    ncp = min(4, nch - c0)
                    sl = piece_idx % 2
                    piece_idx += 1
                    P.dma(wg[sl][:, :, 0:ncp * 128], ex["wg"][:, c0 * 128:(c0 + ncp) * 128].rearrange("(k p) f -> p k f", p=128),
                          writes=[bwg[sl]], q="pool")
                    P.dma(wu[sl][:, :, 0:ncp * 128], ex["wu"][:, c0 * 128:(c0 + ncp) * 128].rearrange("(k p) f -> p k f", p=128),
                          writes=[bwu[sl]], q="pool")
                    P.dma(wd[sl][:, 0:ncp, :], ex["wd"][c0 * 128:(c0 + ncp) * 128, :].rearrange("(c p) d -> p c d", p=128),
                          writes=[bwd[sl]], q="pool")
                    for sbk in range(NSB):
                        if first_piece:
                            if sbk == 0:
                                for n_ in range(0, 4):
                                    prep_tile(n_)
                            if sbk + 1 < NSB:
                                for n_ in range((sbk + 1) * 4, (sbk + 2) * 4):
                                    prep_tile(n_)
                        asl = gcnt % 2
                        gcnt += 1
                        hb = bh[sbk * 4:(sbk + 1) * 4]
                        for c in range(ncp):
                            pp = c % 2
                            for k in range(KD):
                                P.op("pe", lambda e, k=k, c=c, pp=pp, sl=sl, sbk=sbk: e.matmul(
                                    pg[pp][:], lhsT=wg[sl][:, k, c * 128:(c + 1) * 128], rhs=hT[:, k, sbk * 512:(sbk + 1) * 512],
                                    start=(k == 0), stop=(k == KD - 1)), reads=[bwg[sl]] + hb, writes=[bpg[pp]])
                            for k in range(KD):
                                P.op("pe", lambda e, k=k, c=c, pp=pp, sl=sl, sbk=sbk: e.matmul(
                                    pu[pp][:], lhsT=wu[sl][:, k, c * 128:(c + 1) * 128], rhs=hT[:, k, sbk * 512:(sbk + 1) * 512],
                                    start=(k == 0), stop=(k == KD - 1)), reads=[bwu[sl]] + hb, writes=[bpu[pp]])
                            P.op("act", lambda e, pp=pp: e.activation(out=sg[pp][:], in_=pg[pp][:], func=AF.Silu),
                                 reads=[bpg[pp]], writes=[bsg[pp]])
                            P.op("dve", lambda e, pp=pp, asl=asl, c=c: e.tensor_tensor(out=act[asl][:, c, :], in0=sg[pp][:],
                                                                                      in1=pu[pp][:], op=ALU.mult),
                                 reads=[bsg[pp], bpu[pp]], writes=[bact[asl]])
                        for f in pend:
                            f()
                        pend = []

                        def down(asl=asl, sl=sl, sbk=sbk, ncp=ncp, ei=ei):
                            for n4 in range(4):
                                n = sbk * 4 + n4
                                for hf in range(2):
                                    dp = (n4 * 2 + hf) % 2
                                    for c in range(ncp):
                                        P.op("pe", lambda e, c=c, dp=dp, n4=n4, hf=hf: e.matmul(
                                            pd[dp][:], lhsT=act[asl][:, c, n4 * 128:(n4 + 1) * 128],
                                            rhs=wd[sl][:, c, hf * 512:(hf + 1) * 512], start=(c == 0), stop=(c == ncp - 1)),
                                            reads=[bact[asl], bwd[sl]], writes=[bpd[dp]])
                                    if moe:
                                        P.op("dve", lambda e, dp=dp, n=n, hf=hf: e.scalar_tensor_tensor(
                                            out=xacc[:, n, hf * 512:(hf + 1) * 512], in0=pd[dp][:], scalar=gate[:, n, ei:ei + 1],
                                            in1=xacc[:, n, hf * 512:(hf + 1) * 512], op0=ALU.mult, op1=ALU.add),
                                            reads=[bpd[dp], bgate[n], bx[n]], writes=[bx[n]])
                                    else:
                                        P.op("dve", lambda e, dp=dp, n=n, hf=hf: e.tensor_tensor(
                                            out=xacc[:, n, hf * 512:(hf + 1) * 512], in0=pd[dp][:],
                                            in1=xacc[:, n, hf * 512:(hf + 1) * 512], op=ALU.add),
                                            reads=[bpd[dp], bx[n]], writes=[bx[n]])
                        pend.append(down)
                    first_piece = False
            for f in pend:
                f()
            pend = []
            for n in range(NT):
                if final_g is not None:
                    s2 = n % 2
                    P.op("act", lambda e, n=n: e.activation(out=junk[:], in_=xacc[:, n, :], func=AF.Square,
                                                            accum_out=ss[:, n:n + 1]),
                         reads=[bx[n]], writes=[bjunk, bss[n]])
                    rsqrt_op(P, rs[:, n:n + 1], ss[:, n:n + 1], 1.0 / D, bss[n], brs[n])
                    P.op("dve", lambda e, n=n: e.scalar_tensor_tensor(out=xacc[:, n, :], in0=xacc[:, n, :], scalar=rs[:, n:n + 1],
                                                                      in1=fg[:], op0=ALU.mult, op1=ALU.mult),
                         reads=[bx[n], brs[n], bfg], writes=[bx[n]])
            for n0 in range(0, NT, 4):
                P.dma(x_dst.ap[r0 + n0 * 128: r0 + (n0 + 4) * 128, :].rearrange("(n p) d -> p n d", p=128),
                      xacc[:, n0:n0 + 4, :], reads=bx[n0:n0 + 4], writes=[x_dst.b[(r0 + n0 * 128) // 512]])


class KCtx:
    pass


class DBuf:
    def __init__(self, ap, T):
        self.ap = ap
        self.T = T
        self.b = bufs(max(1, T // 512))


def build_program(T, do_mixer=(True, True), do_ffn=(True, True), do_final=True, en=(1, 1, 1, 1), split=False):
    nc = bass.Bass("TRN2", target_bir_lowering=False)
    K = KCtx()
    K.nc, K.T = nc, T
    dr = {}

    def din(name, shape, dt=F32):
        dr[name] = nc.dram_tensor(name, list(shape), dt, kind="ExternalInput").ap()
        return dr[name]
    x = DBuf(din("x", [T, D]), T)
    TO = T // 2 if split else T
    out = DBuf(nc.dram_tensor("out", [TO, D], F32, kind="ExternalOutput").ap(), TO)
    xh = DBuf(nc.dram_tensor("xh", [TO, D], F32, kind="Internal").ap(), TO)
    din("wsel", [128, 2])
    xs_ = [DBuf(nc.dram_tensor("xs%d" % i, [T, D], F32, kind="Internal").ap(), T) for i in range(3)]
    yA = DBuf(nc.dram_tensor("yA", [128, 6, T], BF16, kind="Internal").ap(), T)
    hTd = DBuf(nc.dram_tensor("hTd", [128, KD, T], BF16, kind="Internal").ap(), T)
    din("consts", [128, NCONST])
    din("normg", [128, 4 * KD])
    din("final_norm", [1, D])
    din("w_in", [2, D, IN_COLS]); din("w_out", [2, D, D])
    din("lpa", [2, 128, NLPA]); din("lps", [2, 128, NLPS])
    din("ffn_w_gate", [D, D_FF]); din("ffn_w_up", [D, D_FF]); din("ffn_w_down", [D_FF, D])
    din("moe_router", [D, N_EXP])
    din("moe_w_gate", [N_EXP, D, D_FFE]); din("moe_w_up", [N_EXP, D, D_FFE]); din("moe_w_down", [N_EXP, D_FFE, D])
    with ExitStack() as es:
        P = Prog(nc, es)
        K.P = P
        K.consts = P.sb([128, NCONST]); K.bconst = Buf()
        lo, hi = CONST_COLS["ident"]
        K.ident32 = K.consts[:, lo:hi]
        normg = P.sb([128, 4 * KD])
        wsel = P.sb([128, 2])
        P.epst = P.sb([128, 1])
        beps = Buf()
        P.op("dve", lambda e: e.memset(P.epst[:], EPS), writes=[beps])
        K.ones_bf = P.sb([128, 128], BF16)
        P.op("dve", lambda e: e.memset(K.ones_bf[:], 1.0), writes=[beps])
        K.ident_bf = P.sb([128, 128], BF16)
        P.const_ap(1.0); P.const_ap(64 * EPS)
        P.dma(K.consts[:], dr["consts"], writes=[K.bconst])
        bng = Buf()
        P.dma(normg[:], dr["normg"], writes=[bng])
        P.dma(wsel[:], dr["wsel"], writes=[K.bconst])
        P.op("dve", lambda e: e.tensor_copy(out=K.ident_bf[:], in_=K.ident32), reads=[K.bconst], writes=[beps])
        for e_ in ("dve", "pe", "act", "pool"):
            P.wait_all(e_, [bng, K.bconst, beps])
        cur = x
        free = list(xs_)
        st = NS()
        st.s5_init = st.sc_init = st.gdn_s_init = st.gdn_raw_init = None
        st.s5_out = st.sc_out = st.gdn_s_out = st.gdn_raw_out = None
        for l in range(2):
            if do_mixer[l]:
                with P.scope():
                    lpa = P.sb([128, NLPA]); K.blpa = Buf()
                    P.dma(lpa[:], dr["lpa"][l], writes=[K.blpa])
                    with P.scope():
                        S5 = NS()
                        S5.Wt = P.sb([128, 2, 8, 2, 128], BF16)
                        S5.QT = P.sb([128, 8, 8, 2, 64], BF16)
                        S5.BDT = P.sb([128, 2, 8, 128], BF16)
                        S5.Hr = P.sb([128, 7, 8]); S5.Hi = P.sb([128, 7, 8])
                        S5.b = Buf()
                        if en[0]:
                            with P.scope():
                                lps = P.sb([128, NLPS]); K.blps = Buf()
                                P.dma(lps[:], dr["lps"][l], writes=[K.blps])
                                s5_tables(K, lps, S5)
                        mixer_pass_a(K, l, cur, yA, hTd, normg[:, (2 * l) * KD:(2 * l + 1) * KD], dr["w_in"][l], lpa, S5, st, en=en[:3])
                    sp_ = split and l == 1
                    dst = xh if sp_ else free.pop(0)
                    mixer_pass_b(K, l, cur, dst, yA, hTd, normg[:, (2 * l) * KD:(2 * l + 1) * KD], dr["w_in"][l], dr["w_out"][l], lpa, st,
                                 en_gdn=bool(en[3]), wsel=wsel if sp_ else None)
                    if cur is not x and cur is not xh:
                        free.append(cur)
                    cur = dst
            if do_ffn[l]:
                last = (l == 1) or not (do_ffn[1] or do_mixer[1])
                dst = out if last else free.pop(0)
                if l == 0:
                    experts = [dict(wg=dr["ffn_w_gate"], wu=dr["ffn_w_up"], wd=dr["ffn_w_down"], ff=D_FF)]
                    router = None
                else:
                    experts = [dict(wg=dr["moe_w_gate"][e], wu=dr["moe_w_up"][e], wd=dr["moe_w_down"][e], ff=D_FFE)
                               for e in range(N_EXP)]
                    router = dr["moe_router"]
                ffn_stage(K, cur, dst, normg[:, (2 * l + 1) * KD:(2 * l + 2) * KD], experts, router,
                          dr["final_norm"] if (last and do_final) else None)
                if cur is not x:
                    free.append(cur)
                cur = dst
        K.final = cur
        if cur is not out:
            with P.scope():
                t = P.sb([128, 4, D]); bt = Buf()
                for blk in range(TO // 512):
                    P.dma(t[:], cur.ap[blk * 512:(blk + 1) * 512, :].rearrange("(n p) d -> p n d", p=128), reads=[cur.b[blk]], writes=[bt])
                    P.dma(out.ap[blk * 512:(blk + 1) * 512, :].rearrange("(n p) d -> p n d", p=128), t[:], reads=[bt], writes=[out.b[blk]])
        P.wait_all("sp", out.b)
    K.ninst = P.ninst
    return nc, K


def mm(P, out, lhsT, rhs, start, stop, rd, wr):
    P.op("pe", lambda e: e.matmul(out, lhsT=lhsT, rhs=rhs, start=start, stop=stop), rd, wr)


def tt(P, eng, out, in0, in1, op, rd, wr):
    P.op(eng, lambda e: e.tensor_tensor(out=out, in0=in0, in1=in1, op=op), rd, wr)


def ts(P, eng, out, in0, s1, s2, op0, op1, rd, wr):
    if s2 is None:
        P.op(eng, lambda e: e.tensor_scalar(out=out, in0=in0, scalar1=s1, scalar2=None, op0=op0), rd, wr)
    else:
        P.op(eng, lambda e: e.tensor_scalar(out=out, in0=in0, scalar1=s1, scalar2=s2, op0=op0, op1=op1), rd, wr)


def stt(P, eng, out, in0, scalar, in1, op0, op1, rd, wr):
    P.op(eng, lambda e: e.scalar_tensor_tensor(out=out, in0=in0, scalar=scalar, in1=in1, op0=op0, op1=op1), rd, wr)


def actf(P, out, in_, func, rd, wr, **kw):
    P.op("act", lambda e: e.activation(out=out, in_=in_, func=func, **kw), rd, wr)


def cp(P, eng, out, in_, rd, wr):
    if eng == "act":
        P.op(eng, lambda e: e.activation(out=out, in_=in_, func=AF.Copy), rd, wr)
    else:
        P.op(eng, lambda e: e.tensor_copy(out=out, in_=in_), rd, wr)


MUL, ADD, SUB = ALU.mult, ALU.add, ALU.subtract
MAGIC = 12582912.0
TWO_PI = 2.0 * np.pi

CONST_COLS = {}


def _const_layout():
    o = 0
    for name, n in [("ident", 128), ("triu", 128), ("U1", 128), ("U2", 128), ("maskS", 128), ("maskI", 128),
                    ("onesblk", 128), ("chunk0", 128), ("chunk1", 128), ("headsel", 2), ("mq", 4), ("mask8", 8), ("par", 2), ("pairm", 4)]:
        CONST_COLS[name] = (o, o + n)
        o += n
    return o


NCONST = _const_layout()


def make_consts():
    p = np.arange(128)
    ch = p // 64
    same = (ch[:, None] == ch[None, :])
    c = np.zeros((128, NCONST), np.float32)

    def put(name, a):
        lo, hi = CONST_COLS[name]
        c[:, lo:hi] = a
    put("ident", np.eye(128))
    put("triu", (p[None, :] >= p[:, None]))
    put("U1", same & (p[:, None] <= p[None, :]))
    put("U2", same & (p[:, None] > p[None, :]))
    put("maskS", same & (p[:, None] < p[None, :]))
    put("maskI", same & (p[:, None] <= p[None, :]))
    put("onesblk", same)
    put("chunk0", np.repeat((p < 64)[:, None], 128, 1))
    put("chunk1", np.repeat((p >= 64)[:, None], 128, 1))
    put("headsel", np.stack([p < 64, p >= 64], 1))
    put("mq", np.stack([(p // 16) % 4 == q for q in range(4)], 1))
    put("mask8", np.stack([(p // 16) == g for g in range(8)], 1))
    put("par", np.stack([(p // 16) % 2 == q for q in range(2)], 1))
    put("pairm", np.stack([(p // 32) == q for q in range(4)], 1))
    return c


LPA = {}
LPS = {}


def _lp_layout():
    o = 0
    for name, n in [("s5_d", 2), ("s5_bglu", 2), ("s5_on", 2), ("sc_on", 2), ("sc_conv", 6), ("gdn_conv", 24),
                    ("wglu", 512), ("ln_g", 256), ("ln_b", 256), ("gmn", 256), ("gdnn", 256), ("a_log", 4), ("dtb", 4),
                    ("bsp", 4), ("wsp", 512)]:
        LPA[name] = (o, o + n)
        o += n
    na = o
    o = 0
    for name, n in [("LRp", 8), ("LIp", 8), ("STp", 8), ("CRp", 128), ("CIp", 128), ("LRu", 128), ("LIu", 128), ("STu", 2),
                    ("BRu", 128), ("BIu", 128), ("CRu", 2048), ("CIu", 2048)]:
        LPS[name] = (o, o + n)
        o += n
    return na, o


NLPA, NLPS = _lp_layout()


def pack_layer_params(inp, l):
    a = np.zeros((128, NLPA), np.float32)
    s = np.zeros((128, NLPS), np.float32)

    def pa(name, v):
        lo, hi = LPA[name]
        a[:, lo:hi] = np.asarray(v, np.float32).reshape(128, hi - lo)

    def ps_(name, v):
        lo, hi = LPS[name]
        s[:, lo:hi] = np.asarray(v, np.float32).reshape(128, hi - lo)

    def fm(v, nt):
        return np.asarray(v).reshape(nt, 128).T
    pa("s5_d", fm(inp["s5_d"][l], 2)); pa("s5_bglu", fm(inp["s5_b_glu"][l], 2)); pa("s5_on", fm(inp["s5_out_norm"][l], 2))
    pa("sc_on", fm(inp["sc_out_norm"][l], 2))
    pa("sc_conv", inp["sc_conv"][l].reshape(3, 2, 128).transpose(2, 1, 0))
    pa("gdn_conv", inp["gdn_conv"][l].reshape(4, 6, 128).transpose(2, 1, 0))
    pa("wglu", inp["s5_w_glu"][l].reshape(2, 128, 256).transpose(1, 0, 2))
    rep = lambda v: np.broadcast_to(np.asarray(v)[None, :], (128, len(v)))
    pa("ln_g", rep(inp["sgu_ln_g"][l])); pa("ln_b", rep(inp["sgu_ln_b"][l])); pa("gmn", rep(inp["gmlp_out_norm"][l]))
    pa("gdnn", rep(np.tile(inp["gdn_norm"][l], 4))); pa("a_log", rep(inp["gdn_a_log"][l])); pa("dtb", rep(inp["gdn_dt_bias"][l]))
    pa("bsp", inp["sgu_b"][l].T)
    pa("wsp", inp["sgu_w"][l].transpose(2, 0, 1))
    lam_re, lam_im, st = inp["s5_lam_re"][l], inp["s5_lam_im"][l], inp["s5_log_step"][l]
    pm = lambda v: v.reshape(8, 2, 64).transpose(1, 2, 0).reshape(128, 8)
    ps_("LRp", pm(lam_re)); ps_("LIp", pm(lam_im)); ps_("STp", pm(np.repeat(st[:, None], 64, 1)))
    cpm = lambda c: c.reshape(8, 2, 16, 64).transpose(1, 3, 0, 2).reshape(128, 128)
    ps_("CRp", cpm(inp["s5_c_re"][l])); ps_("CIp", cpm(inp["s5_c_im"][l]))
    um = lambda v: np.repeat(v.reshape(2, 8, 1, 64), 16, 2).transpose(1, 2, 0, 3).reshape(128, 128)
    ps_("LRu", um(lam_re)); ps_("LIu", um(lam_im))
    ps_("STu", np.repeat(st.reshape(2, 8, 1), 16, 2).transpose(1, 2, 0).reshape(128, 2))
    bum = lambda b: b.reshape(2, 8, 64, 16).transpose(1, 3, 0, 2).reshape(128, 128)
    ps_("BRu", bum(inp["s5_b_re"][l])); ps_("BIu", bum(inp["s5_b_im"][l]))
    cum = lambda c: np.repeat(c.reshape(2, 8, 1, 16, 64), 16, 2).transpose(1, 2, 0, 3, 4).reshape(128, 2048)
    ps_("CRu", cum(inp["s5_c_re"][l])); ps_("CIu", cum(inp["s5_c_im"][l]))
    return a, s


def cview(K, name):
    lo, hi = CONST_COLS[name]
    return K.consts[:, lo:hi]


def s5_tables(K, lps, S5):
    P = K.P
    B = S5.b
    R = [B]

    def L(name, shape=None):
        lo, hi = LPS[name]
        v = lps[:, lo:hi]
        return v

    def cexp(dst_r, dst_i, lrdt, lidt, t0, t1, shape_n):
        actf(P, t0, lrdt, AF.Exp, R, R)
        ts(P, "dve", t1, lidt, 1.0 / TWO_PI, MAGIC, MUL, ADD, R, R)
        ts(P, "dve", t1, t1, MAGIC, -TWO_PI, SUB, MUL, R, R)
        tt(P, "dve", t1, t1, lidt, ADD, R, R)
        actf(P, dst_i, t1, AF.Sin, R, R)
        ts(P, "dve", dst_r, lidt, np.pi / 2, None, ADD, None, R, R)
        ts(P, "dve", t1, dst_r, 1.0 / TWO_PI, MAGIC, MUL, ADD, R, R)
        ts(P, "dve", t1, t1, MAGIC, -TWO_PI, SUB, MUL, R, R)
        tt(P, "dve", t1, t1, dst_r, ADD, R, R)
        actf(P, dst_r, t1, AF.Sin, R, R)
        tt(P, "dve", dst_r, dst_r, t0, MUL, R, R)
        tt(P, "dve", dst_i, dst_i, t0, MUL, R, R)

    def cmul(or_, oi, ar, ai, br, bi, t0):
        tt(P, "dve", t0, ai, bi, MUL, R, R)
        tt(P, "dve", or_, ar, br, MUL, R, R)
        tt(P, "dve", or_, or_, t0, SUB, R, R)
        tt(P, "dve", t0, ai, br, MUL, R, R)
        tt(P, "dve", oi, ar, bi, MUL, R, R)
        tt(P, "dve", oi, oi, t0, ADD, R, R)

    with P.scope():
        B.lw = K.blps.lw
        T = [P.sb([128, 128]) for _ in range(10)]
        dtu = P.sb([128, 2])
        Xr = P.sb([128, 8, 128]); Xi = P.sb([128, 8, 128])
        big0 = P.sb([128, 1024]); big1 = P.sb([128, 1024]); val = P.sb([128, 16])
        v3 = lambda t: t[:].rearrange("p (c n) -> p c n", c=2)
        actf(P, dtu[:], L("STu"), AF.Exp, R, R)
        dtb = bcast(dtu[:].unsqueeze(2), [128, 2, 64])
        lrdt, lidt, ar, ai, t0, t1 = T[0], T[1], T[2], T[3], T[4], T[5]
        tt(P, "dve", v3(lrdt), L("LRu").rearrange("p (c n) -> p c n", c=2), dtb, MUL, R, R)
        tt(P, "dve", v3(lidt), L("LIu").rearrange("p (c n) -> p c n", c=2), dtb, MUL, R, R)
        cexp(ar[:], ai[:], lrdt[:], lidt[:], t0[:], t1[:], 128)
        den, am1, fr, fi = T[6], T[7], T[8], T[9]
        tt(P, "dve", den[:], L("LRu"), L("LRu"), MUL, R, R)
        tt(P, "dve", t0[:], L("LIu"), L("LIu"), MUL, R, R)
        tt(P, "dve", den[:], den[:], t0[:], ADD, R, R)
        P.op("dve", lambda e: e.reciprocal(out=den[:], in_=den[:]), R, R)
        ts(P, "dve", am1[:], ar[:], -1.0, None, ADD, None, R, R)
        tt(P, "dve", fr[:], am1[:], L("LRu"), MUL, R, R)
        tt(P, "dve", t0[:], ai[:], L("LIu"), MUL, R, R)
        tt(P, "dve", fr[:], fr[:], t0[:], ADD, R, R)
        tt(P, "dve", fr[:], fr[:], den[:], MUL, R, R)
        tt(P, "dve", fi[:], ai[:], L("LRu"), MUL, R, R)
        tt(P, "dve", t0[:], am1[:], L("LIu"), MUL, R, R)
        tt(P, "dve", fi[:], fi[:], t0[:], SUB, R, R)
        tt(P, "dve", fi[:], fi[:], den[:], MUL, R, R)
        cmul(Xr[:, 0, :], Xi[:, 0, :], fr[:], fi[:], L("BRu"), L("BIu"), t0[:])
        for k in range(7):
            cmul(Xr[:, k + 1, :], Xi[:, k + 1, :], Xr[:, k, :], Xi[:, k, :], ar[:], ai[:], t0[:])
        par = cview(K, "par")
        for jp in range(8):
            for part, X in enumerate((Xr, Xi)):
                src = X[:, 7 - jp, :].rearrange("p (c n) -> p c n", c=2)
                for half in range(2):
                    ts(P, "dve", S5.Wt[:, :, jp, part, half * 64:(half + 1) * 64], src, par[:, half:half + 1], None, MUL, None,
                       R + [K.bconst], [S5.b])
        m8 = cview(K, "mask8")
        for k in range(8):
            for ct in range(2):
                xr = bcast(Xr[:, k, ct * 64:(ct + 1) * 64].unsqueeze(1), [128, 16, 64])
                xi = bcast(Xi[:, k, ct * 64:(ct + 1) * 64].unsqueeze(1), [128, 16, 64])
                lo = LPS["CRu"][0] + ct * 1024
                cr = lps[:, lo:lo + 1024].rearrange("p (q n) -> p q n", q=16)
                lo = LPS["CIu"][0] + ct * 1024
                ci = lps[:, lo:lo + 1024].rearrange("p (q n) -> p q n", q=16)
                b0 = big0[:].rearrange("p (q n) -> p q n", q=16)
                b1 = big1[:].rearrange("p (q n) -> p q n", q=16)
                tt(P, "dve", b0, cr, xr, MUL, R, R)
                tt(P, "dve", b1, ci, xi, MUL, R, R)
                tt(P, "dve", b0, b0, b1, SUB, R, R)
                P.op("dve", lambda e, b0=b0: e.tensor_reduce(out=val[:], in_=b0, axis=AX.X, op=ADD), R, R)
                tt(P, "dve", S5.BDT[:, ct, k, :].rearrange("p (g q) -> p g q", g=8), bcast(val[:].unsqueeze(1), [128, 8, 16]),
                   bcast(m8.unsqueeze(2), [128, 8, 16]), MUL, R + [K.bconst], [S5.b])
        Q = [P.sb([128, 8]) for _ in range(6)]
        Pr = P.sb([128, 9, 8]); Pi = P.sb([128, 9, 8])
        dtp, lrp, lip, t0, t1 = Q[0], Q[1], Q[2], Q[3], Q[4]
        actf(P, dtp[:], L("STp"), AF.Exp, R, R)
        tt(P, "dve", lrp[:], L("LRp"), dtp[:], MUL, R, R)
        tt(P, "dve", lip[:], L("LIp"), dtp[:], MUL, R, R)
        cexp(Pr[:, 1, :], Pi[:, 1, :], lrp[:], lip[:], t0[:], t1[:], 8)
        for k in range(1, 8):
            cmul(Pr[:, k + 1, :], Pi[:, k + 1, :], Pr[:, k, :], Pi[:, k, :], Pr[:, 1, :], Pi[:, 1, :], t0[:])
        cp(P, "dve", S5.Hr[:, 0, :], Pr[:, 8, :], R, [S5.b])
        cp(P, "dve", S5.Hi[:, 0, :], Pi[:, 8, :], R, [S5.b])
        for s in range(6):
            cmul(S5.Hr[:, s + 1, :], S5.Hi[:, s + 1, :], S5.Hr[:, s, :], S5.Hi[:, s, :], S5.Hr[:, s, :], S5.Hi[:, s, :], t0[:])
        P.op("dve", lambda e: e.memset(S5.QT[:], 0.0), R, [S5.b])
        c0 = P.sb([128, 8, 16]); c1 = P.sb([128, 8, 16])
        CR = L("CRp").rearrange("p (r q) -> p r q", r=8)
        CI = L("CIp").rearrange("p (r q) -> p r q", r=8)
        for j in range(8):
            pr_b = bcast(Pr[:, j + 1, :].unsqueeze(2), [128, 8, 16])
            pi_b = bcast(Pi[:, j + 1, :].unsqueeze(2), [128, 8, 16])
            for part in range(2):
                if part == 0:
                    tt(P, "dve", c0[:], CR, pr_b, MUL, R, R)
                    tt(P, "dve", c1[:], CI, pi_b, MUL, R, R)
                    tt(P, "dve", c0[:], c0[:], c1[:], SUB, R, R)
                else:
                    tt(P, "dve", c0[:], CR, pi_b, MUL, R, R)
                    tt(P, "dve", c1[:], CI, pr_b, MUL, R, R)
                    stt(P, "dve", c0[:], c0[:], -1.0, c1[:], MUL, SUB, R, R)
                for gl in range(2):
                    for lp in range(2):
                        cp(P, "dve", S5.QT[gl * 64:(gl + 1) * 64, lp::2, j, part, 32 * lp + 16 * gl:32 * lp + 16 * gl + 16],
                           c0[gl * 64:(gl + 1) * 64, lp::2, :], R, [S5.b])


class NS:
    pass


def load_x_block_hT(K, x_src, blk, xr, bxr, xs2, bxs2, hT, bhT, ptrs, bptrs, normg, rs, brs, ss, bss, junk, bjunk):
    P = K.P
    r0 = blk * 512
    for n in range(4):
        i = n % 2
        xs, bxs = xs2[i], bxs2[i]
        ptr, bptr = ptrs[i], bptrs[i]
        P.dma(xr[i][:], x_src.ap[r0 + n * 128:r0 + (n + 1) * 128, :], reads=[x_src.b[blk]], writes=[bxr[i]])
        actf(P, junk[:], xr[i][:], AF.Square, [bxr[i]], [bjunk, bss[i]], accum_out=ss[:, n:n + 1])
        rsqrt_op(P, rs[:, n:n + 1], ss[:, n:n + 1], 1.0 / D, bss[i], brs[i])
        actf(P, xs[:], xr[i][:], AF.Copy, [bxr[i], brs[i]], [bxs], scale=rs[:, n:n + 1])
        ptb = ptr.bitcast(BF16)
        for k in range(KD):
            P.op("pe", lambda e, k=k, xs=xs, ptb=ptb: e.transpose(out=ptb[:, k * 128:(k + 1) * 128], in_=xs[:, k * 128:(k + 1) * 128],
                                                                  identity=K.ident_bf[:]), [bxs, K.bconst], bptr)
        tt(P, "dve", hT[:, :, n * 128:(n + 1) * 128], ptb[:, 0:1024].rearrange("p (k t) -> p k t", k=KD),
           bcast(normg.unsqueeze(2), [128, KD, 128]), MUL, bptr + [K.bconst], [bhT])


def fm_norm(K, y, by, gain, out_fn, W, bout, perm=False):
    P = K.P
    actf(P, W.sq[:], y[:], AF.Square, [by], [W.bsq])
    for ct in range(2):
        mm(P, W.pn[:, 0:512], K.ones_bf[:], W.sq[:, ct, :], ct == 0, ct == 1, [W.bsq, K.bconst], [W.bpn])
    actf(P, W.rstd[:], W.pn[:, 0:512], AF.Sqrt, [W.bpn], [W.brstd], bias=P.epst[:, 0:1], scale=1.0 / 256)
    P.op("dve", lambda e: e.reciprocal(out=W.rstd[:], in_=W.rstd[:]), [W.brstd], [W.brstd])
    for ct in range(2):
        if perm:
            yi = y[:, ct, :].rearrange("p (j c) -> p j c", j=8)
            ri = W.rstd[:].rearrange("p (j c) -> p j c", j=8)
        else:
            yi, ri = y[:, ct, :], W.rstd[:]
        stt(P, "dve", out_fn(ct), yi, gain[:, ct:ct + 1], ri, MUL, MUL, [by, W.brstd], [bout])


def mixer_pass_a(K, l, x_src, yA, hTd, normg, w_in, lpa, S5, st, en=(1, 1, 1)):
    P, T = K.P, K.T
    NB = T // 512
    PAD = 64
    with P.scope():
        wA = P.sb([128, KD, 1536], BF16); bwA = Buf(); bwA2 = Buf()
        P.dma(wA[:, :, 0:768], w_in[:, 0:768].rearrange("(k p) c -> p k c", p=128), writes=[bwA], q="pool")
        P.dma(wA[:, :, 768:1536], w_in[:, 1800:2568].rearrange("(k p) c -> p k c", p=128), writes=[bwA2], q="pool")
        bW = [bwA, bwA2]
        xr = [P.sb([128, D]) for _ in range(2)]; bxr = bufs(2)
        xs2 = [P.sb([128, D], BF16) for _ in range(2)]; bxs2 = bufs(2)
        hT = P.sb([128, KD, 512], BF16); bhT = Buf()
        junk = P.sb([128, D], BF16); bjunk = Buf()
        ss = P.sb([128, 4]); bss = bufs(2); rs = P.sb([128, 4]); brs = bufs(2)
        ptr = P.ps([128, 1024]); bptr = Buf(True); bptrB = Buf(True)
        pp = [P.ps([128, 512]) for _ in range(2)]; bpp = [Buf(True), Buf(True)]
        pg = [P.ps([128, 512]) for _ in range(2)]; bpg = [Buf(True), Buf(True)]
        pv = P.ps([128, 1024]); bpv = Buf(True)
        W = NS()
        W.sq = P.sb([128, 2, 512], BF16); W.bsq = Buf()
        W.rstd = P.sb([128, 512]); W.brstd = Buf()
        W.pn = pg[1]; W.bpn = bpg[1]
        W2 = NS()
        W2.sq = P.sb([128, 2, 512], BF16); W2.bsq = Buf()
        W2.rstd = P.sb([128, 512]); W2.brstd = Buf()
        W2.pn = pv[:, 512:1024]; W2.bpn = bpv
        ya_sc = P.sb([128, 2, 512]); bya_sc = Buf()
        junk_placeholder = None
        yst = P.sb([128, 6, 512], BF16); byst = bufs(3)
        ya = P.sb([128, 2, 512]); bya = Buf()
        yb = P.sb([128, 2, 512]); byb = Buf()
        P.op("dve", lambda e: e.memset(yst[:], 0.0), [], byst)
        uT = P.sb([128, 2, 512], BF16); buT = Buf()
        uTm = P.sb([128, 4, 2, 512], BF16); buTm = Buf()
        SA = P.sb([128, 8, 2, PAD + 65]); SB = P.sb([128, 8, 2, PAD + 65]); bSA = bufs(2); bSB = bufs(2)
        Sb16 = P.sb([128, 8, 2, 64], BF16); bS16 = Buf()
        hsT = [P.sb([128, 8, 65]) for _ in range(4)]; bhs = bufs(4); bdr, bdi = Buf(), Buf()
        yg = P.sb([128, 2, 512], BF16); byg = Buf()
        sig = P.sb([128, 512]); bsig = Buf()
        wglu = P.sb([128, 2, 256], BF16); bwglu = Buf()
        cp(P, "dve", wglu[:], lpa[:, LPA["wglu"][0]:LPA["wglu"][1]].rearrange("p (k c) -> p k c", k=2), [K.blpa], [bwglu])
        P.op("dve", lambda e: e.memset(SA[:], 0.0), [], bSA)
        P.op("dve", lambda e: e.memset(SB[:], 0.0), [], bSB)
        if st.s5_init is not None:
            P.dma(SA[:, :, :, PAD:PAD + 1], st.s5_init.rearrange("p (r c o) -> p r c o", r=8, c=2), reads=[], writes=bSA)
        Bsb = P.sb([128, 2, 512]); bB = Buf()
        Csb = P.sb([128, 2, 512]); bC = Buf()
        z = P.sb([128, 2, 2 + 512], BF16); bz = Buf()
        dsc = P.sb([128, 2, 3, 128], BF16); bdsc = Buf()
        lo = LPA["sc_conv"][0]
        for ct in range(2):
            for j in range(3):
                ts(P, "dve", dsc[:, ct, j, :], K.ident32[:], lpa[:, lo + ct * 3 + j:lo + ct * 3 + j + 1], None, MUL, None,
                   [K.blpa, K.bconst], [bdsc])
        P.op("dve", lambda e: e.memset(z[:], 0.0), [], [bz])
        if st.sc_init is not None:
            P.dma(z[:, :, 0:2], st.sc_init.rearrange("p (c o) -> p c o", c=2), reads=[], writes=[bz], q="pool")
        wsp = P.sb([128, 4, 128], BF16); bwsp = Buf()
        lo = LPA["wsp"][0]
        tt(P, "dve", wsp[:], lpa[:, lo:lo + 512].rearrange("p (h t) -> p h t", h=4),
           bcast(cview(K, "triu").unsqueeze(1), [128, 4, 128]), MUL, [K.blpa, K.bconst], [bwsp])
        def gm_set(pg_, bpg_):
            return (P.sb([128, 512]), Buf(), P.sb([128, 256]), Buf(), P.sb([128, 256], BF16), Buf(), P.sb([128, 256]), Buf(),
                    P.sb([128, 6]), P.sb([128, 2]), Buf(), P.sb([128, 2]), Buf(), P.sb([128, 256], BF16), Buf(), pg_, bpg_)
        gmA = gm_set(pg, bpg)
        gmB = gm_set(pp, bpp)

        def LA(name):
            lo, hi = LPA[name]
            return lpa[:, lo:hi]

        for blk in range(NB):
            load_x_block_hT(K, x_src, blk, xr, bxr, xs2, bxs2, hT, bhT, [ptr[:], pv[:]], [[bptr, bptrB], [bpv]], normg, rs, brs, ss, bss,
                            junk, bjunk)
            P.dma(hTd.ap[:, :, blk * 512:(blk + 1) * 512], hT[:], reads=[bhT], writes=[hTd.b[blk]])

            def proj_fm(col0, ps_ap, pbuf):
                wi, c = (0, col0) if col0 < 768 else (1, col0 - 1800 + 768)
                for k in range(KD):
                    mm(P, ps_ap, wA[:, k, c:c + 128], hT[:, k, :], k == 0, k == KD - 1, [bW[wi], bhT], [pbuf])
            def g_sc():
                if not en[2]:
                    return
                cp(P, "pool", z[:, :, 0:2], z[:, :, 512:514], [bz], [bz])
                for ct in range(2):
                    proj_fm(1800 + ct * 128, pp[0][:], bpp[0])
                    yield
                    cp(P, "act", Bsb[:, ct, :], pp[0][:], [bpp[0]], [bB])
                    proj_fm(2056 + ct * 128, pp[1][:], bpp[1])
                    yield
                    cp(P, "act", Csb[:, ct, :], pp[1][:], [bpp[1]], [bC])
                    proj_fm(2312 + ct * 128, pp[0][:], bpp[0])
                    yield
                    tt(P, "dve", z[:, ct, 2:514], Csb[:, ct, :], pp[0][:], MUL, [bC, bpp[0]], [bz])
                    yield
                for ct in range(2):
                    for j in range(3):
                        mm(P, pg[0][:], dsc[:, ct, j, :], z[:, ct, j:j + 512], j == 0, j == 2, [bdsc, bz], [bpg[0]])
                    yield
                    tt(P, "dve", ya_sc[:, ct, :], pg[0][:], Bsb[:, ct, :], MUL, [bpg[0], bB], [bya_sc])
                    yield
                fm_norm(K, ya_sc, bya_sc, LA("sc_on"), lambda ct: yst[:, 4 + ct, :], W, byst[2])
                yield
            def g_gm_tile(n, B_):
                gm, bgm, vn, bvn, vnb, bvnb, og, bog, stt6, mv, bmv, gs, bgs, junk2, bjunk2, pg, bpg = B_
                if True:
                    for k in range(KD):
                        mm(P, pg[0][:], hT[:, k, n * 128:(n + 1) * 128], wA[:, k, 256:768], k == 0, k == KD - 1, [bwA, bhT], [bpg[0]])
                    yield
                    actf(P, gm[:], pg[0][:], AF.Gelu_apprx_tanh, [bpg[0]], [bgm])
                    yield
                    P.op("dve", lambda e: e.bn_stats(out=stt6[:], in_=gm[:, 256:512]), [bgm], [bmv])
                    P.op("dve", lambda e: e.bn_aggr(out=mv[:], in_=stt6[:]), [bmv], [bmv])
                    rsqrt_op(P, mv[:, 1:2], mv[:, 1:2], 1.0, bmv, bmv)
                    ts(P, "dve", vn[:], gm[:, 256:512], mv[:, 0:1], mv[:, 1:2], SUB, MUL, [bgm, bmv], [bvn])
                    tt(P, "dve", vn[:], vn[:], LA("ln_g"), MUL, [bvn, K.blpa], [bvn])
                    tt(P, "dve", vnb[:], vn[:], LA("ln_b"), ADD, [bvn, K.blpa], [bvnb])
                    yield
                    for h in range(4):
                        mm(P, pg[1][:, h * 64:(h + 1) * 64], wsp[:, h, :], vnb[:, h * 64:(h + 1) * 64], True, True, [bwsp, bvnb], [bpg[1]])
                    lo = LPA["bsp"][0]
                    for h in range(4):
                        stt(P, "dve", og[:, h * 64:(h + 1) * 64], pg[1][:, h * 64:(h + 1) * 64], lpa[:, lo + h:lo + h + 1],
                            gm[:, h * 64:(h + 1) * 64], ADD, MUL, [bpg[1], bgm, K.blpa], [bog])
                    yield
                    actf(P, junk2[:], og[:], AF.Square, [bog], [bjunk2, bgs], accum_out=gs[:, 0:1])
                    yield
                    rsqrt_op(P, gs[:, 1:2], gs[:, 0:1], 1.0 / 256, bgs, bgs)
                    stt(P, "dve", og[:], og[:], gs[:, 1:2], LA("gmn"), MUL, MUL, [bog, bgs, K.blpa], [bog])
                    for ct in range(2):
                        P.op("pe", lambda e, ct=ct: e.transpose(out=pg[1][:, 256 + ct * 128:256 + (ct + 1) * 128],
                                                                in_=og[:, ct * 128:(ct + 1) * 128], identity=K.ident32[:]),
                             [bog, K.bconst], [bpg[1]])
                    yield
                    cp(P, "act", yst[:, 2:4, n * 128:(n + 1) * 128], pg[1][:, 256:512].rearrange("p (c t) -> p c t", c=2),
                       [bpg[1]], [byst[1]])
                    yield
            def g_gm():
                if not en[1]:
                    return
                for n0 in (0, 2):
                    live_ = [g_gm_tile(n0, gmA), g_gm_tile(n0 + 1, gmB)]
                    while live_:
                        for g_ in list(live_):
                            try:
                                next(g_)
                                yield
                            except StopIteration:
                                live_.remove(g_)

            DBG = 9

            def g_s5():
                if not en[0]:
                    return
                pvh = [pv[:, 0:512], pv[:, 512:1024]]
                for ct in range(2):
                    proj_fm(ct * 128, pvh[ct], bpv)
                    yield
                    cp(P, "act", uT[:, ct, :], pvh[ct], [bpv], [buT])
                    yield
                pm = cview(K, "pairm")
                for q4 in range(4):
                    actf(P, uTm[:, q4, :, :], uT[:], AF.Copy, [buT, K.bconst], [buTm], scale=pm[:, q4:q4 + 1])
                for pr in range(8):
                    ct, q4 = pr // 4, pr % 4
                    for part in range(2):
                        for jp in range(8):
                            mm(P, pv[:, (pr * 2 + part) * 64:(pr * 2 + part + 1) * 64],
                               S5.Wt[:, ct, jp, part, :], uTm[:, q4, ct, jp::8], jp == 0, jp == 7, [S5.b, buTm], [bpv])
                    if pr % 2 == 1:
                        yield
                cp(P, "dve", SA[:, :, :, PAD + 1:PAD + 65], pv[:].rearrange("p (r c n) -> p r c n", r=8, c=2), [bpv], bSA)
                src, dst, bs, bd = SA, SB, bSA, bSB
                for s in range(7 if DBG >= 2 else 0):
                    d = 1 << s
                    L = 65 - d
                    hr = bcast(S5.Hr[:, s, :].unsqueeze(2), [128, 8, L])
                    hi = bcast(S5.Hi[:, s, :].unsqueeze(2), [128, 8, L])
                    sr, si = src[:, :, 0, PAD + d:PAD + 65], src[:, :, 1, PAD + d:PAD + 65]
                    shr, shi = src[:, :, 0, PAD:PAD + L], src[:, :, 1, PAD:PAD + L]
                    dr_, di_ = dst[:, :, 0, PAD + d:PAD + 65], dst[:, :, 1, PAD + d:PAD + 65]
                    cp(P, "act", dst[:, :, :, PAD:PAD + d], src[:, :, :, PAD:PAD + d], bs, bd)
                    tt(P, "dve", hsT[0][:, :, 0:L], shr, hr, MUL, [bs[0], S5.b], [bhs[0]])
                    tt(P, "dve", hsT[1][:, :, 0:L], shi, hi, MUL, [bs[1], S5.b], [bhs[1]])
                    tt(P, "dve", hsT[2][:, :, 0:L], shi, hr, MUL, [bs[1], S5.b], [bhs[2]])
                    tt(P, "dve", hsT[3][:, :, 0:L], shr, hi, MUL, [bs[0], S5.b], [bhs[3]])
                    tt(P, "dve", dr_, sr, hsT[0][:, :, 0:L], ADD, [bs[0], bhs[0]], [bd[0]])
                    tt(P, "dve", di_, si, hsT[2][:, :, 0:L], ADD, [bs[1], bhs[2]], [bd[1]])
                    tt(P, "dve", dr_, dr_, hsT[1][:, :, 0:L], SUB, [bd[0], bhs[1]], [bd[0]])
                    tt(P, "dve", di_, di_, hsT[3][:, :, 0:L], ADD, [bd[1], bhs[3]], [bd[1]])
                    src, dst, bs, bd = dst, src, bd, bs
                    yield
                cp(P, "act", Sb16[:], src[:, :, :, PAD:PAD + 64], bs, [bS16])
                cp(P, "pool", SA[:, :, :, PAD:PAD + 1], src[:, :, :, PAD + 64:PAD + 65], bs, bSA)
                yield
                for ct in range(2 if DBG >= 3 else 0):
                    pa, pb, bpa, bpb = ptr[:, 0:512], ptr[:, 512:1024], bptr, bptrB
                    for j in range(8):
                        for jp in range(j + 1):
                            mm(P, pa[:, j * 64:(j + 1) * 64], S5.BDT[:, ct, j - jp, :], uT[:, ct, jp::8], jp == 0, jp == j,
                               [S5.b, buT], [bpa])
                        if j % 3 == 2:
                            yield
                    for j in range(8):
                        for hh in range(2):
                            i = 0
                            for lp in range(2):
                                pr = 4 * ct + 2 * hh + lp
                                for part in range(2):
                                    mm(P, pb[hh * 64:(hh + 1) * 64, j * 64:(j + 1) * 64], S5.QT[:, pr, j, part, :],
                                       Sb16[:, pr, part, :], i == 0, i == 3, [S5.b, bS16], [bpb])
                                    i += 1
                        if j % 2 == 1:
                            yield
                    lo = LPA["s5_d"][0]
                    stt(P, "dve", ya[:, ct, :].rearrange("p (j c) -> p j c", j=8), uT[:, ct, :].rearrange("p (c j) -> p j c", j=8),
                        lpa[:, lo + ct:lo + ct + 1], pa.rearrange("p (j c) -> p j c", j=8), MUL, ADD, [buT, bpa, K.blpa], [bya])
                    yield
                    tt(P, "dve", ya[:, ct, :], ya[:, ct, :], pb, ADD, [bya, bpb], [bya])
                    yield
                if DBG < 3:
                    P.op("dve", lambda e: e.memset(ya[:], 0.5), [], [bya])
                actf(P, yg[:], ya[:], AF.Gelu_apprx_tanh, [bya], [byg])
                lo = LPA["s5_bglu"][0]
                for mc in range(2 if DBG >= 4 else 0):
                    for kc in range(2):
                        mm(P, pvh[0], wglu[:, kc, mc * 128:(mc + 1) * 128], yg[:, kc, :], kc == 0, kc == 1, [bwglu, byg], [bpv])
                    yield
                    actf(P, sig[:], pvh[0], AF.Sigmoid, [bpv, K.blpa], [bsig], bias=lpa[:, lo + mc:lo + mc + 1])
                    yield
                    tt(P, "dve", yb[:, mc, :], yg[:, mc, :], sig[:], MUL, [byg, bsig], [byb])
                    yield
                if DBG < 4:
                    P.op("dve", lambda e: e.memset(yb[:], 0.5), [], [byb])
                if DBG >= 5:
                    fm_norm(K, yb, byb, LA("s5_on"), lambda ct: yst[:, ct, :].rearrange("p (c j) -> p j c", j=8), W2, byst[0], perm=True)
                yield

            def seq_(*gs):
                for g in gs:
                    yield from g
            live = [g_s5(), seq_(g_sc(), g_gm())]
            wts = {id(live[0]): 2, id(live[1]): 1}
            while live:
                for g in list(live):
                    for _ in range(wts[id(g)]):
                        try:
                            next(g)
                        except StopIteration:
                            live.remove(g)
                            break
            P.dma(yA.ap[:, :, blk * 512:(blk + 1) * 512], yst[:], reads=byst, writes=[yA.b[blk]])
        if st.s5_out is not None:
            P.dma(st.s5_out.rearrange("p (r c o) -> p r c o", r=8, c=2), SA[:, :, :, PAD:PAD + 1], reads=bSA, writes=[st.bout])
            P.dma(st.sc_out.rearrange("p (c o) -> p c o", c=2), z[:, :, 512:514], reads=[bz], writes=[st.bout2])


def mixer_pass_b(K, l, x_src, x_dst, yA, hTd, normg, w_in, w_out, lpa, st, en_gdn=True, wsel=None):
    P, T = K.P, K.T
    NB = T // 512
    with P.scope():
        wB = P.sb([128, KD, 1032], BF16); bwB = Buf()
        P.dma(wB[:], w_in[:, 768:1800].rearrange("(k p) c -> p k c", p=128), writes=[bwB], q="pool")
        wo = P.sb([128, KD, D], BF16); bwo = Buf()
        P.dma(wo[:], w_out.rearrange("(k p) c -> p k c", p=128), writes=[bwo], q="pool")
        xr = [P.sb([128, D]) for _ in range(2)]; bxr = bufs(2)
        xs = P.sb([128, D]); bxs = Buf()
        hT = P.sb([128, KD, 512], BF16); bhT = Buf()
        junk = P.sb([128, D], BF16); bjunk = Buf()
        ss = P.sb([128, 4]); bss = Buf(); rs = P.sb([128, 4]); brs = Buf()
        ptr = P.ps([128, 1024]); bptr = Buf(True); bptrB = Buf(True)
        pq = P.ps([128, 512]); bpq = Buf(True)
        pX = [P.ps([128, 512]) for _ in range(3)]; bpX = [Buf(True) for _ in range(3)]
        psA = P.ps([128, 512]); psB = P.ps([128, 512]); bpsA, bpsB = Buf(True), Buf(True)
        yAt = P.sb([128, 6, 512], BF16); byA = Buf()
        ygT = P.sb([128, 2, 512], BF16); byg = Buf()
        bdst = bufs(4)

        def LA(name):
            lo, hi = LPA[name]
            return lpa[:, lo:hi]
        if en_gdn:
            rawT = P.sb([128, 6, 515], BF16); braw = Buf()
            dgd = P.sb([128, 6, 4, 128], BF16); bdgd = Buf()
            lo = LPA["gdn_conv"][0]
            for ct in range(6):
                for j in range(4):
                    ts(P, "dve", dgd[:, ct, j, :], K.ident32[:], lpa[:, lo + ct * 4 + j:lo + ct * 4 + j + 1], None, MUL, None,
                       [K.blpa, K.bconst], [bdgd])
            P.op("dve", lambda e: e.memset(rawT[:], 0.0), [], [braw])
            qT = P.sb([128, 2, 512]); kT = P.sb([128, 2, 512]); vT = P.sb([128, 2, 512]); bq, bk, bv = Buf(), Buf(), Buf()
            sq = xs[:].rearrange("p (c t) -> p c t", c=2); bsq = bxs
            rk = P.sb([128, 512]); brk = Buf()
            kTm = P.sb([128, 2, 2, 512], BF16); qTm = P.sb([128, 2, 2, 512], BF16); bkm, bqm = Buf(), Buf()
            kTb = P.sb([128, 2, 512], BF16); qTb = P.sb([128, 2, 512], BF16); bkb, bqb = Buf(), Buf()
            kdm = P.sb([128, 2, 256], BF16); bkdm = Buf()
            ktm = P.sb([128, 4, 256]); vtm = P.sb([128, 4, 256]); bktm, bvtm = Buf(), Buf()
            zs = P.sb([128, 4, 256]); bzs = Buf()
            otm1 = P.sb([128, 256]); botm1 = Buf()
            ab4 = P.sb([128, 4, 8]); bab = Buf()
            sc = NS()
            for nm in ("beta", "nbeta", "g", "eG", "neG", "ekd", "rq", "eGrq"):
                setattr(sc, nm, P.sb([128, 4, 4]))
            sc.gtot = P.sb([128, 4, 2, 4]); bsc = bufs(4)
            negA = P.sb([128, 4]); bnegA = Buf()
            actf(P, negA[:], LA("a_log"), AF.Exp, [K.blpa], [bnegA])
            ts(P, "dve", negA[:], negA[:], -1.0, None, MUL, None, [bnegA], [bnegA])
            tmp4 = P.sb([128, 16]); btmp4 = Buf()
            gU2 = P.sb([128, 512]); bgU2 = Buf()
            expD = P.sb([128, 512]); DTs = P.sb([128, 512]); DTi = P.sb([128, 512]); bD = Buf()
            Ma = P.sb([128, 512], BF16); Mb = P.sb([128, 512], BF16); MTa = P.sb([128, 512], BF16); MTb = P.sb([128, 512], BF16)
            bM = Buf(); bMx = {}
            Z = [P.sb([128, 512], BF16) for _ in range(2)]; bZ = bufs(2)
            QKT = [P.sb([128, 512], BF16) for _ in range(2)]; bQK = bufs(2)
            S = P.sb([128, 2, 64]); bS = bufs(4)
            Sb = P.sb([128, 2, 64], BF16); bSb = bufs(2)
            P.op("dve", lambda e: e.memset(S[:], 0.0), [], bS)
            P.op("dve", lambda e: e.memset(Sb[:], 0.0), [], bSb)
            rp = P.sb([128, 4, 64], BF16); vnew = P.sb([128, 4, 64], BF16); tmpo = P.sb([128, 4, 64]); brp, bvn, bto = bufs(4), bufs(4), bufs(4)
            P.op("dve", lambda e: e.memset(rp[:], 0.0), [], brp)
            P.op("dve", lambda e: e.memset(vnew[:], 0.0), [], bvn)
            ygn = P.sb([128, 256]); bygn = Buf()
            on4 = P.sb([128, 8]); bon4 = Buf()
            c1 = P.const_ap(1.0)
            c64e = P.const_ap(64 * EPS)
            U1, U2 = cview(K, "U1"), cview(K, "U2")
            rep4 = lambda name: bcast(cview(K, name).unsqueeze(1), [128, 4, 128])
            v4 = lambda t: t[:].rearrange("p (h c) -> p h c", h=4)
            if st.gdn_s_init is not None:
                P.dma(S[:], st.gdn_s_init.rearrange("p (a b) -> p a b", a=2), reads=[], writes=bS)
                P.dma(rawT[:, :, 0:3], st.gdn_raw_init.rearrange("p (a b) -> p a b", a=6), reads=[], writes=[braw])
        else:
            P.op("dve", lambda e: e.memset(ygT[:], 0.0), [], [byg])

        def block_gen(blk):
            P.dma(hT[:], hTd.ap[:, :, blk * 512:(blk + 1) * 512], reads=[hTd.b[blk]], writes=[bhT])
            GD = 9
            if en_gdn and GD < 9:
                P.op("dve", lambda e: e.memset(ygT[:], 0.0), [], [byg])
            if en_gdn:
                cp(P, "pool", rawT[:, :, 0:3], rawT[:, :, 512:515], [braw], [braw])
                pqs = [(pq, bpq), (psA, bpsA)]
                for ct in range(6):
                    pq_, bpq_ = pqs[ct % 2]
                    for k in range(KD):
                        mm(P, pq_[:], wB[:, k, ct * 128:(ct + 1) * 128], hT[:, k, :], k == 0, k == KD - 1, [bwB, bhT], [bpq_])
                    cp(P, "act", rawT[:, ct, 3:515], pq_[:], [bpq_], [braw])
                    yield
                for ct in range(6):
                    pq_, bpq_ = pqs[ct % 2]
                    for j in range(4):
                        mm(P, pq_[:], dgd[:, ct, j, :], rawT[:, ct, j:j + 512], j == 0, j == 3, [bdgd, braw], [bpq_])
                    dst, bd = [(qT, bq), (kT, bk), (vT, bv)][ct // 2]
                    actf(P, dst[:, ct % 2, :], pq_[:], AF.Silu, [bpq_], [bd])
                    yield
                actf(P, sq, kT[:], AF.Square, [bk], [bsq])
                for ct in range(2 if GD >= 2 else 0):
                    mm(P, pq[:], cview(K, "onesblk"), sq[:, ct, :], True, True, [bsq, K.bconst], [bpq])
                    actf(P, rk[:], pq[:], AF.Sqrt, [bpq], [brk], bias=P.epst[:, 0:1], scale=1.0)
                    P.op("dve", lambda e: e.reciprocal(out=rk[:], in_=rk[:]), [brk], [brk])
                    tt(P, "dve", kT[:, ct, :], kT[:, ct, :], rk[:], MUL, [bk, brk], [bk])
                    yield
                cp(P, "act", kTb[:], kT[:], [bk], [bkb])
                cp(P, "act", qTb[:], qT[:], [bq], [bqb])
                hsel = cview(K, "headsel")
                for hp in range(2 if GD >= 2 else 0):
                    actf(P, kTm[:, hp, :, :], kT[:], AF.Copy, [bk, K.bconst], [bkm], scale=hsel[:, hp:hp + 1])
                    actf(P, qTm[:, hp, :, :], qT[:], AF.Copy, [bq, K.bconst], [bqm], scale=hsel[:, hp:hp + 1])
                actf(P, sq, qT[:], AF.Square, [bq], [bsq])
                for n in range(4 if GD >= 2 else 0):
                    for ct in range(2):
                        mm(P, pX[0][:, n * 4 + 2 * ct:n * 4 + 2 * ct + 2], sq[:, ct, n * 128:(n + 1) * 128], cview(K, "headsel"),
                           True, True, [bsq, K.bconst], [bpX[0]])
                actf(P, tmp4[:], pX[0][:, 0:16], AF.Sqrt, [bpX[0]], [btmp4], bias=c64e, scale=64.0)
                P.op("dve", lambda e: e.reciprocal(out=sc.rq[:].rearrange("p a b -> p (a b)"), in_=tmp4[:]), [btmp4], bsc)
                for n in range(4):
                    pt_, bpt_ = (ptr[:, 0:512], bptr) if n % 2 == 0 else (ptr[:, 512:1024], bptrB)
                    for ct in range(2):
                        P.op("pe", lambda e, n=n, ct=ct, pt_=pt_: e.transpose(out=pt_[:, ct * 128:(ct + 1) * 128],
                                                                             in_=kT[:, ct, n * 128:(n + 1) * 128], identity=K.ident32[:]),
                             [bk, K.bconst], [bpt_])
                        P.op("pe", lambda e, n=n, ct=ct, pt_=pt_: e.transpose(out=pt_[:, 256 + ct * 128:256 + (ct + 1) * 128],
                                                                             in_=vT[:, ct, n * 128:(n + 1) * 128], identity=K.ident32[:]),
                             [bv, K.bconst], [bpt_])
                    cp(P, "act", ktm[:, n, :], pt_[:, 0:256], [bpt_], [bktm])
                    cp(P, "dve", vtm[:, n, :], pt_[:, 256:512], [bpt_], [bvtm])
                    yield
                for n in range(4):
                    pq_, bpq_ = pqs[n % 2]
                    for k in range(KD):
                        mm(P, pq_[:, 0:264], hT[:, k, n * 128:(n + 1) * 128], wB[:, k, 768:1032], k == 0, k == KD - 1, [bwB, bhT], [bpq_])
                    actf(P, zs[:, n, :], pq_[:, 0:256], AF.Silu, [bpq_], [bzs])
                    cp(P, "dve", ab4[:, n, :], pq_[:, 256:264], [bpq_], [bab])
                    yield
                f16 = lambda t: t[:].rearrange("p a b -> p (a b)")
                B_ = list(bsc)
                actf(P, sc.beta[:], ab4[:, :, 4:8], AF.Sigmoid, [bab], B_)
                ts(P, "dve", f16(sc.nbeta), f16(sc.beta), -1.0, None, MUL, None, B_, B_)
                tt(P, "dve", sc.g[:], ab4[:, :, 0:4], bcast(LA("dtb").unsqueeze(1), [128, 4, 4]), ADD, [bab, K.blpa], B_)
                actf(P, f16(sc.g), f16(sc.g), AF.Exp, B_, B_)
                actf(P, f16(sc.g), f16(sc.g), AF.Ln, B_, B_, bias=c1, scale=1.0)
                tt(P, "dve", sc.g[:], sc.g[:], bcast(negA[:].unsqueeze(1), [128, 4, 4]), MUL, B_ + [bnegA], B_)
                mm(P, pX[1][:, 0:16], U1, f16(sc.g), True, True, B_ + [K.bconst], [bpX[1]])
                mm(P, pX[1][:, 16:32], U2, f16(sc.g), True, True, B_ + [K.bconst], [bpX[1]])
                mm(P, pX[1][:, 32:48], cview(K, "chunk0"), f16(sc.g), True, True, B_ + [K.bconst], [bpX[1]])
                mm(P, pX[1][:, 48:64], cview(K, "chunk1"), f16(sc.g), True, True, B_ + [K.bconst], [bpX[1]])
                actf(P, f16(sc.eG), pX[1][:, 0:16], AF.Exp, [bpX[1]], B_)
                actf(P, f16(sc.ekd), pX[1][:, 16:32], AF.Exp, [bpX[1]], B_)
                for cc in range(2):
                    actf(P, sc.gtot[:, :, cc, :], pX[1][:, 32 + 16 * cc:48 + 16 * cc].rearrange("p (a b) -> p a b", a=4), AF.Exp, [bpX[1]], B_)
                ts(P, "dve", f16(sc.neG), f16(sc.eG), -1.0, None, MUL, None, B_, B_)
                tt(P, "dve", f16(sc.eGrq), f16(sc.eG), f16(sc.rq), MUL, B_, B_)
                def prep(n):
                    nb = n % 2
                    tk = slice(n * 128, (n + 1) * 128)
                    for h in range(4):
                        ts(P, "dve", gU2[:, h * 128:(h + 1) * 128], U2, sc.g[:, n, h:h + 1], None, MUL, None, [bsc[n], K.bconst], [bgU2])
                    yield
                    for h in range(4):
                        mm(P, pX[0][:, h * 128:(h + 1) * 128], gU2[:, h * 128:(h + 1) * 128], U1, True, True, [bgU2, K.bconst], [bpX[0]])
                    for h in range(4):
                        pair, hp = h // 2, h % 2
                        mm(P, pX[1][:, h * 128:(h + 1) * 128], kTm[:, hp, pair, tk], kTb[:, pair, tk], True, True, [bkb, bkm], [bpX[1]])
                    for h in range(4):
                        pair, hp = h // 2, h % 2
                        mm(P, pX[2][:, h * 128:(h + 1) * 128], kTm[:, hp, pair, tk], qTb[:, pair, tk], True, True, [bkm, bqb], [bpX[2]])
                    yield
                    actf(P, expD[:], pX[0][:], AF.Exp, [bpX[0]], [bD])
                    yield
                    tt(P, "dve", v4(DTs), v4(expD), rep4("maskS"), MUL, [bD, K.bconst], [bD])
                    tt(P, "pool", v4(DTi), v4(expD), rep4("maskI"), MUL, [bD, K.bconst], [bD])
                    yield
                    for h in range(4):
                        stt(P, "dve", Ma[:, h * 128:(h + 1) * 128], pX[1][:, h * 128:(h + 1) * 128], sc.nbeta[:, n, h:h + 1],
                            DTs[:, h * 128:(h + 1) * 128], MUL, MUL, [bpX[1], bsc[n], bD], [bM])
                    yield
                    tt(P, "dve", QKT[nb][:], pX[2][:], DTi[:], MUL, [bpX[2], bD], [bQK[nb]])
                    tt(P, "dve", v4(Z[nb]), v4(Ma), rep4("ident"), ADD, [bM, K.bconst], [bZ[nb]])
                    pX0b = pX[0][:].bitcast(BF16)
                    for h in range(4):
                        P.op("pe", lambda e, h=h: e.transpose(out=pX0b[:, h * 128:(h + 1) * 128], in_=Ma[:, h * 128:(h + 1) * 128],
                                                              identity=K.ident_bf[:]), [bM, K.bconst], [bpX[0]])
                    yield
                    cp(P, "act", MTa[:], pX0b[:, 0:512], [bpX[0]], [bM])
                    yield
                    for t_ in (Ma, Mb, MTa, MTb):
                        bMx.setdefault(id(t_), Buf())
                    bMx[id(Ma)].lw = bM.lw; bMx[id(MTa)].lw = bM.lw
                    B_ = lambda t_: bMx[id(t_)]
                    Mc, MTc, Mn, MTn = Ma, MTa, Mb, MTb
                    pend = None
                    for lv in range(1, 6):
                        for h in range(4):
                            hs = slice(h * 128, (h + 1) * 128)
                            mm(P, pX[1][:, hs], Mc[:, hs], MTc[:, hs], True, True, [B_(Mc), B_(MTc)], [bpX[1]])
                        if lv < 5:
                            for h in range(4):
                                hs = slice(h * 128, (h + 1) * 128)
                                mm(P, pX[0][:, hs], MTc[:, hs], Mc[:, hs], True, True, [B_(Mc), B_(MTc)], [bpX[0]])
                        if pend is not None:
                            pend()
                        yield
                        cp(P, "act", MTn[:], pX[1][:], [bpX[1]], [B_(MTn)])
                        if lv < 5:
                            cp(P, "dve", Mn[:], pX[0][:], [bpX[0]], [B_(Mn)])
                        yield

                        def zupd(MTn=MTn):
                            for h in range(4):
                                hs = slice(h * 128, (h + 1) * 128)
                                mm(P, pX[2][:, hs], MTn[:, hs], Z[nb][:, hs], True, True, [B_(MTn), bZ[nb]], [bpX[2]])
                            tt(P, "dve", Z[nb][:], Z[nb][:], pX[2][:], ADD, [bZ[nb], bpX[2]], [bZ[nb]])
                        pend = zupd
                        Mc, MTc, Mn, MTn = Mn, MTn, Mc, MTc
                    pend()
                    P.join([bM], [bMx[id(t_)] for t_ in (Ma, Mb, MTa, MTb)] + [bM])

                def rec(n):
                    nb = n % 2
                    tk = slice(n * 128, (n + 1) * 128)
                    bankA, bankB, bankC = psA, psB, ptr[:, 512:1024]
                    bA, bB, bC = bpsA, bpsB, bptrB
                    H = [(h, h // 2, h % 2, slice((h % 2) * 64, (h % 2 + 1) * 64), slice(h * 64, (h + 1) * 64),
                          slice(h * 128, (h + 1) * 128)) for h in range(4)]
                    for cc in range(2):
                        cm = cview(K, "chunk%d" % cc)
                        stt(P, "dve", kdm[:, cc, :].rearrange("p (h c) -> p h c", h=4), ktm[:, n, :].rearrange("p (h c) -> p h c", h=4),
                            cm[:, 0:1], bcast(sc.ekd[:, n, :].unsqueeze(2), [128, 4, 64]), MUL, MUL, [bktm, bsc[n], K.bconst], [bkdm])
                    yield
                    for cc in range(2):
                        tp = slice(cc * 64, (cc + 1) * 64)
                        for h, pair, hp, kp, hc, hs in H:
                            mm(P, bankA[:, hc], kTm[:, hp, pair, tk], Sb[:, pair, :], True, True, [bkm, bSb[pair]], [bA])
                        for h, pair, hp, kp, hc, hs in H:
                            mm(P, bankB[:, hc], qTm[:, hp, pair, tk], Sb[:, pair, :], True, True, [bqm, bSb[pair]], [bB])
                        yield
                        for h, pair, hp, kp, hc, hs in H:
                            stt(P, "dve", rp[tp, h, :], bankA[tp, hc], sc.neG[tp, n, h:h + 1], vtm[tp, n, hc], MUL, ADD,
                                [bA, bsc[n], bvtm], [brp[h]])
                        for h, pair, hp, kp, hc, hs in H:
                            actf(P, tmpo[tp, h, :], bankB[tp, hc], AF.Copy, [bB, bsc[n]], [bto[h]], scale=sc.eGrq[tp, n, h:h + 1])
                        yield
                        for h, pair, hp, kp, hc, hs in H:
                            mm(P, bankC[:, hc], Z[nb][:, hs], rp[:, h, :], True, True, [bZ[nb], brp[h]], [bC])
                        yield
                        for h, pair, hp, kp, hc, hs in H:
                            ts(P, "dve", vnew[tp, h, :], bankC[tp, hc], sc.beta[tp, n, h:h + 1], None, MUL, None, [bC, bsc[n]], [bvn[h]])
                        yield
                        for h, pair, hp, kp, hc, hs in H:
                            mm(P, bankA[:, hc], QKT[nb][:, hs], vnew[:, h, :], True, True, [bQK[nb], bvn[h]], [bA])
                        for h, pair, hp, kp, hc, hs in H:
                            mm(P, bankB[:, hc], kdm[:, cc, pair * 128:(pair + 1) * 128], vnew[:, h, :], True, True, [bkdm, bvn[h]], [bB])
                        yield
                        for h, pair, hp, kp, hc, hs in H:
                            stt(P, "dve", otm1[tp, hc], bankA[tp, hc], sc.rq[tp, n, h:h + 1], tmpo[tp, h, :], MUL, ADD,
                                [bA, bsc[n], bto[h]], [botm1])
                        for h, pair, hp, kp, hc, hs in H:
                            stt(P, "dve", S[kp, pair, :], S[kp, pair, :], sc.gtot[kp, n, cc, h:h + 1], bankB[kp, hc], MUL, ADD,
                                [bS[h], bB, bsc[n]], [bS[h]])
                        for pair in range(2):
                            cp(P, "act", Sb[:, pair, :], S[:, pair, :], [bS[2 * pair], bS[2 * pair + 1]], [bSb[pair]])
                        yield
                    tt(P, "pool", ygn[:], otm1[:], otm1[:], MUL, [botm1], [bygn])
                    P.op("dve", lambda e: e.tensor_reduce(out=on4[:, 0:4], in_=ygn[:].rearrange("p (h c) -> p h c", h=4),
                                                          axis=AX.X, op=ADD), [bygn], [bon4])
                    yield
                    rsqrt_op(P, on4[:, 4:8], on4[:, 0:4], 1.0 / 64, bon4, bon4)
                    yield
                    tt(P, "dve", v4(ygn), otm1[:].rearrange("p (h c) -> p h c", h=4), bcast(on4[:, 4:8].unsqueeze(2), [128, 4, 64]),
                       MUL, [botm1, bon4, bygn], [bygn])
                    tt(P, "dve", ygn[:], ygn[:], LA("gdnn"), MUL, [bygn, K.blpa], [bygn])
                    tt(P, "dve", ygn[:], ygn[:], zs[:, n, :], MUL, [bygn, bzs], [bygn])
                    yield
                    for ct in range(2):
                        P.op("pe", lambda e, ct=ct: e.transpose(out=ptr[:, ct * 128:(ct + 1) * 128], in_=ygn[:, ct * 128:(ct + 1) * 128],
                                                                identity=K.ident32[:]), [bygn, K.bconst], [bptr])
                    yield
                    cp(P, "act", ygT[:, :, n * 128:(n + 1) * 128], ptr[:, 0:256].rearrange("p (c t) -> p c t", c=2), [bptr], [byg])

                def interleave(*gens):
                    live = [g for g in gens if g is not None]
                    while live:
                        for g in list(live):
                            try:
                                next(g)
                            except StopIteration:
                                live.remove(g)
                yield "front_done"
                P.dma(yAt[:], yA.ap[:, :, blk * 512:(blk + 1) * 512], reads=[yA.b[blk]], writes=[byA])
                interleave(prep(0))
                for n in range(4):
                    interleave(rec(n), prep(n + 1) if n < 3 else None)
            else:
                yield "front_done"
                P.dma(yAt[:], yA.ap[:, :, blk * 512:(blk + 1) * 512], reads=[yA.b[blk]], writes=[byA])
            yield "tiles_done"
            ysrc = [yAt[:, 0], yAt[:, 1], yAt[:, 2], yAt[:, 3], ygT[:, 0], ygT[:, 1], yAt[:, 4], yAt[:, 5]]
            def ld_x(n_):
                P.dma(xr[n_ % 2][:], x_src.ap[blk * 512 + n_ * 128:blk * 512 + (n_ + 1) * 128, :], reads=[x_src.b[blk]], writes=[bxr[n_ % 2]])
            ld_x(0)
            ld_x(1)
            for n in range(4):
                i = n % 2
                r0 = blk * 512 + n * 128
                for hf in range(2):
                    pw_, bpw_ = (pq, bpq) if hf == 0 else (psB, bpsB)
                    for k in range(KD):
                        mm(P, pw_[:], ysrc[k][:, n * 128:(n + 1) * 128], wo[:, k, hf * 512:(hf + 1) * 512], k == 0, k == KD - 1,
                           [byA, byg, bwo], [bpw_])
                    tt(P, "dve", xr[i][:, hf * 512:(hf + 1) * 512], xr[i][:, hf * 512:(hf + 1) * 512], pw_[:], ADD, [bxr[i], bpw_], [bxr[i]])
                if wsel is None:
                    P.dma(x_dst.ap[r0:r0 + 128, :], xr[i][:], reads=[bxr[i]], writes=[bdst[n]])
                else:
                    half, g = blk // (NB // 2), blk % (NB // 2)
                    rr = g * 512 + n * 128
                    ts(P, "dve", xr[i][:], xr[i][:], wsel[:, half:half + 1], None, MUL, None, [bxr[i], K.bconst], [bxr[i]])
                    if half == 0:
                        P.dma(x_dst.ap[rr:rr + 128, :], xr[i][:], reads=[bxr[i]], writes=[bdst[n]])
                    else:
                        P.dma(x_dst.ap[rr:rr + 128, :], xr[i][:], reads=[bxr[i], x_dst.b[g]], writes=[bdst[n]], q="pool", accum_op=ADD)
                if n + 2 < 4:
                    ld_x(n + 2)
                yield
            gi = blk if wsel is None else blk % (NB // 2)
            x_dst.b[gi] = Buf()
            P.join([x_dst.b[gi]], bdst)

        def run_until(g, marker):
            for v in g:
                if v == marker:
                    return

        gens = [block_gen(b_) for b_ in range(NB)]
        run_until(gens[0], "front_done")
        for b_ in range(NB):
            run_until(gens[b_], "tiles_done")
            nxt_ = gens[b_ + 1] if b_ + 1 < NB else None
            tail_done = False
            front_done = nxt_ is None
            while not (tail_done and front_done):
                if not tail_done:
                    try:
                        next(gens[b_])
                    except StopIteration:
                        tail_done = True
                if not front_done:
                    if next(nxt_) == "front_done":
                        front_done = True
        if en_gdn and st.gdn_s_out is not None:
            P.dma(st.gdn_s_out.rearrange("p (a b) -> p a b", a=2), S[:], reads=bS, writes=[st.bout3])
            P.dma(st.gdn_raw_out.rearrange("p (a b) -> p a b", a=6), rawT[:, :, 512:515], reads=[braw], writes=[st.bout4])


def host_inputs(inp):
    def ng(v):
        return np.ascontiguousarray(np.asarray(v, np.float32).reshape(KD, 128).T)
    normg = np.concatenate([ng(inp['mix_norm'][0]), ng(inp['ffn_norm'][0]), ng(inp['mix_norm'][1]), ng(inp['ffn_norm'][1])], axis=1)
    lp = [pack_layer_params(inp, l) for l in range(2)]
    f32 = lambda a: np.ascontiguousarray(np.asarray(a, np.float32))
    return dict(consts=make_consts(), normg=normg, final_norm=f32(inp['final_norm'])[None, :],
                w_in=f32(inp['w_in']), w_out=f32(inp['w_out']),
                lpa=np.stack([lp[0][0], lp[1][0]]), lps=np.stack([lp[0][1], lp[1][1]]),
                ffn_w_gate=f32(inp['ffn_w_gate'][0]), ffn_w_up=f32(inp['ffn_w_up'][0]), ffn_w_down=f32(inp['ffn_w_down'][0]),
                moe_router=f32(inp['moe_router'][0]), moe_w_gate=f32(inp['moe_w_gate'][0]), moe_w_up=f32(inp['moe_w_up'][0]),
                moe_w_down=f32(inp['moe_w_down'][0]))


_PROG_CACHE = {}


def kernel(**inputs):
    inp = {k: np.asarray(v) for k, v in inputs.items()}
    x = np.ascontiguousarray(inp["x"], dtype=np.float32)
    B, L, _ = x.shape
    T = L
    if T not in _PROG_CACHE:
        _PROG_CACHE[T] = build_program(T, split=True)[0]
    nc = _PROG_CACHE[T]
    shared = host_inputs(inp)
    n_cores = 2 * B
    in_maps = []
    for c in range(n_cores):
        m = dict(shared)
        m["x"] = x[c // 2]
        w = np.zeros((128, 2), np.float32)
        w[:, c % 2] = 1.0
        m["wsel"] = w
        in_maps.append(m)
    res = run_bass_kernel_spmd(nc, in_maps, core_ids=list(range(n_cores)))
    out = np.zeros((B, L, D), np.float32)
    for c in range(n_cores):
        out[c // 2, (c % 2) * (L // 2):(c % 2 + 1) * (L // 2)] = np.asarray(res.results[c]["out"], dtype=np.float32)
    return out
```
